# Optimizing a Trainium2 kernel written in Bass

```python
import math
import jax, jax.numpy as jnp
from jax import lax
import numpy as np

D_MODEL = 1024
BATCH = 4
SEQ = 4096
DEPTH = 4

MIX_WIDTH = D_MODEL
HEAD_DIM = 64
A_WIDTH = D_MODEL // 4
B_WIDTH = D_MODEL // 2
C_WIDTH = D_MODEL // 4
A_HEADS = A_WIDTH // HEAD_DIM
B_HEADS = B_WIDTH // HEAD_DIM
C_GROUPS = C_WIDTH // HEAD_DIM
IN_COLS = 2 * A_WIDTH + 3 * B_WIDTH + 2 * C_WIDTH
CHUNK = 128
MOBA_BLOCK = 256
MOBA_TOPK = 3
Q_BLK = 64
CONV_WIDTH = 31
MEM_LEN = 256
X_HEADS = 4
X_HEAD_DIM = D_MODEL // X_HEADS
D_FF = 4 * D_MODEL
EPS = 1e-6

kernel_name = "hymba_style_gmlp_moba_conformer_hybrid"


def rms_norm(x, g):
    xf = x.astype(jnp.float32)
    y = xf * lax.rsqrt(jnp.mean(xf * xf, axis=-1, keepdims=True) + EPS)
    return (y * g.astype(jnp.float32)).astype(x.dtype)


def layer_norm(x, g, b):
    xf = x.astype(jnp.float32)
    mu = jnp.mean(xf, axis=-1, keepdims=True)
    xc = xf - mu
    var = jnp.mean(xc * xc, axis=-1, keepdims=True)
    y = xc * lax.rsqrt(var + EPS) * g.astype(jnp.float32) + b.astype(jnp.float32)
    return y.astype(x.dtype)


def alibi_slopes(n_heads):
    return 2.0 ** (-8.0 * jnp.arange(1, n_heads + 1, dtype=jnp.float32) / n_heads)


def spatial_gating(z, ln_g, ln_b, w_s, b_s):
    Bn, S, _ = z.shape
    u, v = jnp.split(z, 2, axis=-1)
    v = layer_norm(v, ln_g, ln_b)
    v = v.reshape(Bn, S // CHUNK, CHUNK, A_HEADS, HEAD_DIM)
    causal = jnp.tril(jnp.ones((CHUNK, CHUNK), dtype=w_s.dtype))
    w = (w_s * causal[None]).astype(v.dtype)
    mixed = jnp.einsum('hts,bcshd->bcthd', w, v) + b_s.T.astype(v.dtype)[None, None, :, :, None]
    return u * mixed.reshape(Bn, S, A_WIDTH)


def moba_attention(q, k, v):
    Bn, H, S, Dh = q.shape
    s_pad = -(-S // MOBA_BLOCK) * MOBA_BLOCK
    pad = ((0, 0), (0, 0), (0, s_pad - S), (0, 0))
    q, k, v = jnp.pad(q, pad), jnp.pad(k, pad), jnp.pad(v, pad)
    nb = s_pad // MOBA_BLOCK
    kk = min(MOBA_TOPK, nb)
    n_q = s_pad // Q_BLK
    scale = Dh ** -0.5
    slopes = alibi_slopes(H)
    kb = k.reshape(Bn, H, nb, MOBA_BLOCK, Dh)
    vb = v.reshape(Bn, H, nb, MOBA_BLOCK, Dh)
    kmean = jnp.mean(kb.astype(jnp.float32), axis=3)
    qc = q.reshape(Bn, H, n_q, Q_BLK, Dh).transpose(2, 0, 1, 3, 4)
    bi = jnp.arange(Bn)[:, None, None, None]
    hi = jnp.arange(H)[None, :, None, None]
    offs = jnp.arange(MOBA_BLOCK)

    def one_block(args):
        qi, ci = args
        t = ci * Q_BLK + jnp.arange(Q_BLK)
        own = (ci * Q_BLK) // MOBA_BLOCK
        gate = jnp.einsum('bhqd,bhnd->bhqn', qi.astype(jnp.float32), kmean)
        gate = jnp.where(jnp.arange(nb) < own, gate, -jnp.inf)
        _, idx = lax.top_k(gate, kk)
        valid = idx < own
        ksel = kb[bi, hi, idx]
        vsel = vb[bi, hi, idx]
        s_past = jnp.einsum('bhqd,bhqkld->bhqkl', qi, ksel).astype(jnp.float32) * scale
        kpos = idx[..., None] * MOBA_BLOCK + offs
        dist = (t[None, None, :, None, None] - kpos).astype(jnp.float32)
        s_past = jnp.where(valid[..., None], s_past - slopes[None, :, None, None, None] * dist, -jnp.inf)
        k_own = lax.dynamic_index_in_dim(kb, own, axis=2, keepdims=False)
        v_own = lax.dynamic_index_in_dim(vb, own, axis=2, keepdims=False)
        s_own = jnp.einsum('bhqd,bhld->bhql', qi, k_own).astype(jnp.float32) * scale
        kpos_own = own * MOBA_BLOCK + offs
        dist_own = (t[:, None] - kpos_own[None, :]).astype(jnp.float32)
        s_own = jnp.where((dist_own >= 0)[None, None],
                          s_own - slopes[None, :, None, None] * dist_own[None, None], -jnp.inf)
        s_all = jnp.concatenate([s_past.reshape(Bn, H, Q_BLK, kk * MOBA_BLOCK), s_own], axis=-1)
        p = jax.nn.softmax(s_all, axis=-1).astype(v.dtype)
        p_past = p[..., :kk * MOBA_BLOCK].reshape(Bn, H, Q_BLK, kk, MOBA_BLOCK)
        p_own = p[..., kk * MOBA_BLOCK:]
        return (jnp.einsum('bhqkl,bhqkld->bhqd', p_past, vsel)
                + jnp.einsum('bhql,bhld->bhqd', p_own, v_own))

    out = lax.map(one_block, (qc, jnp.arange(n_q, dtype=jnp.int32)))
    out = out.transpose(1, 0, 3, 2, 4).reshape(Bn, s_pad, H * Dh)
    return out[:, :S]


def conformer_conv(z, w_dw, b_dw, gn_g, gn_b):
    Bn, S, _ = z.shape
    a, g = jnp.split(z, 2, axis=-1)
    y = a * jax.nn.sigmoid(g)
    rhs = w_dw.astype(y.dtype)[:, None, :]
    y = lax.conv_general_dilated(y, rhs, window_strides=(1,), padding=[(CONV_WIDTH - 1, 0)],
                                 dimension_numbers=('NWC', 'WIO', 'NWC'),
                                 feature_group_count=C_WIDTH)
    y = y + b_dw.astype(y.dtype)
    y = y.reshape(Bn, S, C_GROUPS, HEAD_DIM)
    y = layer_norm(y, gn_g.reshape(C_GROUPS, HEAD_DIM), gn_b.reshape(C_GROUPS, HEAD_DIM))
    return jax.nn.silu(y.reshape(Bn, S, C_WIDTH))


def memory_cross_attention(h, m, w_q, w_kv, w_o):
    Bn, S, _ = h.shape
    M = m.shape[1]
    q = (h @ w_q).reshape(Bn, S, X_HEADS, X_HEAD_DIM)
    k, v = jnp.split(m @ w_kv, 2, axis=-1)
    k = k.reshape(Bn, M, X_HEADS, X_HEAD_DIM)
    v = v.reshape(Bn, M, X_HEADS, X_HEAD_DIM)
    s = jnp.einsum('bshd,bmhd->bhsm', q, k).astype(jnp.float32) * (X_HEAD_DIM ** -0.5)
    p = jax.nn.softmax(s, axis=-1).astype(v.dtype)
    o = jnp.einsum('bhsm,bmhd->bshd', p, v).reshape(Bn, S, D_MODEL)
    return o @ w_o


def setup_inputs(seed: int = 0) -> dict:
    key = jax.random.key(seed)
    ks = jax.random.split(key, 24)
    f32 = jnp.float32
    L = DEPTH

    def nrm(k, shape, scale):
        return jax.random.normal(k, shape, f32) * scale

    def gain(k, shape):
        return 1.0 + 0.05 * jax.random.normal(k, shape, f32)

    return {
        "x": nrm(ks[0], (BATCH, SEQ, D_MODEL), 1.0),
        "mem": nrm(ks[1], (BATCH, MEM_LEN, D_MODEL), 1.0),
        "pre_mix_g": gain(ks[2], (L, D_MODEL)),
        "w_in": nrm(ks[3], (L, D_MODEL, IN_COLS), D_MODEL ** -0.5),
        "gate_ln_g": gain(ks[4], (L, A_WIDTH)),
        "gate_ln_b": nrm(ks[5], (L, A_WIDTH), 0.02),
        "w_s": nrm(ks[6], (L, A_HEADS, CHUNK, CHUNK), CHUNK ** -0.5),
        "b_s": nrm(ks[7], (L, A_HEADS, CHUNK), 0.02),
        "w_dw": nrm(ks[8], (L, CONV_WIDTH, C_WIDTH), CONV_WIDTH ** -0.5),
        "b_dw": nrm(ks[9], (L, C_WIDTH), 0.02),
        "conv_gn_g": gain(ks[10], (L, C_WIDTH)),
        "conv_gn_b": nrm(ks[11], (L, C_WIDTH), 0.02),
        "w_out": nrm(ks[12], (L, MIX_WIDTH, D_MODEL), MIX_WIDTH ** -0.5),
        "post_mix_g": gain(ks[13], (L, D_MODEL)),
        "pre_x_g": gain(ks[14], (L, D_MODEL)),
        "mem_g": gain(ks[15], (L, D_MODEL)),
        "w_xq": nrm(ks[16], (L, D_MODEL, D_MODEL), D_MODEL ** -0.5),
        "w_xkv": nrm(ks[17], (L, D_MODEL, 2 * D_MODEL), D_MODEL ** -0.5),
        "w_xo": nrm(ks[18], (L, D_MODEL, D_MODEL), D_MODEL ** -0.5),
        "post_x_g": gain(ks[19], (L, D_MODEL)),
        "pre_ffn_g": gain(ks[20], (L, D_MODEL)),
        "w_ff1": nrm(ks[21], (L, D_MODEL, D_FF), D_MODEL ** -0.5),
        "w_ff2": nrm(ks[22], (L, D_FF, D_MODEL), D_FF ** -0.5),
        "post_ffn_g": gain(ks[23], (L, D_MODEL)),
    }


def reference(x, mem, pre_mix_g, w_in, gate_ln_g, gate_ln_b, w_s, b_s, w_dw, b_dw,
              conv_gn_g, conv_gn_b, w_out, post_mix_g, pre_x_g, mem_g, w_xq, w_xkv,
              w_xo, post_x_g, pre_ffn_g, w_ff1, w_ff2, post_ffn_g):
    Bn, S, _ = x.shape
    split_a = 2 * A_WIDTH
    split_b = split_a + 3 * B_WIDTH
    for l in range(DEPTH):
        h = rms_norm(x, pre_mix_g[l])
        z = h @ w_in[l]
        za, zb, zc = z[..., :split_a], z[..., split_a:split_b], z[..., split_b:]
        ya = spatial_gating(jax.nn.gelu(za, approximate=False),
                            gate_ln_g[l], gate_ln_b[l], w_s[l], b_s[l])
        q, k, v = jnp.split(zb, 3, axis=-1)
        q, k, v = (a.reshape(Bn, S, B_HEADS, HEAD_DIM).transpose(0, 2, 1, 3) for a in (q, k, v))
        yb = moba_attention(q, k, v)
        yc = conformer_conv(zc, w_dw[l], b_dw[l], conv_gn_g[l], conv_gn_b[l])
        y = jnp.concatenate([ya, yb, yc], axis=-1) @ w_out[l]
        x = x + rms_norm(y, post_mix_g[l])
        h = rms_norm(x, pre_x_g[l])
        m = rms_norm(mem, mem_g[l])
        x = x + rms_norm(memory_cross_attention(h, m, w_xq[l], w_xkv[l], w_xo[l]), post_x_g[l])
        h = rms_norm(x, pre_ffn_g[l])
        f = jnp.square(jax.nn.relu(h @ w_ff1[l])) @ w_ff2[l]
        x = x + rms_norm(f, post_ffn_g[l])
    return x
```

```python
import numpy as np
import ml_dtypes
from contextlib import ExitStack
import concourse.bass as bass
import concourse.mybir as mybir
from concourse.bass_utils import run_bass_kernel_spmd

F32 = mybir.dt.float32
BF16 = mybir.dt.bfloat16
AF = mybir.ActivationFunctionType
ALU = mybir.AluOpType
AX = mybir.AxisListType

D = 1024
NCH = 8
T = 2048
NT = 4
TT = 512
SEQ = 4096
DEPTH = 4
EPS = 1e-6
NEG = -30000.0
NSLOT = 6

GV_PRE_MIX, GV_POST_MIX, GV_PRE_X, GV_MEM, GV_POST_X, GV_PRE_FFN, GV_POST_FFN = 0, 8, 16, 24, 32, 40, 48
GV_LN_G, GV_LN_B, GV_BDW, GV_GN_G, GV_GN_B, GV_WDW = 56, 58, 60, 62, 64, 66
NGV = 66 + 62


class Buf:
    __slots__ = ("name", "w", "rd", "sem", "cnt")

    def __init__(self, name):
        self.name = name
        self.w = None
        self.rd = {}
        self.sem = None
        self.cnt = 0


class Sched:
    def __init__(self, nc, es):
        self.nc = nc
        self.es = es
        self.engs = {"pe": nc.tensor, "act": nc.scalar, "dve": nc.vector, "pool": nc.gpsimd, "sp": nc.sync}
        self.esem = {}
        self.ecnt = {}
        for e in ("pe", "act", "dve", "pool"):
            self.esem[e] = es.enter_context(nc.semaphore("sem_" + e))
            self.ecnt[e] = 0
        self.known = {e: {} for e in self.engs}
        self.dma_sems = {}
        self.nsem = 0
        import os
        self.nops = 0
        self.limit = int(os.environ.get('OPCUT', '0'))

    def buf(self, name):
        return Buf(name)

    def _collect(self, eng, r, w):
        need = {}

        def add(tok):
            if tok is None:
                return
            s, v, src = tok
            if src == "pe" and eng == "pe":
                return
            if need.get(s, 0) < v:
                need[s] = v

        for b in r:
            add(b.w)
        for b in w:
            add(b.w)
            for s, (v, src) in b.rd.items():
                add((s, v, src))
        return need

    def _wait(self, eng, need):
        kn = self.known[eng]
        e = self.engs[eng]
        for s, v in need.items():
            if kn.get(s, 0) < v:
                e.wait_ge(s, v)
                kn[s] = v

    def _record(self, tok, r, w):
        s, v, src = tok
        for b in r:
            old = b.rd.get(s)
            if old is None or old[0] < v:
                b.rd[s] = (v, src)
        for b in w:
            b.w = tok
            b.rd = {}

    def op(self, eng, fn, r=(), w=()):
        self.nops += 1
        if self.limit and self.nops > self.limit:
            return None
        need = self._collect(eng, r, w)
        self._wait(eng, need)
        ins = fn()
        self.ecnt[eng] += 1
        s = self.esem[eng]
        ins.then_inc(s, 1)
        tok = (s, self.ecnt[eng], eng)
        self._record(tok, r, w)
        return tok

    def dma(self, queue, fn, r=(), w=(), semkey=None, persistent=False, inc=16, extra=()):
        self.nops += 1
        if self.limit and self.nops > self.limit:
            return None
        need = self._collect("dma_" + queue, r, w)
        for tk in extra:
            if tk is not None and need.get(tk[0], 0) < tk[1]:
                need[tk[0]] = tk[1]
        self._wait(queue, need)
        kb = semkey if semkey is not None else w[0]
        if kb.sem is None:
            kb.sem = self.es.enter_context(self.nc.semaphore("dsem%d" % self.nsem))
            self.nsem += 1
            self.dma_sems[kb.sem] = [0, persistent]
        ins = fn()
        kb.cnt += inc
        ins.then_inc(kb.sem, inc)
        self.dma_sems[kb.sem][0] = kb.cnt
        tok = (kb.sem, kb.cnt, "dma")
        self._record(tok, r, w)
        return tok

    def barrier(self):
        need = {}
        for e in ("pe", "act", "dve", "pool"):
            if self.ecnt[e] > 0:
                need[self.esem[e]] = self.ecnt[e]
        for s, (c, pers) in self.dma_sems.items():
            if c > 0 and not pers:
                need[s] = c
        for e in ("pe", "act", "dve", "sp"):
            self._wait(e, need)

    def final_wait(self, eng, toks):
        need = {}
        toks = [t for t in toks if t is not None]
        for (s, v, _) in toks:
            need[s] = max(need.get(s, 0), v)
        self._wait(eng, need)


class GV:
    def __init__(self, t, l):
        self.t, self.l = t, l

    def __getitem__(self, idx):
        rows, cols = idx
        return self.t[rows, self.l, cols]


class StopBuild(Exception):
    pass


class Builder:
    def __init__(self, mode, stop_after=None):
        self.stop_after = stop_after
        self.mode = mode
        self.nc = bass.Bass("TRN2", target_bir_lowering=False)
        self.es = ExitStack()

    def sb(self, name, shape, dt, es=None):
        self.nsb = getattr(self, "nsb", 0) + 1
        return (es or self.es).enter_context(self.nc.sbuf_tensor("s_%s_%d" % (name, self.nsb), shape, dt))

    def din(self, name, shape, dt=F32):
        return self.nc.dram_tensor(name, shape, dt, kind="ExternalInput").ap()

    def dout(self, name, shape, dt=F32):
        return self.nc.dram_tensor(name, shape, dt, kind="ExternalOutput").ap()

    def bank(self):
        i = self.bank_i
        self.bank_i = (i + 1) % self.nrot
        return i

    def ring_load(self, src_ap, shape3):
        nc, S = self.nc, self.S
        i = self.ring_i
        self.ring_i = (i + 1) % NSLOT
        a, b = shape3
        view = self.ring[:, i, 0:a * b].rearrange("p (a b) -> p a b", a=a)
        buf = self.ring_b[i]
        if self.mode != "fused":
            S.dma("pool", lambda: nc.gpsimd.dma_start(out=view, in_=src_ap), w=[buf], persistent=True)
            return view, buf
        for k8 in range(8):
            j = self.stg_i
            self.stg_i = 1 - j
            row, col = (k8 * 512) // b, (k8 * 512) % b
            S.dma("sp", lambda j=j, row=row, col=col: nc.sync.dma_start(out=self.stg[:, j, :],
                                                                        in_=src_ap[:, row, col:col + 512]),
                  w=[self.stgb[j]], persistent=True)
            S.op("pool", lambda j=j, k8=k8, i=i: nc.gpsimd.tensor_copy(out=self.ring[:, i, k8 * 512:(k8 + 1) * 512],
                                                                       in_=self.stg[:, j, :]),
                 r=[self.stgb[j]], w=[buf])
        return view, buf

    def rms_rstd(self, src_ap, src_bufs, n, sq_t, sq_b, rstd_t, rstd_b, nch=NCH, scale=1.0 / D):
        nc, S = self.nc, self.S
        for c in range(nch):
            S.op("act", lambda c=c: nc.scalar.activation(out=sq_t[:, c, 0:n], in_=src_ap[:, c, :], func=AF.Square),
                 r=src_bufs, w=[sq_b])
        bk = self.bank()
        ps = self.ps[:, bk, 0:n]

        def mm():
            for c in range(nch):
                ins = nc.tensor.matmul(ps, lhsT=self.ones_bf[:, :], rhs=sq_t[:, c, 0:n],
                                       start=(c == 0), stop=(c == nch - 1))
            return ins
        S.op("pe", mm, r=[sq_b, self.cb], w=[self.pb[bk]])
        S.op("dve", lambda: nc.vector.tensor_scalar(out=rstd_t[:, 0:n], in0=ps, scalar1=scale, scalar2=EPS,
                                                    op0=ALU.mult, op1=ALU.add), r=[self.pb[bk]], w=[rstd_b])
        S.op("act", lambda: nc.scalar.activation(out=rstd_t[:, 0:n], in_=rstd_t[:, 0:n], func=AF.Sqrt),
             r=[rstd_b], w=[rstd_b])
        import os
        if os.environ.get('KVCUT') == '3':
            raise StopBuild()
        S.op("dve", lambda: nc.vector.reciprocal(out=rstd_t[:, 0:n], in_=rstd_t[:, 0:n]), r=[rstd_b], w=[rstd_b])
        if os.environ.get('KVCUT') == '4':
            raise StopBuild()

    def normalize(self, src_fn, src_bufs, gcol, out_fn, out_bufs, rstd_t, rstd_b, n, nch=NCH):
        nc, S = self.nc, self.S
        for c in range(nch):
            S.op("dve", lambda c=c: nc.vector.scalar_tensor_tensor(
                out=out_fn(c), in0=src_fn(c), scalar=self.gv[:, gcol + c:gcol + c + 1], in1=rstd_t[:, 0:n],
                op0=ALU.mult, op1=ALU.mult), r=list(src_bufs) + [rstd_b, self.cb], w=out_bufs)

    def post_norm_residual(self, y_t, y_b, tt, gcol, sq_t, sq_b, rstd_t, rstd_b):
        nc, S = self.nc, self.S
        self.rms_rstd(y_t[:, :, :], [y_b], TT, sq_t, sq_b, rstd_t, rstd_b)
        sl = slice(tt * TT, (tt + 1) * TT)
        for c in range(NCH):
            S.op("dve", lambda c=c: nc.vector.tensor_tensor(out=y_t[:, c, :], in0=y_t[:, c, :], in1=rstd_t[:, :],
                                                            op=ALU.mult), r=[y_b, rstd_b], w=[y_b])
            S.op("dve", lambda c=c: nc.vector.scalar_tensor_tensor(
                out=self.x[:, c, sl], in0=y_t[:, c, :], scalar=self.gv[:, gcol + c:gcol + c + 1],
                in1=self.x[:, c, sl], op0=ALU.mult, op1=ALU.add), r=[y_b, self.cb, self.xb[tt]], w=[self.xb[tt]])

    def proj_fm(self, w_view, w_buf, col0, h_fn, h_bufs, n, kch=NCH):
        nc, S = self.nc, self.S
        bk = self.bank()
        ps = self.ps[:, bk, 0:n]

        def mm():
            for k in range(kch):
                ins = nc.tensor.matmul(ps, lhsT=w_view[:, k, col0:col0 + 128], rhs=h_fn(k),
                                       start=(k == 0), stop=(k == kch - 1))
            return ins
        S.op("pe", mm, r=[w_buf] + list(h_bufs), w=[self.pb[bk]])
        return bk

    def proj_tm(self, w_view, w_buf, col0, ncols, h_fn, h_bufs, kch=NCH):
        nc, S = self.nc, self.S
        bk = self.bank()
        ps = self.ps[:, bk, 0:ncols]

        def mm():
            for k in range(kch):
                ins = nc.tensor.matmul(ps, lhsT=h_fn(k), rhs=w_view[:, k, col0:col0 + ncols],
                                       start=(k == 0), stop=(k == kch - 1))
            return ins
        S.op("pe", mm, r=[w_buf] + list(h_bufs), w=[self.pb[bk]])
        return bk

    def build_fused(self, nlayers=DEPTH, ncores=8):
        nc, es = self.nc, self.es
        with es:
            self.S = S = Sched(nc, es)
            self.bank_i = 0
            self.nrot = 8
            self.ring_i = 0
            NL = nlayers
            xT_d = self.din("xT", [128, NCH, T])
            memT_d = self.din("memT", [128, NCH, 256])
            gv_d = self.din("gv", [128, DEPTH, NGV])
            ones_d = self.din("ones_bf", [128, 128], BF16)
            gb_d = self.din("gb", [128, 16])
            hsc_d = self.din("hsc", [128, 1])
            wsT_d = self.din("wsT", [DEPTH, 128, 4, 128])
            bsb_d = self.din("bsb", [DEPTH, 128, 4, 128])
            w_in_d = self.din("w_in", [DEPTH, D, 2560])
            w_out_d = self.din("w_out", [DEPTH, D, D])
            w_xq_d = self.din("w_xq", [DEPTH, D, D])
            w_xkv_d = self.din("w_xkv", [DEPTH, D, 2 * D])
            w_xo_d = self.din("w_xo", [DEPTH, D, D])
            w_ff1_d = self.din("w_ff1", [DEPTH, D, 4 * D])
            w_ff2_d = self.din("w_ff2", [DEPTH, 4 * D, D])
            ident_d = self.din("ident", [128, 128])
            blk_d = self.din("blkones", [128, 128], BF16)
            tril_d = self.din("trilT", [128, 128])
            cmask_d = self.din("cmask", [128, 4, 512], BF16)
            kbt_d = self.din("kbt", [64, SEQ], BF16)
            qbs_d = self.din("qbs", [64, 4, T], BF16)
            x_o = self.dout("x_o", [128, NCH, T])
            CW = [1024] * 8 + [48]
            xin = [[nc.dram_tensor("xin%d_%d" % (i, j), [128, CW[j]], F32).ap() for j in range(9)] for i in range(2)]
            gath = [[nc.dram_tensor("gath%d_%d" % (i, j), [256, CW[j]], F32).ap() for j in range(9)] for i in range(2)]
            xin_b = [[S.buf("xin%d_%d" % (i, j)) for j in range(9)] for i in range(2)]
            gath_b = [[S.buf("gath%d_%d" % (i, j)) for j in range(9)] for i in range(2)]
            groups = [[2 * i, 2 * i + 1] for i in range(ncores // 2)]

            self.x = self.sb("x", [128, NCH, T], F32)
            self.xb = [S.buf("x%d" % i) for i in range(NT)]
            gvall = self.sb("gvall", [128, DEPTH, NGV], F32)
            self.ones_bf = self.sb("ones", [128, 128], BF16)
            self.eps_t = self.sb("eps", [128, 1], F32)
            self.cb = S.buf("consts")
            self.ring = self.sb("ring", [128, NSLOT, 4096], BF16)
            self.ring_b = [S.buf("ring%d" % i) for i in range(NSLOT)]
            self.stg = self.sb("stg", [128, 2, 512], F32)
            self.stgb = [S.buf("stg0"), S.buf("stg1")]
            self.stg_i = 0
            self.ps = es.enter_context(nc.psum_tensor("ps", [128, 8, 512], F32))
            self.pb = [S.buf("bank%d" % i) for i in range(8)]
            for tt in range(NT):
                S.dma("sp", lambda tt=tt: nc.sync.dma_start(out=self.x[:, :, tt * TT:(tt + 1) * TT],
                                                            in_=xT_d[:, :, tt * TT:(tt + 1) * TT]), w=[self.xb[tt]])
            S.dma("sp", lambda: nc.sync.dma_start(out=gvall[:, :, :], in_=gv_d), w=[self.cb])
            S.dma("sp", lambda: nc.sync.dma_start(out=self.ones_bf[:, :], in_=ones_d), w=[self.cb])
            S.op("dve", lambda: nc.vector.memset(self.eps_t[:, :], EPS), w=[self.cb])
            for l in range(NL):
                L = {"layer": l, "nlayers": NL, "x_o": x_o, "gv": GV(gvall, l),
                     "w_in_v": w_in_d[l].rearrange("(k p) n -> p k n", p=128),
                     "w_out_d": w_out_d[l], "w_xq_d": w_xq_d[l], "w_xkv_d": w_xkv_d[l], "w_xo_d": w_xo_d[l],
                     "w_ff1_d": w_ff1_d[l], "w_ff2_d": w_ff2_d[l], "wsT_d": wsT_d[l], "bsb_d": bsb_d[l],
                     "ident_d": ident_d, "blk_d": blk_d, "tril_d": tril_d, "cmask_d": cmask_d, "kbt_d": kbt_d,
                     "qbs_d": qbs_d, "gb_d": gb_d, "hsc_d": hsc_d, "memT_d": memT_d,
                     "xin": xin, "gath": gath, "xin_b": xin_b, "gath_b": gath_b, "groups": groups}
                self.build_main(L)
            print("fused nops", S.nops, "nsem", S.nsem)
        return nc

    def build(self):
        nc, es = self.nc, self.es
        mode = self.mode
        with es:
            self.S = S = Sched(nc, es)
            import os
            self.bank_i = int(os.environ.get('BANKSHIFT', '0'))
            self.nrot = 8
            self.ring_i = 0
            self.okey = None
            xT_d = self.din("xT", [128, NCH, T])
            gv_d = self.din("gv", [128, NGV])
            w_in_d = self.din("w_in", [D, 2560])
            ones_d = self.din("ones_bf", [128, 128], BF16)
            if mode == "kv":
                kt_o = self.dout("kt_o", [128, 4, T], BF16)
                v_o = self.dout("v_o", [128, 16, 512], BF16)
                ks_o = self.dout("ks_o", [128, 4, 8])
                yh_o = self.dout("yh_o", [128, 2, 32], BF16)
            else:
                memT_d = self.din("memT", [128, NCH, 256])
                kt_d = self.din("kt_rel", [128, 4, SEQ], BF16)
                v_d = self.din("v_rel", [128, 32, 512], BF16)
                ks_d = self.din("ks_rel", [128, 4, 16])
                yh_d = self.din("yh_in", [128, 2, 32], BF16)
                gb_d = self.din("gb", [128, 16])
                wsT_d = self.din("wsT", [128, 4, 128])
                bsb_d = self.din("bsb", [128, 4, 128])
                w_out_d = self.din("w_out", [D, D])
                w_xq_d = self.din("w_xq", [D, D])
                w_xkv_d = self.din("w_xkv", [D, 2 * D])
                w_xo_d = self.din("w_xo", [D, D])
                w_ff1_d = self.din("w_ff1", [D, 4 * D])
                w_ff2_d = self.din("w_ff2", [4 * D, D])
                ident_d = self.din("ident", [128, 128])
                blk_d = self.din("blkones", [128, 128], BF16)
                tril_d = self.din("trilT", [128, 128])
                cmask_d = self.din("cmask", [128, 4, 512], BF16)
                kbt_d = self.din("kbt", [64, SEQ], BF16)
                qbs_d = self.din("qbs", [64, 4, T], BF16)
                x_o = self.dout("x_o", [128, NCH, T])
                if self.stop_after in ("p1", "p2a"):
                    dbg_o = self.dout("dbg_o", [128, 8, T], BF16)

            self.x = self.sb("x", [128, NCH, T], F32)
            self.xb = [S.buf("x%d" % i) for i in range(NT)]
            self.gv = self.sb("gv", [128, NGV], F32)
            self.ones_bf = self.sb("ones", [128, 128], BF16)
            self.eps_t = self.sb("eps", [128, 1], F32)
            self.cb = S.buf("consts")
            self.ring = self.sb("ring", [128, NSLOT, 4096], BF16)
            self.ring_b = [S.buf("ring%d" % i) for i in range(NSLOT)]
            self.ps = es.enter_context(nc.psum_tensor("ps", [128, 8, 512], F32))
            self.pb = [S.buf("bank%d" % i) for i in range(8)]

            for tt in range(NT):
                S.dma("sp", lambda tt=tt: nc.sync.dma_start(out=self.x[:, :, tt * TT:(tt + 1) * TT],
                                                            in_=xT_d[:, :, tt * TT:(tt + 1) * TT]), w=[self.xb[tt]])
            S.dma("sp", lambda: nc.sync.dma_start(out=self.gv[:, :], in_=gv_d), w=[self.cb])
            S.dma("sp", lambda: nc.sync.dma_start(out=self.ones_bf[:, :], in_=ones_d), w=[self.cb])
            S.op("dve", lambda: nc.vector.memset(self.eps_t[:, :], EPS), w=[self.cb])
            w_in_v = w_in_d.rearrange("(k p) n -> p k n", p=128)
            import os
            if os.environ.get('KVCUT') == '2':
                S.barrier()
                return nc

            if mode == "kv":
                try:
                    self.build_kv(w_in_v, kt_o, v_o, ks_o, yh_o)
                except StopBuild:
                    S.barrier()
            else:
                self.build_main(locals())
        return nc

    def p1_norm(self, les, gcol):
        nc, S = self.nc, self.S
        self.hT = self.sb("hT", [128, NCH, T], BF16, les)
        self.hb = [S.buf("h%d" % i) for i in range(NT)]
        self.sq = self.sb("sq", [128, NCH, TT], BF16, les)
        self.sqb = S.buf("sq")
        self.rstd = self.sb("rstd", [128, TT], F32, les)
        self.rstdb = S.buf("rstd")
        for tt in range(NT):
            sl = slice(tt * TT, (tt + 1) * TT)
            self.rms_rstd(self.x[:, :, sl], [self.xb[tt]], TT, self.sq, self.sqb, self.rstd, self.rstdb)
            self.normalize(lambda c: self.x[:, c, sl], [self.xb[tt]], gcol, lambda c: self.hT[:, c, sl],
                           [self.hb[tt]], self.rstd, self.rstdb, TT)

    def h_fn(self, tt, lo=0, n=TT):
        return lambda k: self.hT[:, k, tt * TT + lo:tt * TT + lo + n]

    def glu_block(self, wv, wb, tt, lo, n, y_out_ap, y_bufs, sig_t, sig_b):
        nc, S = self.nc, self.S
        for c in range(2):
            bg = self.proj_fm(wv, wb, 256 + 128 * c, self.h_fn(tt, lo, n), [self.hb[tt]], n)
            S.op("act", lambda bg=bg: nc.scalar.activation(out=sig_t[:, 0:n], in_=self.ps[:, bg, 0:n],
                                                           func=AF.Sigmoid), r=[self.pb[bg]], w=[sig_b])
            ba = self.proj_fm(wv, wb, 128 * c, self.h_fn(tt, lo, n), [self.hb[tt]], n)
            S.op("dve", lambda ba=ba, c=c: nc.vector.tensor_tensor(out=y_out_ap(c), in0=self.ps[:, ba, 0:n],
                                                                   in1=sig_t[:, 0:n], op=ALU.mult),
                 r=[self.pb[ba], sig_b], w=y_bufs)

    def build_kv(self, w_in_v, kt_o, v_o, ks_o, yh_o):
        nc, S = self.nc, self.S
        with ExitStack() as les:
            self.p1_norm(les, GV_PRE_MIX)
            import os
            if os.environ.get('KVCUT') == '1':
                S.barrier()
                return
            kst = self.sb("kst", [128, 4, TT], BF16, les)
            kstb = S.buf("kst")
            ksum = self.sb("ksum", [128, 4, 8], F32, les)
            ksumb = S.buf("ksum")
            vst = self.sb("vst", [128, 4, 512], BF16, les)
            vstb = S.buf("vst")
            sig = self.sb("sig", [128, TT], F32, les)
            sigb = S.buf("sig")
            yh = self.sb("yh", [128, 2, 32], BF16, les)
            yhb = S.buf("yh")
            outs = []
            wv, wb = self.ring_load(w_in_v[:, :, 1024:1536], (8, 512))
            for tt in range(NT):
                for c in range(4):
                    bk = self.proj_fm(wv, wb, 128 * c, self.h_fn(tt), [self.hb[tt]], TT)
                    S.op("act", lambda bk=bk, c=c: nc.scalar.copy(out=kst[:, c, :], in_=self.ps[:, bk, :]),
                         r=[self.pb[bk]], w=[kstb, self.pb[bk]])
                    if os.environ.get('KVCUT') == '5':
                        S.barrier()
                        return
                    S.op("dve", lambda bk=bk, c=c, tt=tt: nc.vector.tensor_reduce(
                        out=ksum[:, c, 2 * tt:2 * tt + 2], in_=self.ps[:, bk, :].rearrange("p (a b) -> p a b", a=2),
                        axis=AX.X, op=ALU.add), r=[self.pb[bk]], w=[ksumb])
                outs.append(S.dma("sp", lambda tt=tt: nc.sync.dma_start(out=kt_o[:, :, tt * TT:(tt + 1) * TT],
                                                                        in_=kst[:, :, :]), r=[kstb],
                                  semkey=kstb))
            outs.append(S.dma("sp", lambda: nc.sync.dma_start(out=ks_o, in_=ksum[:, :, :]), r=[ksumb],
                              semkey=ksumb))
            wv, wb = self.ring_load(w_in_v[:, :, 1536:2048], (8, 512))
            for tt in range(NT):
                for s in range(4):
                    bk = self.proj_tm(wv, wb, 0, 512, self.h_fn(tt, 128 * s, 128), [self.hb[tt]])
                    S.op("act", lambda bk=bk, s=s: nc.scalar.copy(out=vst[:, s, :], in_=self.ps[:, bk, :]),
                         r=[self.pb[bk]], w=[vstb])
                outs.append(S.dma("sp", lambda tt=tt: nc.sync.dma_start(out=v_o[:, 4 * tt:4 * tt + 4, :],
                                                                        in_=vst[:, :, :]), r=[vstb],
                                  semkey=vstb))
            wv, wb = self.ring_load(w_in_v[:, :, 2048:2560], (8, 512))
            self.glu_block(wv, wb, 3, TT - 32, 32, lambda c: yh[:, c, :], [yhb], sig, sigb)
            outs.append(S.dma("sp", lambda: nc.sync.dma_start(out=yh_o, in_=yh[:, :, :]), r=[yhb], semkey=yhb))
            S.final_wait("sp", outs)
            if S.limit:
                S.barrier()
            print('KV nops', S.nops)


    def post_norm_res(self, y3, y_b, tt, gcol, sq_t, sq_b, rstd_t, rstd_b):
        nc, S = self.nc, self.S
        self.rms_rstd(y3, [y_b], TT, sq_t, sq_b, rstd_t, rstd_b)
        sl = slice(tt * TT, (tt + 1) * TT)
        for c in range(NCH):
            S.op("dve", lambda c=c: nc.vector.tensor_tensor(out=y3[:, c, :], in0=y3[:, c, :], in1=rstd_t[:, :],
                                                            op=ALU.mult), r=[y_b, rstd_b], w=[y_b])
            S.op("dve", lambda c=c: nc.vector.scalar_tensor_tensor(
                out=self.x[:, c, sl], in0=y3[:, c, :], scalar=self.gv[:, gcol + c:gcol + c + 1],
                in1=self.x[:, c, sl], op0=ALU.mult, op1=ALU.add), r=[y_b, self.cb, self.xb[tt]], w=[self.xb[tt]])

    def evac(self, bk, out_ap, out_bufs, n=TT, func=None, scale=1.0, eng="act"):
        nc, S = self.nc, self.S
        if eng == "act":
            S.op("act", lambda: nc.scalar.activation(out=out_ap, in_=self.ps[:, bk, 0:n],
                                                     func=(func or AF.Copy), scale=scale),
                 r=[self.pb[bk]], w=out_bufs)
        else:
            S.op("dve", lambda: nc.vector.tensor_copy(out=out_ap, in_=self.ps[:, bk, 0:n]),
                 r=[self.pb[bk]], w=out_bufs)

    def build_main(self, L):
        nc, S = self.nc, self.S
        x_o = L["x_o"]
        w_in_v = L["w_in_v"]
        if "gv" in L:
            self.gv = L["gv"]
        vw = lambda d: d.rearrange("(k p) n -> p k n", p=128)
        w_out_v, w_xq_v, w_xkv_v, w_xo_v = vw(L["w_out_d"]), vw(L["w_xq_d"]), vw(L["w_xkv_d"]), vw(L["w_xo_d"])
        w_ff1_v, w_ff2_v = vw(L["w_ff1_d"]), vw(L["w_ff2_d"])
        fused = self.mode == "fused"
        lyr = L.get("layer", 0)
        if not getattr(self, "consts_done", False):
            self.consts_done = True
            self.ident = self.sb("ident", [128, 128], F32)
            self.blk = self.sb("blk", [128, 128], BF16)
            self.onesE = self.sb("onesE", [128, 128], BF16)
            self.onesO = self.sb("onesO", [128, 128], BF16)
            self.gbt = self.sb("gb", [128, 16], F32)
            self.hsc = self.sb("hsc", [128, 1], F32)
            S.dma("sp", lambda: nc.sync.dma_start(out=self.ident[:, :], in_=L["ident_d"]), w=[self.cb])
            S.dma("sp", lambda: nc.sync.dma_start(out=self.blk[:, :], in_=L["blk_d"]), w=[self.cb])
            S.dma("sp", lambda: nc.sync.dma_start(out=self.gbt[:, :], in_=L["gb_d"]), w=[self.cb])
            if fused:
                S.dma("sp", lambda: nc.sync.dma_start(out=self.hsc[:, :], in_=L["hsc_d"]), w=[self.cb])
            S.op("dve", lambda: nc.vector.memset(self.onesE[:, :], 0.0), w=[self.cb])
            S.op("dve", lambda: nc.vector.memset(self.onesO[:, :], 0.0), w=[self.cb])
            S.op("dve", lambda: nc.vector.memset(self.onesE[:, 0:64], 1.0), w=[self.cb])
            S.op("dve", lambda: nc.vector.memset(self.onesO[:, 64:128], 1.0), w=[self.cb])
        ident, blk, onesE, onesO, gb = self.ident, self.blk, self.onesE, self.onesO, self.gbt
        if fused:
            par = lyr % 2
            xin, gath = L["xin"][par], L["gath"][par]
            xin_b, gath_b = L["xin_b"][par], L["gath_b"][par]
            xb16 = [a.bitcast(BF16) for a in xin]
            gb16 = [a.bitcast(BF16)[0:128, :] for a in gath]
            xinK = lambda c: xb16[c]
            xinV = lambda t_: xb16[4 + t_].rearrange("p (s f) -> p s f", s=4)
            xinS = xb16[8][:, 0:32].rearrange("p (c b) -> p c b", c=4)
            xinH = xb16[8][:, 32:96].rearrange("p (c j) -> p c j", c=2)
            g0K = lambda c: gb16[c]
            g0V = lambda t_: gb16[4 + t_].rearrange("p (s f) -> p s f", s=4)
            g0S = gb16[8][:, 0:32].rearrange("p (c b) -> p c b", c=4)
            g0H = gb16[8][:, 32:96].rearrange("p (c j) -> p c j", c=2)

        with ExitStack() as lay:
            yaT = self.sb("yaT", [128, 2, T], BF16, lay)
            qT = self.sb("qT", [128, 4, T], BF16, lay)
            ycT = self.sb("ycT", [128, 2, T], BF16, lay)
            yab = [S.buf("ya%d" % i) for i in range(NT)]
            qb_ = [[S.buf("q%d_%d" % (i, p)) for p in range(4)] for i in range(NT)]
            ycb = [S.buf("yc%d" % i) for i in range(NT)]
            with ExitStack() as les:
                h = self.sb("h", [128, NCH, TT], BF16, les)
                hb = S.buf("h")
                sq = self.sb("sq", [128, NCH, TT], BF16, les)
                sqb = S.buf("sq")
                rstd = self.sb("rstd", [128, TT], F32, les)
                rstdb = S.buf("rstd")
                sig = self.sb("sig", [128, TT], F32, les)
                sigb = S.buf("sig")
                ybuf = self.sb("ybuf", [128, 2, 32 + T], BF16, les)
                ybb = [S.buf("yb%d" % i) for i in range(NT)]
                yhb = S.buf("yhalo")
                acc = self.sb("acc", [128, 2, TT], F32, les)
                accb = S.buf("acc")
                accq = self.sb("accq", [128, 2, 2, TT], BF16, les)
                accqb = S.buf("accq")
                mean = self.sb("mean", [128, TT], F32, les)
                meanb = S.buf("mean")
                var = self.sb("var", [128, TT], F32, les)
                varb = S.buf("var")
                u = self.sb("u", [128, 2, TT], BF16, les)
                ub = S.buf("u")
                vg = self.sb("vg", [128, 256], F32, les)
                vgb = S.buf("vg")
                vsq = self.sb("vsq", [128, 256], F32, les)
                vsqb = S.buf("vsq")
                vhat = self.sb("vhat", [128, 256], BF16, les)
                vhatb = S.buf("vhat")
                st = self.sb("st", [128, 8], F32, les)
                stb = S.buf("st")
                mixt = self.sb("mixt", [128, 128], F32, les)
                mixb = S.buf("mixt")
                wsf = self.sb("wsf", [128, 4, 128], F32, les)
                wsb = self.sb("wsb", [128, 4, 128], BF16, les)
                trilT = self.sb("trilT", [128, 128], F32, les)
                bsb = self.sb("bsb", [128, 4, 128], F32, les)
                cbias = self.sb("cbias", [128, 2, 128], F32, les)
                gmb = S.buf("gmlp_consts")
                S.dma("sp", lambda: nc.sync.dma_start(out=wsf[:, :, :], in_=L["wsT_d"]), w=[gmb])
                S.dma("sp", lambda: nc.sync.dma_start(out=trilT[:, :], in_=L["tril_d"]), w=[gmb])
                S.dma("sp", lambda: nc.sync.dma_start(out=bsb[:, :, :], in_=L["bsb_d"]), w=[gmb])
                if not fused:
                    S.dma("sp", lambda: nc.sync.dma_start(out=ybuf[:, :, 0:32], in_=L["yh_d"]), w=[yhb])
                else:
                    kst = self.sb("kst", [128, 4, TT], BF16, les)
                    kstb = S.buf("kst")
                    vst = kst
                    vstb = kstb
                    ksum = self.sb("ksum", [128, 4, 8], F32, les)
                    ksumb = S.buf("ksum")
                    ksb16 = self.sb("ksb16", [128, 4, 8], BF16, les)
                    ksb16b = S.buf("ksb16")
                    xtoks = [[] for _ in range(9)]
                    kstk = [S.buf("kstk%d" % c) for c in range(4)]
                for hh in range(4):
                    S.op("dve", lambda hh=hh: nc.vector.tensor_tensor(out=wsb[:, hh, :], in0=wsf[:, hh, :],
                                                                      in1=trilT[:, :], op=ALU.mult), r=[gmb], w=[gmb])
                for hh in range(4):
                    bk = self.bank()
                    S.op("pe", lambda hh=hh, bk=bk: nc.tensor.matmul(self.ps[:, bk, 0:128], lhsT=self.ones_bf[:, :],
                                                                     rhs=wsb[:, hh, :], start=True, stop=True),
                         r=[gmb, self.cb], w=[self.pb[bk]])
                    c, e = hh // 2, hh % 2
                    rs = slice(64 * e, 64 * e + 64)
                    S.op("dve", lambda hh=hh, bk=bk, c=c, rs=rs: nc.vector.scalar_tensor_tensor(
                        out=cbias[rs, c, :], in0=self.ps[rs, bk, 0:128], scalar=self.gv[rs, GV_LN_B + c:GV_LN_B + c + 1],
                        in1=bsb[rs, hh, :], op0=ALU.mult, op1=ALU.add), r=[self.pb[bk], gmb, self.cb], w=[gmb])

                wq_v, wq_b = self.ring_load(w_in_v[:, :, 512:1024], (8, 512))
                wu_v, wu_b = self.ring_load(w_in_v[:, :, 0:512], (8, 512))
                wc_v, wc_b = self.ring_load(w_in_v[:, :, 2048:2560], (8, 512))
                if fused:
                    wk_v, wk_b = self.ring_load(w_in_v[:, :, 1024:1536], (8, 512))
                    wvv_v, wvv_b = self.ring_load(w_in_v[:, :, 1536:2048], (8, 512))
                hf = lambda lo=0, n=TT: (lambda k: h[:, k, lo:lo + n])
                for tt in range(NT):
                    sl = slice(tt * TT, (tt + 1) * TT)
                    self.rms_rstd(self.x[:, :, sl], [self.xb[tt]], TT, sq, sqb, rstd, rstdb)
                    self.normalize(lambda c: self.x[:, c, sl], [self.xb[tt]], GV_PRE_MIX, lambda c: h[:, c, :], [hb],
                                   rstd, rstdb, TT)
                    for c in range(4):
                        bk = self.proj_fm(wq_v, wq_b, 128 * c, hf(), [hb], TT)
                        self.evac(bk, qT[:, c, sl], [qb_[tt][c]], scale=0.125)
                    if fused:
                        for c in range(4):
                            bk = self.proj_fm(wk_v, wk_b, 128 * c, hf(), [hb], TT)
                            S.op("act", lambda bk=bk, c=c: nc.scalar.copy(out=kst[:, c, :], in_=self.ps[:, bk, :]),
                                 r=[self.pb[bk]], w=[kstb, self.pb[bk]])
                            S.op("dve", lambda bk=bk, c=c, tt=tt: nc.vector.tensor_reduce(
                                out=ksum[:, c, 2 * tt:2 * tt + 2],
                                in_=self.ps[:, bk, :].rearrange("p (a b) -> p a b", a=2), axis=AX.X, op=ALU.add),
                                r=[self.pb[bk]], w=[ksumb])
                        for c in range(4):
                            xtoks[c].append(S.dma("sp", lambda sl=sl, c=c: nc.sync.dma_start(out=xinK(c)[:, sl],
                                                                                             in_=kst[:, c, :]),
                                                  r=[kstb], w=[xin_b[c]], semkey=kstk[c]))
                        for s4 in range(4):
                            bk = self.proj_tm(wvv_v, wvv_b, 0, 512, hf(128 * s4, 128), [hb])
                            self.evac(bk, vst[:, s4, :], [vstb])
                        xtoks[4 + tt].append(S.dma("sp", lambda tt=tt: nc.sync.dma_start(out=xinV(tt), in_=vst[:, :, :]),
                                                   r=[vstb], w=[xin_b[4 + tt]], semkey=vstb))
                    for c in range(2):
                        bk = self.proj_fm(wu_v, wu_b, 128 * c, hf(), [hb], TT)
                        self.evac(bk, u[:, c, :], [ub], func=AF.Gelu)
                    for s4 in range(4):
                        bk = self.proj_tm(wu_v, wu_b, 256, 256, hf(128 * s4, 128), [hb])
                        S.op("act", lambda bk=bk: nc.scalar.activation(out=vg[:, :], in_=self.ps[:, bk, 0:256],
                                                                       func=AF.Gelu), r=[self.pb[bk]], w=[vgb])
                        S.op("dve", lambda: nc.vector.tensor_reduce(out=st[:, 0:1], in_=vg[:, :], axis=AX.X, op=ALU.add),
                             r=[vgb], w=[stb])
                        S.op("dve", lambda: nc.vector.tensor_tensor(out=vsq[:, :], in0=vg[:, :], in1=vg[:, :],
                                                                    op=ALU.mult), r=[vgb], w=[vsqb])
                        S.op("dve", lambda: nc.vector.tensor_reduce(out=st[:, 1:2], in_=vsq[:, :], axis=AX.X, op=ALU.add),
                             r=[vsqb, stb], w=[stb])
                        S.op("dve", lambda: nc.vector.tensor_scalar(out=st[:, 2:3], in0=st[:, 0:1], scalar1=1.0 / 256,
                                                                    scalar2=None, op0=ALU.mult), r=[stb], w=[stb])
                        S.op("dve", lambda: nc.vector.tensor_tensor(out=st[:, 3:4], in0=st[:, 2:3], in1=st[:, 2:3],
                                                                    op=ALU.mult), r=[stb], w=[stb])
                        S.op("dve", lambda: nc.vector.scalar_tensor_tensor(out=st[:, 4:5], in0=st[:, 1:2],
                                                                           scalar=1.0 / 256, in1=st[:, 3:4],
                                                                           op0=ALU.mult, op1=ALU.subtract),
                             r=[stb], w=[stb])
                        S.op("act", lambda: nc.scalar.activation(out=st[:, 5:6], in_=st[:, 4:5], func=AF.Sqrt,
                                                                 bias=self.eps_t[:, 0:1], scale=1.0),
                             r=[stb, self.cb], w=[stb])
                        S.op("dve", lambda: nc.vector.reciprocal(out=st[:, 6:7], in_=st[:, 5:6]), r=[stb], w=[stb])
                        S.op("dve", lambda: nc.vector.tensor_scalar(out=vhat[:, :], in0=vg[:, :], scalar1=st[:, 2:3],
                                                                    scalar2=st[:, 6:7], op0=ALU.subtract, op1=ALU.mult),
                             r=[vgb, stb], w=[vhatb])
                        for c in range(2):
                            for e in range(2):
                                hh = 2 * c + e
                                rs = slice(64 * e, 64 * e + 64)
                                bk = self.bank()
                                S.op("pe", lambda bk=bk, c=c, hh=hh: nc.tensor.matmul(
                                    self.ps[:, bk, 0:128], lhsT=vhat[:, 128 * c:128 * c + 128], rhs=wsb[:, hh, :],
                                    start=True, stop=True), r=[vhatb, gmb], w=[self.pb[bk]])
                                S.op("dve", lambda bk=bk, c=c, rs=rs: nc.vector.scalar_tensor_tensor(
                                    out=mixt[rs, :], in0=self.ps[rs, bk, 0:128],
                                    scalar=self.gv[rs, GV_LN_G + c:GV_LN_G + c + 1], in1=cbias[rs, c, :],
                                    op0=ALU.mult, op1=ALU.add), r=[self.pb[bk], gmb, self.cb], w=[mixb])
                            tsl = slice(tt * TT + 128 * s4, tt * TT + 128 * s4 + 128)
                            S.op("dve", lambda c=c, tsl=tsl, s4=s4: nc.vector.tensor_tensor(
                                out=yaT[:, c, tsl], in0=u[:, c, 128 * s4:128 * s4 + 128], in1=mixt[:, :], op=ALU.mult),
                                r=[ub, mixb], w=[yab[tt]])
                    self.hT = h
                    self.hb = {tt: hb}
                    self.h_fn = lambda tt_, lo=0, n=TT: (lambda k: h[:, k, lo:lo + n])
                    self.glu_block(wc_v, wc_b, tt, 0, TT, lambda c: ybuf[:, c, 32 + tt * TT:32 + (tt + 1) * TT],
                                   [ybb[tt]], sig, sigb)
                if fused:
                    S.op("act", lambda: nc.scalar.copy(out=ksb16[:, :, :], in_=ksum[:, :, :]), r=[ksumb], w=[ksb16b])
                    xtoks[8].append(S.dma("sp", lambda: nc.sync.dma_start(out=xinS, in_=ksb16[:, :, :]),
                                          r=[ksb16b], w=[xin_b[8]], semkey=ksb16b))
                    xtoks[8].append(S.dma("sp", lambda: nc.sync.dma_start(out=xinH, in_=ybuf[:, :, T:T + 32]),
                                          r=[ybb[3]], w=[xin_b[8]], semkey=yhb))
                    for j9 in (8, 0, 1, 2, 3, 4, 5, 6, 7):
                        S.dma("pool", lambda j9=j9: nc.gpsimd.collective_compute(
                            "AllGather", ALU.bypass, replica_groups=L["groups"], ins=[xin[j9]], outs=[gath[j9]]),
                            r=[xin_b[j9]], w=[gath_b[j9]], inc=1, extra=xtoks[j9])
                    S.dma("sp", lambda: nc.sync.dma_start(out=ybuf[:, :, 0:32], in_=g0H), r=[gath_b[8]], w=[yhb])
                    S.op("dve", lambda: nc.vector.tensor_scalar(out=ybuf[:, :, 0:32], in0=ybuf[:, :, 0:32],
                                                                scalar1=self.hsc[:, 0:1], scalar2=None, op0=ALU.mult),
                         r=[yhb, self.cb], w=[yhb])
                for tt in range(NT):
                    sl = slice(tt * TT, (tt + 1) * TT)
                    rbufs = [ybb[tt]] + ([ybb[tt - 1]] if tt > 0 else [yhb])
                    for c in range(2):
                        for k in range(31):
                            src = ybuf[:, c, 32 + tt * TT - 30 + k:32 + tt * TT - 30 + k + TT]
                            wk = self.gv[:, GV_WDW + 2 * k + c:GV_WDW + 2 * k + c + 1]
                            if k == 0:
                                S.op("dve", lambda c=c, src=src, wk=wk: nc.vector.tensor_scalar(
                                    out=acc[:, c, :], in0=src, scalar1=wk, scalar2=self.gv[:, GV_BDW + c:GV_BDW + c + 1],
                                    op0=ALU.mult, op1=ALU.add), r=rbufs + [self.cb], w=[accb])
                            else:
                                S.op("dve", lambda c=c, src=src, wk=wk: nc.vector.scalar_tensor_tensor(
                                    out=acc[:, c, :], in0=src, scalar=wk, in1=acc[:, c, :], op0=ALU.mult, op1=ALU.add),
                                    r=rbufs + [self.cb, accb], w=[accb])
                    S.op("act", lambda: nc.scalar.copy(out=accq[:, 0, :, :], in_=acc[:, :, :]), r=[accb], w=[accqb])
                    S.op("act", lambda: nc.scalar.activation(out=accq[:, 1, :, :], in_=acc[:, :, :], func=AF.Square),
                         r=[accb], w=[accqb])
                    for c in range(2):
                        b1 = self.bank()
                        S.op("pe", lambda b1=b1, c=c: nc.tensor.matmul(self.ps[:, b1, :], lhsT=blk[:, :],
                                                                       rhs=accq[:, 0, c, :], start=True, stop=True),
                             r=[accqb, self.cb], w=[self.pb[b1]])
                        b2 = self.bank()
                        S.op("pe", lambda b2=b2, c=c: nc.tensor.matmul(self.ps[:, b2, :], lhsT=blk[:, :],
                                                                       rhs=accq[:, 1, c, :], start=True, stop=True),
                             r=[accqb, self.cb], w=[self.pb[b2]])
                        S.op("dve", lambda b1=b1: nc.vector.tensor_scalar(out=mean[:, :], in0=self.ps[:, b1, :],
                                                                          scalar1=1.0 / 64, scalar2=None, op0=ALU.mult),
                             r=[self.pb[b1]], w=[meanb])
                        S.op("dve", lambda: nc.vector.tensor_tensor(out=var[:, :], in0=mean[:, :], in1=mean[:, :],
                                                                    op=ALU.mult), r=[meanb], w=[varb])
                        S.op("dve", lambda b2=b2: nc.vector.scalar_tensor_tensor(
                            out=var[:, :], in0=self.ps[:, b2, :], scalar=1.0 / 64, in1=var[:, :], op0=ALU.mult,
                            op1=ALU.subtract), r=[self.pb[b2], varb], w=[varb])
                        S.op("act", lambda: nc.scalar.activation(out=var[:, :], in_=var[:, :], func=AF.Sqrt,
                                                                 bias=self.eps_t[:, 0:1], scale=1.0),
                             r=[varb, self.cb], w=[varb])
                        S.op("dve", lambda: nc.vector.reciprocal(out=var[:, :], in_=var[:, :]), r=[varb], w=[varb])
                        S.op("dve", lambda c=c: nc.vector.tensor_tensor(out=acc[:, c, :], in0=acc[:, c, :],
                                                                        in1=mean[:, :], op=ALU.subtract),
                             r=[accb, meanb], w=[accb])
                        S.op("dve", lambda c=c: nc.vector.tensor_tensor(out=acc[:, c, :], in0=acc[:, c, :],
                                                                        in1=var[:, :], op=ALU.mult),
                             r=[accb, varb], w=[accb])
                        S.op("act", lambda c=c, sl=sl: nc.scalar.activation(
                            out=ycT[:, c, sl], in_=acc[:, c, :], func=AF.Silu,
                            bias=self.gv[:, GV_GN_B + c:GV_GN_B + c + 1], scale=self.gv[:, GV_GN_G + c:GV_GN_G + c + 1]),
                            r=[accb, self.cb], w=[ycb[tt]])
            S.barrier()
            if self.stop_after == "p1":
                return self.debug_out(L, yaT, qT, ycT)
            with ExitStack() as les:
                ktp = self.sb("ktp", [128, SEQ], BF16, les)
                ktpb = S.buf("ktp")
                vpe = self.sb("vpe", [128, 32, 128], BF16, les)
                vpo = self.sb("vpo", [128, 32, 128], BF16, les)
                vpb = S.buf("vp")
                kbt = self.sb("kbt", [64, SEQ], BF16, les)
                cmask = self.sb("cmask", [128, 4, 512], BF16, les)
                mcb = S.buf("moba_consts")
                qbt = self.sb("qbt", [64, T], BF16, les)
                qbtb = [S.buf("qbt%d" % i) for i in range(NT)]
                ksf = self.sb("ksf", [128, 16], F32, les)
                ksh = self.sb("ksh", [128, 16], BF16, les)
                kmb = self.sb("kmb", [128, 16], BF16, les)
                kmbb = S.buf("kmb")
                gt = self.sb("gt", [128, 16], F32, les)
                gtb = S.buf("gt")
                m8 = self.sb("m8", [128, 8], F32, les)
                m8b = S.buf("m8")
                mbt = self.sb("mbt", [128, 64], F32, les)
                mbtb = S.buf("mbt")
                NPT = 4
                pt = self.sb("pt", [128, NPT, TT], BF16, les)
                ptb = [S.buf("pt%d" % i) for i in range(NPT)]
                rec = self.sb("rec", [128, TT], F32, les)
                recb = S.buf("rec")
                S.dma("sp", lambda: nc.sync.dma_start(out=kbt[:, :], in_=L["kbt_d"]), w=[mcb])
                S.dma("sp", lambda: nc.sync.dma_start(out=cmask[:, :, :], in_=L["cmask_d"]), w=[mcb])
                S.op("dve", lambda: nc.vector.memset(vpe[:, :, :], 0.0), w=[vpb])
                S.op("dve", lambda: nc.vector.memset(vpo[:, :, :], 0.0), w=[vpb])
                S.op("dve", lambda: nc.vector.memset(mbt[:, :], NEG), w=[mbtb])
                pti = 0
                for p in range(4):
                    if not fused:
                        S.dma("sp", lambda p=p: nc.sync.dma_start(out=ktp[:, :], in_=L["kt_d"][:, p, :]), w=[ktpb])
                        for g4 in range(4):
                            S.dma("sp", lambda p=p, g4=g4: nc.sync.dma_start(
                                out=vpe[:, 8 * g4:8 * g4 + 8, 0:64],
                                in_=L["v_d"][:, 8 * g4:8 * g4 + 8, 128 * p:128 * p + 64]), w=[vpb])
                            S.dma("sp", lambda p=p, g4=g4: nc.sync.dma_start(
                                out=vpo[:, 8 * g4:8 * g4 + 8, 64:128],
                                in_=L["v_d"][:, 8 * g4:8 * g4 + 8, 128 * p + 64:128 * p + 128]), w=[vpb])
                        S.dma("sp", lambda p=p: nc.sync.dma_start(out=ksf[:, :], in_=L["ks_d"][:, p, :]), w=[kmbb])
                        S.op("act", lambda: nc.scalar.activation(out=kmb[:, :], in_=ksf[:, :], func=AF.Copy,
                                                                 scale=1.0 / 32), r=[kmbb], w=[kmbb])
                    else:
                        S.dma("sp", lambda p=p: nc.sync.dma_start(out=ktp[:, 0:T], in_=g0K(p)),
                              r=[gath_b[p]], w=[ktpb])
                        S.dma("sp", lambda p=p: nc.sync.dma_start(out=ktp[:, T:SEQ], in_=xinK(p)),
                              r=[xin_b[p]], w=[ktpb])
                        for r4 in range(8):
                            if r4 < 4:
                                src, sb_ = g0V(r4), gath_b[4 + r4]
                            else:
                                src, sb_ = xinV(r4 - 4), xin_b[4 + r4 - 4]
                            S.dma("sp", lambda p=p, r4=r4, src=src: nc.sync.dma_start(
                                out=vpe[:, 4 * r4:4 * r4 + 4, 0:64], in_=src[:, :, 128 * p:128 * p + 64]),
                                r=[sb_], w=[vpb])
                            S.dma("sp", lambda p=p, r4=r4, src=src: nc.sync.dma_start(
                                out=vpo[:, 4 * r4:4 * r4 + 4, 64:128], in_=src[:, :, 128 * p + 64:128 * p + 128]),
                                r=[sb_], w=[vpb])
                        S.dma("sp", lambda p=p: nc.sync.dma_start(out=ksh[:, 0:8], in_=g0S[:, p, :]),
                              r=[gath_b[8]], w=[kmbb])
                        S.dma("sp", lambda p=p: nc.sync.dma_start(out=ksh[:, 8:16], in_=xinS[:, p, :]),
                              r=[xin_b[8]], w=[kmbb])
                        S.op("act", lambda: nc.scalar.activation(out=kmb[:, :], in_=ksh[:, :], func=AF.Copy,
                                                                 scale=1.0 / 32), r=[kmbb], w=[kmbb])
                    S.dma("sp", lambda p=p: nc.sync.dma_start(out=qbt[:, :], in_=L["qbs_d"][:, p, :]), w=qbtb)
                    for tt in range(NT):
                        bT = self.bank()
                        for s4 in range(4):
                            own = 8 + 2 * tt + (1 if s4 >= 2 else 0)
                            tsl = slice(tt * TT + 128 * s4, tt * TT + 128 * s4 + 128)
                            for e in range(2):
                                rs = slice(64 * e, 64 * e + 64)
                                off = 32 * e
                                bk = self.bank()
                                if bk == bT:
                                    bk = self.bank()
                                S.op("pe", lambda bk=bk, rs=rs, tsl=tsl, p=p: nc.tensor.matmul(
                                    self.ps[:, bk, 0:16], lhsT=qT[rs, p, tsl], rhs=kmb[rs, :], start=True, stop=True),
                                    r=[qb_[tt][p], kmbb], w=[self.pb[bk]])
                                S.op("dve", lambda bk=bk, own=own: nc.vector.tensor_tensor(
                                    out=gt[:, 0:own], in0=self.ps[:, bk, 0:own], in1=gb[:, 0:own], op=ALU.add),
                                    r=[self.pb[bk], self.cb], w=[gtb])
                                S.op("dve", lambda own=own: nc.vector.max(out=m8[:, :], in_=gt[:, 0:own]),
                                     r=[gtb], w=[m8b])
                                S.op("dve", lambda own=own, off=off: nc.vector.tensor_scalar(
                                    out=mbt[:, off:off + own], in0=gt[:, 0:own], scalar1=m8[:, 2:3], scalar2=NEG,
                                    op0=ALU.is_lt, op1=ALU.mult), r=[gtb, m8b], w=[mbtb])
                                S.op("dve", lambda own=own, off=off: nc.vector.tensor_tensor(
                                    out=mbt[:, off:off + own], in0=mbt[:, off:off + own], in1=gb[:, 0:own], op=ALU.add),
                                    r=[mbtb, self.cb], w=[mbtb])
                                S.op("dve", lambda own=own, off=off: nc.vector.memset(mbt[:, off + own:off + own + 1], 0.0),
                                     r=[], w=[mbtb])
                                if own + 1 < 16:
                                    S.op("dve", lambda own=own, off=off: nc.vector.memset(
                                        mbt[:, off + own + 1:off + 16], NEG), r=[], w=[mbtb])
                            S.op("pe", lambda bT=bT, s4=s4: nc.tensor.transpose(
                                self.ps[0:64, bT, 128 * s4:128 * s4 + 128], mbt[:, :], ident[:, :]),
                                r=[mbtb, self.cb], w=[self.pb[bT]])
                        sl = slice(tt * TT, (tt + 1) * TT)
                        S.op("act", lambda bT=bT, sl=sl: nc.scalar.copy(out=qbt[0:16, sl], in_=self.ps[0:16, bT, :]),
                             r=[self.pb[bT]], w=[qbtb[tt]])
                        S.op("act", lambda bT=bT, sl=sl: nc.scalar.copy(out=qbt[32:48, sl], in_=self.ps[32:48, bT, :]),
                             r=[self.pb[bT]], w=[qbtb[tt]])
                    self.nrot = 6
                    if self.bank_i >= 6:
                        self.bank_i = 0
                    for tt in range(NT):
                        sl = slice(tt * TT, (tt + 1) * TT)
                        nkt = 20 + 4 * tt
                        OB, DB = 6, 7
                        first = True
                        for kt in range(nkt):
                            ksl = slice(128 * kt, 128 * kt + 128)
                            d = kt - (16 + 4 * tt)
                            for e in range(2):
                                rs = slice(64 * e, 64 * e + 64)
                                bs = slice(32 * e, 32 * e + 20)
                                bk = self.bank()

                                def mm(bk=bk, rs=rs, bs=bs, ksl=ksl, sl=sl, p=p):
                                    nc.tensor.matmul(self.ps[:, bk, :], lhsT=ktp[rs, ksl], rhs=qT[rs, p, sl],
                                                     start=True, stop=False)
                                    return nc.tensor.matmul(self.ps[:, bk, :], lhsT=kbt[bs, ksl], rhs=qbt[bs, sl],
                                                            start=False, stop=True)
                                S.op("pe", mm, r=[ktpb, qb_[tt][p], mcb, qbtb[tt]], w=[self.pb[bk]])
                                pi = pti
                                pti = (pti + 1) % NPT
                                if d >= 0:
                                    S.op("dve", lambda bk=bk, d=d: nc.vector.tensor_tensor(
                                        out=self.ps[:, bk, :], in0=self.ps[:, bk, :], in1=cmask[:, d, :], op=ALU.add),
                                        r=[self.pb[bk], mcb], w=[self.pb[bk]])
                                S.op("act", lambda bk=bk, pi=pi: nc.scalar.activation(
                                    out=pt[:, pi, :], in_=self.ps[:, bk, :], func=AF.Exp), r=[self.pb[bk]], w=[ptb[pi]])
                                vv = vpe if e == 0 else vpo
                                oo = onesE if e == 0 else onesO
                                last = (kt == nkt - 1 and e == 1)

                                def mm2(vv=vv, oo=oo, kt=kt, pi=pi, first=first, last=last):
                                    nc.tensor.matmul(self.ps[:, OB, :], lhsT=vv[:, kt, :], rhs=pt[:, pi, :],
                                                     start=first, stop=last)
                                    return nc.tensor.matmul(self.ps[:, DB, :], lhsT=oo[:, :], rhs=pt[:, pi, :],
                                                            start=first, stop=last)
                                S.op("pe", mm2, r=[vpb, ptb[pi], self.cb], w=[self.pb[OB], self.pb[DB]])
                                first = False
                        S.op("dve", lambda: nc.vector.reciprocal(out=rec[:, :], in_=self.ps[:, DB, :]),
                             r=[self.pb[DB]], w=[recb])
                        S.op("dve", lambda p=p, sl=sl: nc.vector.tensor_tensor(
                            out=qT[:, p, sl], in0=self.ps[:, OB, :], in1=rec[:, :], op=ALU.mult),
                            r=[self.pb[OB], recb], w=[qb_[tt][p]])
                    self.nrot = 8
            S.barrier()
            if self.stop_after == "p2a":
                return self.debug_out(L, yaT, qT, ycT)
            with ExitStack() as les:
                yt = self.sb("yt", [128, NCH, TT], F32, les)
                ytb = S.buf("yt")
                sq = self.sb("sq2", [128, NCH, TT], BF16, les)
                sqb = S.buf("sq2")
                rstd = self.sb("rstd2", [128, TT], F32, les)
                rstdb = S.buf("rstd2")
                wo = [self.ring_load(w_out_v[:, :, 512 * j:512 * j + 512], (8, 512)) for j in range(2)]
                for tt in range(NT):
                    sl = slice(tt * TT, (tt + 1) * TT)

                    def cat(k, sl=sl):
                        if k < 2:
                            return yaT[:, k, sl]
                        if k < 6:
                            return qT[:, k - 2, sl]
                        return ycT[:, k - 6, sl]
                    cbufs = [yab[tt], ycb[tt]] + qb_[tt]
                    for oc in range(8):
                        wv, wb = wo[oc // 4]
                        bk = self.proj_fm(wv, wb, 128 * (oc % 4), cat, cbufs, TT)
                        self.evac(bk, yt[:, oc, :], [ytb])
                    self.post_norm_res(yt[:, :, :], ytb, tt, GV_POST_MIX, sq, sqb, rstd, rstdb)
            S.barrier()
        if self.stop_after == "p2":
            return self.finish(x_o)
        with ExitStack() as les:
            h = self.sb("h3", [128, NCH, TT], BF16, les)
            hb = S.buf("h3")
            sq = self.sb("sq3", [128, NCH, TT], BF16, les)
            sqb = S.buf("sq3")
            rstd = self.sb("rstd3", [128, TT], F32, les)
            rstdb = S.buf("rstd3")
            mT = self.sb("mT", [128, NCH, 256], F32, les)
            mTb = S.buf("mT")
            mh = self.sb("mh", [128, NCH, 256], BF16, les)
            mhb = S.buf("mh")
            kx = self.sb("kx", [128, NCH, 256], BF16, les)
            kxb = S.buf("kx")
            vx = self.sb("vx", [128, 2, D], BF16, les)
            vxb = S.buf("vx")
            qx = self.sb("qx", [128, NCH, TT], BF16, les)
            qxb = S.buf("qx")
            px = self.sb("px", [128, 2, 2, TT], BF16, les)
            pxb = [S.buf("px0"), S.buf("px1")]
            rec = self.sb("rec3", [128, TT], F32, les)
            recb = S.buf("rec3")
            ox = self.sb("ox", [128, NCH, TT], BF16, les)
            oxb = S.buf("ox")
            yt = self.sb("yt3", [128, NCH, TT], F32, les)
            ytb = S.buf("yt3")
            S.dma("sp", lambda: nc.sync.dma_start(out=mT[:, :, :], in_=L["memT_d"]), w=[mTb])
            self.rms_rstd(mT[:, :, :], [mTb], 256, sq, sqb, rstd, rstdb)
            self.normalize(lambda c: mT[:, c, :], [mTb], GV_MEM, lambda c: mh[:, c, :], [mhb], rstd, rstdb, 256)
            mf = lambda lo=0, n=256: (lambda k: mh[:, k, lo:lo + n])
            wkv = [self.ring_load(w_xkv_v[:, :, 512 * j:512 * j + 512], (8, 512)) for j in range(4)]
            for oc in range(8):
                wv, wb = wkv[oc // 4]
                bk = self.proj_fm(wv, wb, 128 * (oc % 4), mf(), [mhb], 256)
                self.evac(bk, kx[:, oc, :], [kxb], n=256, scale=1.0 / 16)
            for mt in range(2):
                for hf2 in range(2):
                    wv, wb = wkv[2 + hf2]
                    bk = self.proj_tm(wv, wb, 0, 512, mf(128 * mt, 128), [mhb])
                    self.evac(bk, vx[:, mt, 512 * hf2:512 * hf2 + 512], [vxb])
            wq = [self.ring_load(w_xq_v[:, :, 512 * j:512 * j + 512], (8, 512)) for j in range(2)]
            wo = [self.ring_load(w_xo_v[:, :, 512 * j:512 * j + 512], (8, 512)) for j in range(2)]
            pxi = 0
            for tt in range(NT):
                sl = slice(tt * TT, (tt + 1) * TT)
                self.rms_rstd(self.x[:, :, sl], [self.xb[tt]], TT, sq, sqb, rstd, rstdb)
                self.normalize(lambda c: self.x[:, c, sl], [self.xb[tt]], GV_PRE_X, lambda c: h[:, c, :], [hb],
                               rstd, rstdb, TT)
                hf = lambda k: h[:, k, :]
                for oc in range(8):
                    wv, wb = wq[oc // 4]
                    bk = self.proj_fm(wv, wb, 128 * (oc % 4), hf, [hb], TT)
                    self.evac(bk, qx[:, oc, :], [qxb])
                for hd in range(4):
                    pi = pxi
                    pxi = 1 - pxi
                    for mt in range(2):
                        bk = self.bank()

                        def mm(bk=bk, hd=hd, mt=mt):
                            nc.tensor.matmul(self.ps[:, bk, :], lhsT=kx[:, 2 * hd, 128 * mt:128 * mt + 128],
                                             rhs=qx[:, 2 * hd, :], start=True, stop=False)
                            return nc.tensor.matmul(self.ps[:, bk, :], lhsT=kx[:, 2 * hd + 1, 128 * mt:128 * mt + 128],
                                                    rhs=qx[:, 2 * hd + 1, :], start=False, stop=True)
                        S.op("pe", mm, r=[kxb, qxb], w=[self.pb[bk]])
                        S.op("act", lambda bk=bk, pi=pi, mt=mt: nc.scalar.activation(
                            out=px[:, pi, mt, :], in_=self.ps[:, bk, :], func=AF.Exp), r=[self.pb[bk]], w=[pxb[pi]])
                    bd = self.bank()

                    def mmd(bd=bd, pi=pi):
                        nc.tensor.matmul(self.ps[:, bd, :], lhsT=self.ones_bf[:, :], rhs=px[:, pi, 0, :],
                                         start=True, stop=False)
                        return nc.tensor.matmul(self.ps[:, bd, :], lhsT=self.ones_bf[:, :], rhs=px[:, pi, 1, :],
                                                start=False, stop=True)
                    S.op("pe", mmd, r=[pxb[pi], self.cb], w=[self.pb[bd]])
                    S.op("dve", lambda bd=bd: nc.vector.reciprocal(out=rec[:, :], in_=self.ps[:, bd, :]),
                         r=[self.pb[bd]], w=[recb])
                    for c in range(2):
                        bo = self.bank()

                        def mmo(bo=bo, pi=pi, hd=hd, c=c):
                            cc = (2 * hd + c) * 128
                            nc.tensor.matmul(self.ps[:, bo, :], lhsT=vx[:, 0, cc:cc + 128], rhs=px[:, pi, 0, :],
                                             start=True, stop=False)
                            return nc.tensor.matmul(self.ps[:, bo, :], lhsT=vx[:, 1, cc:cc + 128], rhs=px[:, pi, 1, :],
                                                    start=False, stop=True)
                        S.op("pe", mmo, r=[vxb, pxb[pi]], w=[self.pb[bo]])
                        S.op("dve", lambda bo=bo, hd=hd, c=c: nc.vector.tensor_tensor(
                            out=ox[:, 2 * hd + c, :], in0=self.ps[:, bo, :], in1=rec[:, :], op=ALU.mult),
                            r=[self.pb[bo], recb], w=[oxb])
                of = lambda k: ox[:, k, :]
                for oc in range(8):
                    wv, wb = wo[oc // 4]
                    bk = self.proj_fm(wv, wb, 128 * (oc % 4), of, [oxb], TT)
                    self.evac(bk, yt[:, oc, :], [ytb])
                self.post_norm_res(yt[:, :, :], ytb, tt, GV_POST_X, sq, sqb, rstd, rstdb)
        S.barrier()
        if self.stop_after == "p3":
            return self.finish(x_o)
        with ExitStack() as les:
            hh = self.sb("hf", [128, NCH, 2 * TT], BF16, les)
            hhb = [S.buf("hf0"), S.buf("hf1")]
            sq = self.sb("sq4", [128, NCH, TT], BF16, les)
            sqb = S.buf("sq4")
            rstd = self.sb("rstd4", [128, TT], F32, les)
            rstdb = S.buf("rstd4")
            yacc = self.sb("yacc", [128, NCH, 2 * TT], F32, les)
            yaccb = [S.buf("yacc0"), S.buf("yacc1")]
            ft = self.sb("ft", [128, 2, NCH, TT], BF16, les)
            ftb = [S.buf("ft0"), S.buf("ft1")]
            fi = 0
            for half in range(2):
                for j in range(2):
                    tt = 2 * half + j
                    sl = slice(tt * TT, (tt + 1) * TT)
                    jl = slice(j * TT, (j + 1) * TT)
                    self.rms_rstd(self.x[:, :, sl], [self.xb[tt]], TT, sq, sqb, rstd, rstdb)
                    self.normalize(lambda c: self.x[:, c, sl], [self.xb[tt]], GV_PRE_FFN, lambda c: hh[:, c, jl],
                                   [hhb[j]], rstd, rstdb, TT)
                for qtr in range(4):
                    w1 = [self.ring_load(w_ff1_v[:, :, 1024 * qtr + 512 * i:1024 * qtr + 512 * i + 512], (8, 512))
                          for i in range(2)]
                    w2 = [self.ring_load(w_ff2_v[:, 8 * qtr + 4 * i:8 * qtr + 4 * i + 4, :], (4, 1024))
                          for i in range(2)]
                    for j in range(2):
                        jl = slice(j * TT, (j + 1) * TT)
                        hf = lambda k, jl=jl: hh[:, k, jl]
                        f = fi
                        fi = 1 - fi
                        for fc in range(8):
                            wv, wb = w1[fc // 4]
                            bk = self.proj_fm(wv, wb, 128 * (fc % 4), hf, [hhb[j]], TT)
                            self.evac(bk, ft[:, f, fc, :], [ftb[f]], func=AF.Relu)
                        S.op("dve", lambda f=f: nc.vector.tensor_tensor(out=ft[:, f, :, :], in0=ft[:, f, :, :],
                                                                        in1=ft[:, f, :, :], op=ALU.mult),
                             r=[ftb[f]], w=[ftb[f]])
                        for oc in range(8):
                            bk = self.bank()

                            def mm(bk=bk, oc=oc, f=f, w2=w2):
                                for kc in range(8):
                                    wv, _ = w2[kc // 4]
                                    ins = nc.tensor.matmul(self.ps[:, bk, :], lhsT=wv[:, kc % 4, 128 * oc:128 * oc + 128],
                                                           rhs=ft[:, f, kc, :], start=(kc == 0), stop=(kc == 7))
                                return ins
                            S.op("pe", mm, r=[w2[0][1], w2[1][1], ftb[f]], w=[self.pb[bk]])
                            if qtr == 0:
                                self.evac(bk, yacc[:, oc, jl], [yaccb[j]])
                            else:
                                S.op("dve", lambda bk=bk, oc=oc, jl=jl: nc.vector.tensor_tensor(
                                    out=yacc[:, oc, jl], in0=self.ps[:, bk, :], in1=yacc[:, oc, jl], op=ALU.add),
                                    r=[self.pb[bk], yaccb[j]], w=[yaccb[j]])
                for j in range(2):
                    tt = 2 * half + j
                    self.post_norm_res(yacc[:, :, j * TT:(j + 1) * TT], yaccb[j], tt, GV_POST_FFN, sq, sqb, rstd, rstdb)
        S.barrier()
        if fused and lyr < L["nlayers"] - 1:
            return
        return self.finish(x_o)

    def finish(self, x_o):
        nc, S = self.nc, self.S
        outs = []
        for tt in range(NT):
            outs.append(S.dma("sp", lambda tt=tt: nc.sync.dma_start(out=x_o[:, :, tt * TT:(tt + 1) * TT],
                                                                    in_=self.x[:, :, tt * TT:(tt + 1) * TT]),
                              r=[self.xb[tt]], semkey=self.xb[tt]))
        S.final_wait("sp", outs)

    def debug_out(self, L, yaT, qT, ycT):
        nc, S = self.nc, self.S
        dbg = L["dbg_o"]
        kb = Buf("dbg")
        t1 = S.dma("sp", lambda: nc.sync.dma_start(out=dbg[:, 0:2, :], in_=yaT[:, :, :]), semkey=kb)
        t2 = S.dma("sp", lambda: nc.sync.dma_start(out=dbg[:, 2:6, :], in_=qT[:, :, :]), semkey=kb)
        t3 = S.dma("sp", lambda: nc.sync.dma_start(out=dbg[:, 6:8, :], in_=ycT[:, :, :]), semkey=kb)
        S.final_wait("sp", [t3])


_PROGS = {}


def get_prog(mode):
    if mode not in _PROGS:
        _PROGS[mode] = Builder(mode).build()
    return _PROGS[mode]


def to_fm(a):
    t, f = a.shape
    return np.ascontiguousarray(a.reshape(t, f // 128, 128).transpose(2, 1, 0))


def from_fm(a):
    p, c, t = a.shape
    return np.ascontiguousarray(a.transpose(2, 1, 0).reshape(t, c * p))


def col(v):
    return np.ascontiguousarray(v.reshape(-1, 128).T)


def make_gv(inp, l):
    gv = np.zeros((128, NGV), np.float32)
    for off, key in ((GV_PRE_MIX, "pre_mix_g"), (GV_POST_MIX, "post_mix_g"), (GV_PRE_X, "pre_x_g"),
                     (GV_MEM, "mem_g"), (GV_POST_X, "post_x_g"), (GV_PRE_FFN, "pre_ffn_g"),
                     (GV_POST_FFN, "post_ffn_g")):
        gv[:, off:off + 8] = col(inp[key][l])
    for off, key in ((GV_LN_G, "gate_ln_g"), (GV_LN_B, "gate_ln_b"), (GV_BDW, "b_dw"), (GV_GN_G, "conv_gn_g"),
                     (GV_GN_B, "conv_gn_b")):
        gv[:, off:off + 2] = col(inp[key][l])
    wdw = inp["w_dw"][l]
    for k in range(31):
        gv[:, GV_WDW + 2 * k:GV_WDW + 2 * k + 2] = col(wdw[k])
    return gv


def run_kv(inp, l, xs):
    nc = get_prog("kv")
    ones = np.ones((128, 128), ml_dtypes.bfloat16)
    gv = make_gv(inp, l)
    w_in = np.ascontiguousarray(inp["w_in"][l])
    in_maps = [{"xT": xs[c], "gv": gv, "w_in": w_in, "ones_bf": ones} for c in range(8)]
    res = run_bass_kernel_spmd(nc, in_maps, core_ids=list(range(8)))
    return res.results


BF = ml_dtypes.bfloat16


def static_tables():
    t = {}
    t["ones_bf"] = np.ones((128, 128), BF)
    t["ident"] = np.eye(128, dtype=np.float32)
    blk = np.zeros((128, 128), np.float32)
    blk[:64, :64] = 1
    blk[64:, 64:] = 1
    t["blkones"] = blk.astype(BF)
    s_ = np.arange(128)[:, None]
    t_ = np.arange(128)[None, :]
    t["trilT"] = (s_ <= t_).astype(np.float32)
    k_ = np.arange(128)[:, None, None]
    d_ = np.arange(4)[None, :, None]
    q_ = np.arange(512)[None, None, :]
    t["cmask"] = np.where((128 * d_ + k_) <= q_, 0.0, NEG).astype(np.float32).astype(BF)
    s = np.arange(SEQ)
    kb = np.zeros((64, SEQ), np.float32)
    for e in range(2):
        o = 32 * e
        kb[o + (s // 256), s] = 1.0
        kb[o + 16] = (s // 64) * 64
        kb[o + 17] = s % 64
        kb[o + 18] = 1.0
        kb[o + 19] = 1.0
    t["kbt"] = kb.astype(BF)
    tq = 2048 + np.arange(T)
    qb = np.zeros((64, 4, T), np.float32)
    for p in range(4):
        for e in range(2):
            hh = 2 * p + e
            slope = 2.0 ** (-(hh + 1))
            o = 32 * e
            qb[o + 16, p] = slope
            qb[o + 17, p] = slope
            qb[o + 18, p] = -slope * ((tq // 64) * 64)
            qb[o + 19, p] = -slope * (tq % 64)
    t["qbs"] = qb.astype(BF)
    return t


def run_main(inp, l, xs, kvres, stop_after=None):
    key = "main" if stop_after is None else "main_" + stop_after
    if key not in _PROGS:
        _PROGS[key] = Builder("main", stop_after).build()
    nc = _PROGS[key]
    st = static_tables()
    gv = make_gv(inp, l)
    wsT = np.ascontiguousarray(inp["w_s"][l].transpose(2, 0, 1))
    bsb = np.ascontiguousarray(np.broadcast_to(inp["b_s"][l][None], (128, 4, 128))).astype(np.float32)
    common = {"gv": gv, "ones_bf": st["ones_bf"], "ident": st["ident"], "blkones": st["blkones"],
              "trilT": st["trilT"], "cmask": st["cmask"], "kbt": st["kbt"], "qbs": st["qbs"],
              "wsT": wsT, "bsb": bsb}
    for k_ in ("w_in", "w_out", "w_xq", "w_xkv", "w_xo", "w_ff1", "w_ff2"):
        common[k_] = np.ascontiguousarray(inp[k_][l])
    in_maps = []
    for c in range(8):
        b, half = c // 2, c % 2
        ra, rb = kvres[2 * b], kvres[2 * b + 1]
        kt = np.zeros((128, 4, SEQ), BF)
        vr = np.zeros((128, 32, 512), BF)
        ks = np.zeros((128, 4, 16), np.float32)
        gb = np.zeros((128, 16), np.float32)
        yh = np.zeros((128, 2, 32), BF)
        if half == 1:
            kt[:, :, :T] = ra["kt_o"]
            kt[:, :, T:] = rb["kt_o"]
            vr[:, :16] = ra["v_o"]
            vr[:, 16:] = rb["v_o"]
            ks[:, :, :8] = ra["ks_o"]
            ks[:, :, 8:] = rb["ks_o"]
            yh[:] = ra["yh_o"]
        else:
            kt[:, :, T:] = ra["kt_o"]
            vr[:, 16:] = ra["v_o"]
            ks[:, :, 8:] = ra["ks_o"]
            gb[:, :8] = -1e30
        m = dict(common)
        m.update({"xT": xs[c], "memT": to_fm(inp["mem"][b]), "kt_rel": kt, "v_rel": vr, "ks_rel": ks, "gb": gb,
                  "yh_in": yh})
        in_maps.append(m)
    return nc, in_maps


def kernel_unfused(**inp):
    inp = {k: np.asarray(v) for k, v in inp.items()}
    x = inp["x"]
    xs = [to_fm(x[c // 2, (c % 2) * T:(c % 2 + 1) * T]) for c in range(8)]
    for l in range(DEPTH):
        kvres = run_kv(inp, l, xs)
        nc, in_maps = run_main(inp, l, xs, kvres)
        res = run_bass_kernel_spmd(nc, in_maps, core_ids=list(range(8)))
        xs = [np.asarray(res.results[c]["x_o"]) for c in range(8)]
    out = np.zeros_like(x)
    for c in range(8):
        out[c // 2, (c % 2) * T:(c % 2 + 1) * T] = from_fm(xs[c])
    return out


def fused_inputs(inp, cores=range(8)):
    st = static_tables()
    x = inp["x"]
    gv = np.ascontiguousarray(np.stack([make_gv(inp, l) for l in range(DEPTH)], axis=1))
    wsT = np.ascontiguousarray(inp["w_s"].transpose(0, 3, 1, 2))
    bsb = np.ascontiguousarray(np.broadcast_to(inp["b_s"][:, None], (DEPTH, 128, 4, 128))).astype(np.float32)
    common = {"gv": gv, "ones_bf": st["ones_bf"], "ident": st["ident"], "blkones": st["blkones"],
              "trilT": st["trilT"], "cmask": st["cmask"], "kbt": st["kbt"], "qbs": st["qbs"], "wsT": wsT, "bsb": bsb}
    for k_ in ("w_in", "w_out", "w_xq", "w_xkv", "w_xo", "w_ff1", "w_ff2"):
        common[k_] = np.ascontiguousarray(inp[k_])
    in_maps = []
    for c in cores:
        b, half = c // 2, c % 2
        gb = np.zeros((128, 16), np.float32)
        if half == 0:
            gb[:, :8] = -1e30
        hsc = np.full((128, 1), float(half), np.float32)
        m = dict(common)
        m.update({"xT": to_fm(x[b, half * T:(half + 1) * T]), "memT": to_fm(inp["mem"][b]), "gb": gb, "hsc": hsc})
        in_maps.append(m)
    return in_maps


def kernel_fused(**inp):
    inp = {k: np.asarray(v) for k, v in inp.items()}
    if "fused" not in _PROGS:
        b = Builder("fused")
        _PROGS["fused"] = b.build_fused()
    nc = _PROGS["fused"]
    in_maps = fused_inputs(inp)
    res = run_bass_kernel_spmd(nc, in_maps, core_ids=list(range(8)))
    x = inp["x"]
    out = np.zeros_like(x)
    for c in range(8):
        out[c // 2, (c % 2) * T:(c % 2 + 1) * T] = from_fm(np.asarray(res.results[c]["x_o"]))
    return out


def kernel(**inp):
    return kernel_fused(**inp)
```

```python
import numpy as np
import ml_dtypes
from contextlib import ExitStack
import concourse.bass as bass
import concourse.mybir as mybir
from concourse.bass_utils import run_bass_kernel_spmd

F32 = mybir.dt.float32
BF16 = mybir.dt.bfloat16
AF = mybir.ActivationFunctionType
ALU = mybir.AluOpType
AX = mybir.AxisListType

D = 1024
NCH = 8
T = 2048
NT = 4
TT = 512
SEQ = 4096
DEPTH = 4
EPS = 1e-6
NEG = -30000.0
NSLOT = 6

GV_PRE_MIX, GV_POST_MIX, GV_PRE_X, GV_MEM, GV_POST_X, GV_PRE_FFN, GV_POST_FFN = 0, 8, 16, 24, 32, 40, 48
GV_LN_G, GV_LN_B, GV_BDW, GV_GN_G, GV_GN_B, GV_WDW = 56, 58, 60, 62, 64, 66
NGV = 66 + 62


class Buf:
    __slots__ = ("name", "w", "rd", "sem", "cnt")

    def __init__(self, name):
        self.name = name
        self.w = None
        self.rd = {}
        self.sem = None
        self.cnt = 0


class Sched:
    def __init__(self, nc, es):
        self.nc = nc
        self.es = es
        self.engs = {"pe": nc.tensor, "act": nc.scalar, "dve": nc.vector, "pool": nc.gpsimd, "sp": nc.sync}
        self.esem = {}
        self.ecnt = {}
        for e in ("pe", "act", "dve", "pool"):
            self.esem[e] = es.enter_context(nc.semaphore("sem_" + e))
            self.ecnt[e] = 0
        self.known = {e: {} for e in self.engs}
        self.dma_sems = {}
        self.nsem = 0
        import os
        self.nops = 0
        self.limit = int(os.environ.get('OPCUT', '0'))

    def buf(self, name):
        return Buf(name)

    def _collect(self, eng, r, w):
        need = {}

        def add(tok):
            if tok is None:
                return
            s, v, src = tok
            if src == "pe" and eng == "pe":
                return
            if need.get(s, 0) < v:
                need[s] = v

        for b in r:
            add(b.w)
        for b in w:
            add(b.w)
            for s, (v, src) in b.rd.items():
                add((s, v, src))
        return need

    def _wait(self, eng, need):
        kn = self.known[eng]
        e = self.engs[eng]
        for s, v in need.items():
            if kn.get(s, 0) < v:
                e.wait_ge(s, v)
                kn[s] = v

    def _record(self, tok, r, w):
        s, v, src = tok
        for b in r:
            old = b.rd.get(s)
            if old is None or old[0] < v:
                b.rd[s] = (v, src)
        for b in w:
            b.w = tok
            b.rd = {}

    def op(self, eng, fn, r=(), w=()):
        self.nops += 1
        if self.limit and self.nops > self.limit:
            return None
        need = self._collect(eng, r, w)
        self._wait(eng, need)
        ins = fn()
        self.ecnt[eng] += 1
        s = self.esem[eng]
        ins.then_inc(s, 1)
        tok = (s, self.ecnt[eng], eng)
        self._record(tok, r, w)
        return tok

    def dma(self, queue, fn, r=(), w=(), semkey=None, persistent=False, inc=16, extra=()):
        self.nops += 1
        if self.limit and self.nops > self.limit:
            return None
        need = self._collect("dma_" + queue, r, w)
        for tk in extra:
            if tk is not None and need.get(tk[0], 0) < tk[1]:
                need[tk[0]] = tk[1]
        self._wait(queue, need)
        kb = semkey if semkey is not None else w[0]
        if kb.sem is None:
            kb.sem = self.es.enter_context(self.nc.semaphore("dsem%d" % self.nsem))
            self.nsem += 1
            self.dma_sems[kb.sem] = [0, persistent]
        ins = fn()
        kb.cnt += inc
        ins.then_inc(kb.sem, inc)
        self.dma_sems[kb.sem][0] = kb.cnt
        tok = (kb.sem, kb.cnt, "dma")
        self._record(tok, r, w)
        return tok

    def barrier(self):
        need = {}
        for e in ("pe", "act", "dve", "pool"):
            if self.ecnt[e] > 0:
                need[self.esem[e]] = self.ecnt[e]
        for s, (c, pers) in self.dma_sems.items():
            if c > 0 and not pers:
                need[s] = c
        for e in ("pe", "act", "dve", "sp"):
            self._wait(e, need)

    def final_wait(self, eng, toks):
        need = {}
        toks = [t for t in toks if t is not None]
        for (s, v, _) in toks:
            need[s] = max(need.get(s, 0), v)
        self._wait(eng, need)


class GV:
    def __init__(self, t, l):
        self.t, self.l = t, l

    def __getitem__(self, idx):
        rows, cols = idx
        return self.t[rows, self.l, cols]


class StopBuild(Exception):
    pass


class Builder:
    def __init__(self, mode, stop_after=None):
        self.stop_after = stop_after
        self.mode = mode
        self.nc = bass.Bass("TRN2", target_bir_lowering=False)
        self.es = ExitStack()

    def sb(self, name, shape, dt, es=None):
        self.nsb = getattr(self, "nsb", 0) + 1
        return (es or self.es).enter_context(self.nc.sbuf_tensor("s_%s_%d" % (name, self.nsb), shape, dt))

    def din(self, name, shape, dt=F32):
        return self.nc.dram_tensor(name, shape, dt, kind="ExternalInput").ap()

    def dout(self, name, shape, dt=F32):
        return self.nc.dram_tensor(name, shape, dt, kind="ExternalOutput").ap()

    def bank(self):
        i = self.bank_i
        self.bank_i = (i + 1) % self.nrot
        return i

    def ring_load(self, src_ap, shape3):
        nc, S = self.nc, self.S
        i = self.ring_i
        self.ring_i = (i + 1) % NSLOT
        a, b = shape3
        view = self.ring[:, i, 0:a * b].rearrange("p (a b) -> p a b", a=a)
        buf = self.ring_b[i]
        if self.mode != "fused":
            S.dma("pool", lambda: nc.gpsimd.dma_start(out=view, in_=src_ap), w=[buf], persistent=True)
            return view, buf
        for k8 in range(8):
            j = self.stg_i
            self.stg_i = 1 - j
            row, col = (k8 * 512) // b, (k8 * 512) % b
            S.dma("sp", lambda j=j, row=row, col=col: nc.sync.dma_start(out=self.stg[:, j, :],
                                                                        in_=src_ap[:, row, col:col + 512]),
                  w=[self.stgb[j]], persistent=True)
            S.op("pool", lambda j=j, k8=k8, i=i: nc.gpsimd.tensor_copy(out=self.ring[:, i, k8 * 512:(k8 + 1) * 512],
                                                                       in_=self.stg[:, j, :]),
                 r=[self.stgb[j]], w=[buf])
        return view, buf

    def rms_rstd(self, src_ap, src_bufs, n, sq_t, sq_b, rstd_t, rstd_b, nch=NCH, scale=1.0 / D):
        nc, S = self.nc, self.S
        for c in range(nch):
            S.op("act", lambda c=c: nc.scalar.activation(out=sq_t[:, c, 0:n], in_=src_ap[:, c, :], func=AF.Square),
                 r=src_bufs, w=[sq_b])
        bk = self.bank()
        ps = self.ps[:, bk, 0:n]

        def mm():
            for c in range(nch):
                ins = nc.tensor.matmul(ps, lhsT=self.ones_bf[:, :], rhs=sq_t[:, c, 0:n],
                                       start=(c == 0), stop=(c == nch - 1))
            return ins
        S.op("pe", mm, r=[sq_b, self.cb], w=[self.pb[bk]])
        S.op("dve", lambda: nc.vector.tensor_scalar(out=rstd_t[:, 0:n], in0=ps, scalar1=scale, scalar2=EPS,
                                                    op0=ALU.mult, op1=ALU.add), r=[self.pb[bk]], w=[rstd_b])
        S.op("act", lambda: nc.scalar.activation(out=rstd_t[:, 0:n], in_=rstd_t[:, 0:n], func=AF.Sqrt),
             r=[rstd_b], w=[rstd_b])
        import os
        if os.environ.get('KVCUT') == '3':
            raise StopBuild()
        S.op("dve", lambda: nc.vector.reciprocal(out=rstd_t[:, 0:n], in_=rstd_t[:, 0:n]), r=[rstd_b], w=[rstd_b])
        if os.environ.get('KVCUT') == '4':
            raise StopBuild()

    def normalize(self, src_fn, src_bufs, gcol, out_fn, out_bufs, rstd_t, rstd_b, n, nch=NCH):
        nc, S = self.nc, self.S
        for c in range(nch):
            S.op("dve", lambda c=c: nc.vector.scalar_tensor_tensor(
                out=out_fn(c), in0=src_fn(c), scalar=self.gv[:, gcol + c:gcol + c + 1], in1=rstd_t[:, 0:n],
                op0=ALU.mult, op1=ALU.mult), r=list(src_bufs) + [rstd_b, self.cb], w=out_bufs)

    def post_norm_residual(self, y_t, y_b, tt, gcol, sq_t, sq_b, rstd_t, rstd_b):
        nc, S = self.nc, self.S
        self.rms_rstd(y_t[:, :, :], [y_b], TT, sq_t, sq_b, rstd_t, rstd_b)
        sl = slice(tt * TT, (tt + 1) * TT)
        for c in range(NCH):
            S.op("dve", lambda c=c: nc.vector.tensor_tensor(out=y_t[:, c, :], in0=y_t[:, c, :], in1=rstd_t[:, :],
                                                            op=ALU.mult), r=[y_b, rstd_b], w=[y_b])
            S.op("dve", lambda c=c: nc.vector.scalar_tensor_tensor(
                out=self.x[:, c, sl], in0=y_t[:, c, :], scalar=self.gv[:, gcol + c:gcol + c + 1],
                in1=self.x[:, c, sl], op0=ALU.mult, op1=ALU.add), r=[y_b, self.cb, self.xb[tt]], w=[self.xb[tt]])

    def proj_fm(self, w_view, w_buf, col0, h_fn, h_bufs, n, kch=NCH):
        nc, S = self.nc, self.S
        bk = self.bank()
        ps = self.ps[:, bk, 0:n]

        def mm():
            for k in range(kch):
                ins = nc.tensor.matmul(ps, lhsT=w_view[:, k, col0:col0 + 128], rhs=h_fn(k),
                                       start=(k == 0), stop=(k == kch - 1))
            return ins
        S.op("pe", mm, r=[w_buf] + list(h_bufs), w=[self.pb[bk]])
        return bk

    def proj_tm(self, w_view, w_buf, col0, ncols, h_fn, h_bufs, kch=NCH):
        nc, S = self.nc, self.S
        bk = self.bank()
        ps = self.ps[:, bk, 0:ncols]

        def mm():
            for k in range(kch):
                ins = nc.tensor.matmul(ps, lhsT=h_fn(k), rhs=w_view[:, k, col0:col0 + ncols],
                                       start=(k == 0), stop=(k == kch - 1))
            return ins
        S.op("pe", mm, r=[w_buf] + list(h_bufs), w=[self.pb[bk]])
        return bk

    def build_fused(self, nlayers=DEPTH, ncores=8):
        nc, es = self.nc, self.es
        with es:
            self.S = S = Sched(nc, es)
            self.bank_i = 0
            self.nrot = 8
            self.ring_i = 0
            NL = nlayers
            xT_d = self.din("xT", [128, NCH, T])
            memT_d = self.din("memT", [128, NCH, 256])
            gv_d = self.din("gv", [128, DEPTH, NGV])
            ones_d = self.din("ones_bf", [128, 128], BF16)
            gb_d = self.din("gb", [128, 16])
            hsc_d = self.din("hsc", [128, 1])
            wsT_d = self.din("wsT", [DEPTH, 128, 4, 128])
            bsb_d = self.din("bsb", [DEPTH, 128, 4, 128])
            w_in_d = self.din("w_in", [DEPTH, D, 2560])
            w_out_d = self.din("w_out", [DEPTH, D, D])
            w_xq_d = self.din("w_xq", [DEPTH, D, D])
            w_xkv_d = self.din("w_xkv", [DEPTH, D, 2 * D])
            w_xo_d = self.din("w_xo", [DEPTH, D, D])
            w_ff1_d = self.din("w_ff1", [DEPTH, D, 4 * D])
            w_ff2_d = self.din("w_ff2", [DEPTH, 4 * D, D])
            ident_d = self.din("ident", [128, 128])
            blk_d = self.din("blkones", [128, 128], BF16)
            tril_d = self.din("trilT", [128, 128])
            cmask_d = self.din("cmask", [128, 4, 512], BF16)
            kbt_d = self.din("kbt", [64, SEQ], BF16)
            qbs_d = self.din("qbs", [64, 4, T], BF16)
            x_o = self.dout("x_o", [128, NCH, T])
            CW = [1024] * 8 + [48]
            xin = [[nc.dram_tensor("xin%d_%d" % (i, j), [128, CW[j]], F32).ap() for j in range(9)] for i in range(2)]
            gath = [[nc.dram_tensor("gath%d_%d" % (i, j), [256, CW[j]], F32).ap() for j in range(9)] for i in range(2)]
            xin_b = [[S.buf("xin%d_%d" % (i, j)) for j in range(9)] for i in range(2)]
            gath_b = [[S.buf("gath%d_%d" % (i, j)) for j in range(9)] for i in range(2)]
            groups = [[2 * i, 2 * i + 1] for i in range(ncores // 2)]

            self.x = self.sb("x", [128, NCH, T], F32)
            self.xb = [S.buf("x%d" % i) for i in range(NT)]
            gvall = self.sb("gvall", [128, DEPTH, NGV], F32)
            self.ones_bf = self.sb("ones", [128, 128], BF16)
            self.eps_t = self.sb("eps", [128, 1], F32)
            self.cb = S.buf("consts")
            self.ring = self.sb("ring", [128, NSLOT, 4096], BF16)
            self.ring_b = [S.buf("ring%d" % i) for i in range(NSLOT)]
            self.stg = self.sb("stg", [128, 2, 512], F32)
            self.stgb = [S.buf("stg0"), S.buf("stg1")]
            self.stg_i = 0
            self.ps = es.enter_context(nc.psum_tensor("ps", [128, 8, 512], F32))
            self.pb = [S.buf("bank%d" % i) for i in range(8)]
            for tt in range(NT):
                S.dma("sp", lambda tt=tt: nc.sync.dma_start(out=self.x[:, :, tt * TT:(tt + 1) * TT],
                                                            in_=xT_d[:, :, tt * TT:(tt + 1) * TT]), w=[self.xb[tt]])
            S.dma("sp", lambda: nc.sync.dma_start(out=gvall[:, :, :], in_=gv_d), w=[self.cb])
            S.dma("sp", lambda: nc.sync.dma_start(out=self.ones_bf[:, :], in_=ones_d), w=[self.cb])
            S.op("dve", lambda: nc.vector.memset(self.eps_t[:, :], EPS), w=[self.cb])
            for l in range(NL):
                L = {"layer": l, "nlayers": NL, "x_o": x_o, "gv": GV(gvall, l),
                     "w_in_v": w_in_d[l].rearrange("(k p) n -> p k n", p=128),
                     "w_out_d": w_out_d[l], "w_xq_d": w_xq_d[l], "w_xkv_d": w_xkv_d[l], "w_xo_d": w_xo_d[l],
                     "w_ff1_d": w_ff1_d[l], "w_ff2_d": w_ff2_d[l], "wsT_d": wsT_d[l], "bsb_d": bsb_d[l],
                     "ident_d": ident_d, "blk_d": blk_d, "tril_d": tril_d, "cmask_d": cmask_d, "kbt_d": kbt_d,
                     "qbs_d": qbs_d, "gb_d": gb_d, "hsc_d": hsc_d, "memT_d": memT_d,
                     "xin": xin, "gath": gath, "xin_b": xin_b, "gath_b": gath_b, "groups": groups}
                self.build_main(L)
            print("fused nops", S.nops, "nsem", S.nsem)
        return nc

    def build(self):
        nc, es = self.nc, self.es
        mode = self.mode
        with es:
            self.S = S = Sched(nc, es)
            import os
            self.bank_i = int(os.environ.get('BANKSHIFT', '0'))
            self.nrot = 8
            self.ring_i = 0
            self.okey = None
            xT_d = self.din("xT", [128, NCH, T])
            gv_d = self.din("gv", [128, NGV])
            w_in_d = self.din("w_in", [D, 2560])
            ones_d = self.din("ones_bf", [128, 128], BF16)
            if mode == "kv":
                kt_o = self.dout("kt_o", [128, 4, T], BF16)
                v_o = self.dout("v_o", [128, 16, 512], BF16)
                ks_o = self.dout("ks_o", [128, 4, 8])
                yh_o = self.dout("yh_o", [128, 2, 32], BF16)
            else:
                memT_d = self.din("memT", [128, NCH, 256])
                kt_d = self.din("kt_rel", [128, 4, SEQ], BF16)
                v_d = self.din("v_rel", [128, 32, 512], BF16)
                ks_d = self.din("ks_rel", [128, 4, 16])
                yh_d = self.din("yh_in", [128, 2, 32], BF16)
                gb_d = self.din("gb", [128, 16])
                wsT_d = self.din("wsT", [128, 4, 128])
                bsb_d = self.din("bsb", [128, 4, 128])
                w_out_d = self.din("w_out", [D, D])
                w_xq_d = self.din("w_xq", [D, D])
                w_xkv_d = self.din("w_xkv", [D, 2 * D])
                w_xo_d = self.din("w_xo", [D, D])
                w_ff1_d = self.din("w_ff1", [D, 4 * D])
                w_ff2_d = self.din("w_ff2", [4 * D, D])
                ident_d = self.din("ident", [128, 128])
                blk_d = self.din("blkones", [128, 128], BF16)
                tril_d = self.din("trilT", [128, 128])
                cmask_d = self.din("cmask", [128, 4, 512], BF16)
                kbt_d = self.din("kbt", [64, SEQ], BF16)
                qbs_d = self.din("qbs", [64, 4, T], BF16)
                x_o = self.dout("x_o", [128, NCH, T])
                if self.stop_after in ("p1", "p2a"):
                    dbg_o = self.dout("dbg_o", [128, 8, T], BF16)

            self.x = self.sb("x", [128, NCH, T], F32)
            self.xb = [S.buf("x%d" % i) for i in range(NT)]
            self.gv = self.sb("gv", [128, NGV], F32)
            self.ones_bf = self.sb("ones", [128, 128], BF16)
            self.eps_t = self.sb("eps", [128, 1], F32)
            self.cb = S.buf("consts")
            self.ring = self.sb("ring", [128, NSLOT, 4096], BF16)
            self.ring_b = [S.buf("ring%d" % i) for i in range(NSLOT)]
            self.ps = es.enter_context(nc.psum_tensor("ps", [128, 8, 512], F32))
            self.pb = [S.buf("bank%d" % i) for i in range(8)]

            for tt in range(NT):
                S.dma("sp", lambda tt=tt: nc.sync.dma_start(out=self.x[:, :, tt * TT:(tt + 1) * TT],
                                                            in_=xT_d[:, :, tt * TT:(tt + 1) * TT]), w=[self.xb[tt]])
            S.dma("sp", lambda: nc.sync.dma_start(out=self.gv[:, :], in_=gv_d), w=[self.cb])
            S.dma("sp", lambda: nc.sync.dma_start(out=self.ones_bf[:, :], in_=ones_d), w=[self.cb])
            S.op("dve", lambda: nc.vector.memset(self.eps_t[:, :], EPS), w=[self.cb])
            w_in_v = w_in_d.rearrange("(k p) n -> p k n", p=128)
            import os
            if os.environ.get('KVCUT') == '2':
                S.barrier()
                return nc

            if mode == "kv":
                try:
                    self.build_kv(w_in_v, kt_o, v_o, ks_o, yh_o)
                except StopBuild:
                    S.barrier()
            else:
                self.build_main(locals())
        return nc

    def p1_norm(self, les, gcol):
        nc, S = self.nc, self.S
        self.hT = self.sb("hT", [128, NCH, T], BF16, les)
        self.hb = [S.buf("h%d" % i) for i in range(NT)]
        self.sq = self.sb("sq", [128, NCH, TT], BF16, les)
        self.sqb = S.buf("sq")
        self.rstd = self.sb("rstd", [128, TT], F32, les)
        self.rstdb = S.buf("rstd")
        for tt in range(NT):
            sl = slice(tt * TT, (tt + 1) * TT)
            self.rms_rstd(self.x[:, :, sl], [self.xb[tt]], TT, self.sq, self.sqb, self.rstd, self.rstdb)
            self.normalize(lambda c: self.x[:, c, sl], [self.xb[tt]], gcol, lambda c: self.hT[:, c, sl],
                           [self.hb[tt]], self.rstd, self.rstdb, TT)

    def h_fn(self, tt, lo=0, n=TT):
        return lambda k: self.hT[:, k, tt * TT + lo:tt * TT + lo + n]

    def glu_block(self, wv, wb, tt, lo, n, y_out_ap, y_bufs, sig_t, sig_b):
        nc, S = self.nc, self.S
        for c in range(2):
            bg = self.proj_fm(wv, wb, 256 + 128 * c, self.h_fn(tt, lo, n), [self.hb[tt]], n)
            S.op("act", lambda bg=bg: nc.scalar.activation(out=sig_t[:, 0:n], in_=self.ps[:, bg, 0:n],
                                                           func=AF.Sigmoid), r=[self.pb[bg]], w=[sig_b])
            ba = self.proj_fm(wv, wb, 128 * c, self.h_fn(tt, lo, n), [self.hb[tt]], n)
            S.op("dve", lambda ba=ba, c=c: nc.vector.tensor_tensor(out=y_out_ap(c), in0=self.ps[:, ba, 0:n],
                                                                   in1=sig_t[:, 0:n], op=ALU.mult),
                 r=[self.pb[ba], sig_b], w=y_bufs)

    def build_kv(self, w_in_v, kt_o, v_o, ks_o, yh_o):
        nc, S = self.nc, self.S
        with ExitStack() as les:
            self.p1_norm(les, GV_PRE_MIX)
            import os
            if os.environ.get('KVCUT') == '1':
                S.barrier()
                return
            kst = self.sb("kst", [128, 4, TT], BF16, les)
            kstb = S.buf("kst")
            ksum = self.sb("ksum", [128, 4, 8], F32, les)
            ksumb = S.buf("ksum")
            vst = self.sb("vst", [128, 4, 512], BF16, les)
            vstb = S.buf("vst")
            sig = self.sb("sig", [128, TT], F32, les)
            sigb = S.buf("sig")
            yh = self.sb("yh", [128, 2, 32], BF16, les)
            yhb = S.buf("yh")
            outs = []
            wv, wb = self.ring_load(w_in_v[:, :, 1024:1536], (8, 512))
            for tt in range(NT):
                for c in range(4):
                    bk = self.proj_fm(wv, wb, 128 * c, self.h_fn(tt), [self.hb[tt]], TT)
                    S.op("act", lambda bk=bk, c=c: nc.scalar.copy(out=kst[:, c, :], in_=self.ps[:, bk, :]),
                         r=[self.pb[bk]], w=[kstb, self.pb[bk]])
                    if os.environ.get('KVCUT') == '5':
                        S.barrier()
                        return
                    S.op("dve", lambda bk=bk, c=c, tt=tt: nc.vector.tensor_reduce(
                        out=ksum[:, c, 2 * tt:2 * tt + 2], in_=self.ps[:, bk, :].rearrange("p (a b) -> p a b", a=2),
                        axis=AX.X, op=ALU.add), r=[self.pb[bk]], w=[ksumb])
                outs.append(S.dma("sp", lambda tt=tt: nc.sync.dma_start(out=kt_o[:, :, tt * TT:(tt + 1) * TT],
                                                                        in_=kst[:, :, :]), r=[kstb],
                                  semkey=kstb))
            outs.append(S.dma("sp", lambda: nc.sync.dma_start(out=ks_o, in_=ksum[:, :, :]), r=[ksumb],
                              semkey=ksumb))
            wv, wb = self.ring_load(w_in_v[:, :, 1536:2048], (8, 512))
            for tt in range(NT):
                for s in range(4):
                    bk = self.proj_tm(wv, wb, 0, 512, self.h_fn(tt, 128 * s, 128), [self.hb[tt]])
                    S.op("act", lambda bk=bk, s=s: nc.scalar.copy(out=vst[:, s, :], in_=self.ps[:, bk, :]),
                         r=[self.pb[bk]], w=[vstb])
                outs.append(S.dma("sp", lambda tt=tt: nc.sync.dma_start(out=v_o[:, 4 * tt:4 * tt + 4, :],
                                                                        in_=vst[:, :, :]), r=[vstb],
                                  semkey=vstb))
            wv, wb = self.ring_load(w_in_v[:, :, 2048:2560], (8, 512))
            self.glu_block(wv, wb, 3, TT - 32, 32, lambda c: yh[:, c, :], [yhb], sig, sigb)
            outs.append(S.dma("sp", lambda: nc.sync.dma_start(out=yh_o, in_=yh[:, :, :]), r=[yhb], semkey=yhb))
            S.final_wait("sp", outs)
            if S.limit:
                S.barrier()
            print('KV nops', S.nops)


    def post_norm_res(self, y3, y_b, tt, gcol, sq_t, sq_b, rstd_t, rstd_b):
        nc, S = self.nc, self.S
        self.rms_rstd(y3, [y_b], TT, sq_t, sq_b, rstd_t, rstd_b)
        sl = slice(tt * TT, (tt + 1) * TT)
        for c in range(NCH):
            S.op("dve", lambda c=c: nc.vector.tensor_tensor(out=y3[:, c, :], in0=y3[:, c, :], in1=rstd_t[:, :],
                                                            op=ALU.mult), r=[y_b, rstd_b], w=[y_b])
            S.op("dve", lambda c=c: nc.vector.scalar_tensor_tensor(
                out=self.x[:, c, sl], in0=y3[:, c, :], scalar=self.gv[:, gcol + c:gcol + c + 1],
                in1=self.x[:, c, sl], op0=ALU.mult, op1=ALU.add), r=[y_b, self.cb, self.xb[tt]], w=[self.xb[tt]])

    def evac(self, bk, out_ap, out_bufs, n=TT, func=None, scale=1.0, eng="act"):
        nc, S = self.nc, self.S
        if eng == "act":
            S.op("act", lambda: nc.scalar.activation(out=out_ap, in_=self.ps[:, bk, 0:n],
                                                     func=(func or AF.Copy), scale=scale),
                 r=[self.pb[bk]], w=out_bufs)
        else:
            S.op("dve", lambda: nc.vector.tensor_copy(out=out_ap, in_=self.ps[:, bk, 0:n]),
                 r=[self.pb[bk]], w=out_bufs)

    def build_main(self, L):
        nc, S = self.nc, self.S
        x_o = L["x_o"]
        w_in_v = L["w_in_v"]
        if "gv" in L:
            self.gv = L["gv"]
        vw = lambda d: d.rearrange("(k p) n -> p k n", p=128)
        w_out_v, w_xq_v, w_xkv_v, w_xo_v = vw(L["w_out_d"]), vw(L["w_xq_d"]), vw(L["w_xkv_d"]), vw(L["w_xo_d"])
        w_ff1_v, w_ff2_v = vw(L["w_ff1_d"]), vw(L["w_ff2_d"])
        fused = self.mode == "fused"
        lyr = L.get("layer", 0)
        if not getattr(self, "consts_done", False):
            self.consts_done = True
            self.ident = self.sb("ident", [128, 128], F32)
            self.blk = self.sb("blk", [128, 128], BF16)
            self.onesE = self.sb("onesE", [128, 128], BF16)
            self.onesO = self.sb("onesO", [128, 128], BF16)
            self.gbt = self.sb("gb", [128, 16], F32)
            self.hsc = self.sb("hsc", [128, 1], F32)
            S.dma("sp", lambda: nc.sync.dma_start(out=self.ident[:, :], in_=L["ident_d"]), w=[self.cb])
            S.dma("sp", lambda: nc.sync.dma_start(out=self.blk[:, :], in_=L["blk_d"]), w=[self.cb])
            S.dma("sp", lambda: nc.sync.dma_start(out=self.gbt[:, :], in_=L["gb_d"]), w=[self.cb])
            if fused:
                S.dma("sp", lambda: nc.sync.dma_start(out=self.hsc[:, :], in_=L["hsc_d"]), w=[self.cb])
            S.op("dve", lambda: nc.vector.memset(self.onesE[:, :], 0.0), w=[self.cb])
            S.op("dve", lambda: nc.vector.memset(self.onesO[:, :], 0.0), w=[self.cb])
            S.op("dve", lambda: nc.vector.memset(self.onesE[:, 0:64], 1.0), w=[self.cb])
            S.op("dve", lambda: nc.vector.memset(self.onesO[:, 64:128], 1.0), w=[self.cb])
        ident, blk, onesE, onesO, gb = self.ident, self.blk, self.onesE, self.onesO, self.gbt
        if fused:
            par = lyr % 2
            xin, gath = L["xin"][par], L["gath"][par]
            xin_b, gath_b = L["xin_b"][par], L["gath_b"][par]
            xb16 = [a.bitcast(BF16) for a in xin]
            gb16 = [a.bitcast(BF16)[0:128, :] for a in gath]
            xinK = lambda c: xb16[c]
            xinV = lambda t_: xb16[4 + t_].rearrange("p (s f) -> p s f", s=4)
            xinS = xb16[8][:, 0:32].rearrange("p (c b) -> p c b", c=4)
            xinH = xb16[8][:, 32:96].rearrange("p (c j) -> p c j", c=2)
            g0K = lambda c: gb16[c]
            g0V = lambda t_: gb16[4 + t_].rearrange("p (s f) -> p s f", s=4)
            g0S = gb16[8][:, 0:32].rearrange("p (c b) -> p c b", c=4)
            g0H = gb16[8][:, 32:96].rearrange("p (c j) -> p c j", c=2)

        with ExitStack() as lay:
            yaT = self.sb("yaT", [128, 2, T], BF16, lay)
            qT = self.sb("qT", [128, 4, T], BF16, lay)
            ycT = self.sb("ycT", [128, 2, T], BF16, lay)
            yab = [S.buf("ya%d" % i) for i in range(NT)]
            qb_ = [[S.buf("q%d_%d" % (i, p)) for p in range(4)] for i in range(NT)]
            ycb = [S.buf("yc%d" % i) for i in range(NT)]
            with ExitStack() as les:
                h = self.sb("h", [128, NCH, TT], BF16, les)
                hb = S.buf("h")
                sq = self.sb("sq", [128, NCH, TT], BF16, les)
                sqb = S.buf("sq")
                rstd = self.sb("rstd", [128, TT], F32, les)
                rstdb = S.buf("rstd")
                sig = self.sb("sig", [128, TT], F32, les)
                sigb = S.buf("sig")
                ybuf = self.sb("ybuf", [128, 2, 32 + T], BF16, les)
                ybb = [S.buf("yb%d" % i) for i in range(NT)]
                yhb = S.buf("yhalo")
                acc = self.sb("acc", [128, 2, TT], F32, les)
                accb = S.buf("acc")
                accq = self.sb("accq", [128, 2, 2, TT], BF16, les)
                accqb = S.buf("accq")
                mean = self.sb("mean", [128, TT], F32, les)
                meanb = S.buf("mean")
                var = self.sb("var", [128, TT], F32, les)
                varb = S.buf("var")
                u = self.sb("u", [128, 2, TT], BF16, les)
                ub = S.buf("u")
                vg = self.sb("vg", [128, 256], F32, les)
                vgb = S.buf("vg")
                vsq = self.sb("vsq", [128, 256], F32, les)
                vsqb = S.buf("vsq")
                vhat = self.sb("vhat", [128, 256], BF16, les)
                vhatb = S.buf("vhat")
                st = self.sb("st", [128, 8], F32, les)
                stb = S.buf("st")
                mixt = self.sb("mixt", [128, 128], F32, les)
                mixb = S.buf("mixt")
                wsf = self.sb("wsf", [128, 4, 128], F32, les)
                wsb = self.sb("wsb", [128, 4, 128], BF16, les)
                trilT = self.sb("trilT", [128, 128], F32, les)
                bsb = self.sb("bsb", [128, 4, 128], F32, les)
                cbias = self.sb("cbias", [128, 2, 128], F32, les)
                gmb = S.buf("gmlp_consts")
                S.dma("sp", lambda: nc.sync.dma_start(out=wsf[:, :, :], in_=L["wsT_d"]), w=[gmb])
                S.dma("sp", lambda: nc.sync.dma_start(out=trilT[:, :], in_=L["tril_d"]), w=[gmb])
                S.dma("sp", lambda: nc.sync.dma_start(out=bsb[:, :, :], in_=L["bsb_d"]), w=[gmb])
                if not fused:
                    S.dma("sp", lambda: nc.sync.dma_start(out=ybuf[:, :, 0:32], in_=L["yh_d"]), w=[yhb])
                else:
                    kst = self.sb("kst", [128, 4, TT], BF16, les)
                    kstb = S.buf("kst")
                    vst = kst
                    vstb = kstb
                    ksum = self.sb("ksum", [128, 4, 8], F32, les)
                    ksumb = S.buf("ksum")
                    ksb16 = self.sb("ksb16", [128, 4, 8], BF16, les)
                    ksb16b = S.buf("ksb16")
                    xtoks = [[] for _ in range(9)]
                    kstk = [S.buf("kstk%d" % c) for c in range(4)]
                for hh in range(4):
                    S.op("dve", lambda hh=hh: nc.vector.tensor_tensor(out=wsb[:, hh, :], in0=wsf[:, hh, :],
                                                                      in1=trilT[:, :], op=ALU.mult), r=[gmb], w=[gmb])
                for hh in range(4):
                    bk = self.bank()
                    S.op("pe", lambda hh=hh, bk=bk: nc.tensor.matmul(self.ps[:, bk, 0:128], lhsT=self.ones_bf[:, :],
                                                                     rhs=wsb[:, hh, :], start=True, stop=True),
                         r=[gmb, self.cb], w=[self.pb[bk]])
                    c, e = hh // 2, hh % 2
                    rs = slice(64 * e, 64 * e + 64)
                    S.op("dve", lambda hh=hh, bk=bk, c=c, rs=rs: nc.vector.scalar_tensor_tensor(
                        out=cbias[rs, c, :], in0=self.ps[rs, bk, 0:128], scalar=self.gv[rs, GV_LN_B + c:GV_LN_B + c + 1],
                        in1=bsb[rs, hh, :], op0=ALU.mult, op1=ALU.add), r=[self.pb[bk], gmb, self.cb], w=[gmb])

                wq_v, wq_b = self.ring_load(w_in_v[:, :, 512:1024], (8, 512))
                wu_v, wu_b = self.ring_load(w_in_v[:, :, 0:512], (8, 512))
                wc_v, wc_b = self.ring_load(w_in_v[:, :, 2048:2560], (8, 512))
                if fused:
                    wk_v, wk_b = self.ring_load(w_in_v[:, :, 1024:1536], (8, 512))
                    wvv_v, wvv_b = self.ring_load(w_in_v[:, :, 1536:2048], (8, 512))
                hf = lambda lo=0, n=TT: (lambda k: h[:, k, lo:lo + n])
                for tt in range(NT):
                    sl = slice(tt * TT, (tt + 1) * TT)
                    self.rms_rstd(self.x[:, :, sl], [self.xb[tt]], TT, sq, sqb, rstd, rstdb)
                    self.normalize(lambda c: self.x[:, c, sl], [self.xb[tt]], GV_PRE_MIX, lambda c: h[:, c, :], [hb],
                                   rstd, rstdb, TT)
                    for c in range(4):
                        bk = self.proj_fm(wq_v, wq_b, 128 * c, hf(), [hb], TT)
                        self.evac(bk, qT[:, c, sl], [qb_[tt][c]], scale=0.125)
                    if fused:
                        for c in range(4):
                            bk = self.proj_fm(wk_v, wk_b, 128 * c, hf(), [hb], TT)
                            S.op("act", lambda bk=bk, c=c: nc.scalar.copy(out=kst[:, c, :], in_=self.ps[:, bk, :]),
                                 r=[self.pb[bk]], w=[kstb, self.pb[bk]])
                            S.op("dve", lambda bk=bk, c=c, tt=tt: nc.vector.tensor_reduce(
                                out=ksum[:, c, 2 * tt:2 * tt + 2],
                                in_=self.ps[:, bk, :].rearrange("p (a b) -> p a b", a=2), axis=AX.X, op=ALU.add),
                                r=[self.pb[bk]], w=[ksumb])
                        for c in range(4):
                            xtoks[c].append(S.dma("sp", lambda sl=sl, c=c: nc.sync.dma_start(out=xinK(c)[:, sl],
                                                                                             in_=kst[:, c, :]),
                                                  r=[kstb], w=[xin_b[c]], semkey=kstk[c]))
                        for s4 in range(4):
                            bk = self.proj_tm(wvv_v, wvv_b, 0, 512, hf(128 * s4, 128), [hb])
                            self.evac(bk, vst[:, s4, :], [vstb])
                        xtoks[4 + tt].append(S.dma("sp", lambda tt=tt: nc.sync.dma_start(out=xinV(tt), in_=vst[:, :, :]),
                                                   r=[vstb], w=[xin_b[4 + tt]], semkey=vstb))
                    for c in range(2):
                        bk = self.proj_fm(wu_v, wu_b, 128 * c, hf(), [hb], TT)
                        self.evac(bk, u[:, c, :], [ub], func=AF.Gelu)
                    for s4 in range(4):
                        bk = self.proj_tm(wu_v, wu_b, 256, 256, hf(128 * s4, 128), [hb])
                        S.op("act", lambda bk=bk: nc.scalar.activation(out=vg[:, :], in_=self.ps[:, bk, 0:256],
                                                                       func=AF.Gelu), r=[self.pb[bk]], w=[vgb])
                        S.op("dve", lambda: nc.vector.tensor_reduce(out=st[:, 0:1], in_=vg[:, :], axis=AX.X, op=ALU.add),
                             r=[vgb], w=[stb])
                        S.op("dve", lambda: nc.vector.tensor_tensor(out=vsq[:, :], in0=vg[:, :], in1=vg[:, :],
                                                                    op=ALU.mult), r=[vgb], w=[vsqb])
                        S.op("dve", lambda: nc.vector.tensor_reduce(out=st[:, 1:2], in_=vsq[:, :], axis=AX.X, op=ALU.add),
                             r=[vsqb, stb], w=[stb])
                        S.op("dve", lambda: nc.vector.tensor_scalar(out=st[:, 2:3], in0=st[:, 0:1], scalar1=1.0 / 256,
                                                                    scalar2=None, op0=ALU.mult), r=[stb], w=[stb])
                        S.op("dve", lambda: nc.vector.tensor_tensor(out=st[:, 3:4], in0=st[:, 2:3], in1=st[:, 2:3],
                                                                    op=ALU.mult), r=[stb], w=[stb])
                        S.op("dve", lambda: nc.vector.scalar_tensor_tensor(out=st[:, 4:5], in0=st[:, 1:2],
                                                                           scalar=1.0 / 256, in1=st[:, 3:4],
                                                                           op0=ALU.mult, op1=ALU.subtract),
                             r=[stb], w=[stb])
                        S.op("act", lambda: nc.scalar.activation(out=st[:, 5:6], in_=st[:, 4:5], func=AF.Sqrt,
                                                                 bias=self.eps_t[:, 0:1], scale=1.0),
                             r=[stb, self.cb], w=[stb])
                        S.op("dve", lambda: nc.vector.reciprocal(out=st[:, 6:7], in_=st[:, 5:6]), r=[stb], w=[stb])
                        S.op("dve", lambda: nc.vector.tensor_scalar(out=vhat[:, :], in0=vg[:, :], scalar1=st[:, 2:3],
                                                                    scalar2=st[:, 6:7], op0=ALU.subtract, op1=ALU.mult),
                             r=[vgb, stb], w=[vhatb])
                        for c in range(2):
                            for e in range(2):
                                hh = 2 * c + e
                                rs = slice(64 * e, 64 * e + 64)
                                bk = self.bank()
                                S.op("pe", lambda bk=bk, c=c, hh=hh: nc.tensor.matmul(
                                    self.ps[:, bk, 0:128], lhsT=vhat[:, 128 * c:128 * c + 128], rhs=wsb[:, hh, :],
                                    start=True, stop=True), r=[vhatb, gmb], w=[self.pb[bk]])
                                S.op("dve", lambda bk=bk, c=c, rs=rs: nc.vector.scalar_tensor_tensor(
                                    out=mixt[rs, :], in0=self.ps[rs, bk, 0:128],
                                    scalar=self.gv[rs, GV_LN_G + c:GV_LN_G + c + 1], in1=cbias[rs, c, :],
                                    op0=ALU.mult, op1=ALU.add), r=[self.pb[bk], gmb, self.cb], w=[mixb])
                            tsl = slice(tt * TT + 128 * s4, tt * TT + 128 * s4 + 128)
                            S.op("dve", lambda c=c, tsl=tsl, s4=s4: nc.vector.tensor_tensor(
                                out=yaT[:, c, tsl], in0=u[:, c, 128 * s4:128 * s4 + 128], in1=mixt[:, :], op=ALU.mult),
                                r=[ub, mixb], w=[yab[tt]])
                    self.hT = h
                    self.hb = {tt: hb}
                    self.h_fn = lambda tt_, lo=0, n=TT: (lambda k: h[:, k, lo:lo + n])
                    self.glu_block(wc_v, wc_b, tt, 0, TT, lambda c: ybuf[:, c, 32 + tt * TT:32 + (tt + 1) * TT],
                                   [ybb[tt]], sig, sigb)
                if fused:
                    S.op("act", lambda: nc.scalar.copy(out=ksb16[:, :, :], in_=ksum[:, :, :]), r=[ksumb], w=[ksb16b])
                    xtoks[8].append(S.dma("sp", lambda: nc.sync.dma_start(out=xinS, in_=ksb16[:, :, :]),
                                          r=[ksb16b], w=[xin_b[8]], semkey=ksb16b))
                    xtoks[8].append(S.dma("sp", lambda: nc.sync.dma_start(out=xinH, in_=ybuf[:, :, T:T + 32]),
                                          r=[ybb[3]], w=[xin_b[8]], semkey=yhb))
                    for j9 in (8, 0, 1, 2, 3, 4, 5, 6, 7):
                        S.dma("pool", lambda j9=j9: nc.gpsimd.collective_compute(
                            "AllGather", ALU.bypass, replica_groups=L["groups"], ins=[xin[j9]], outs=[gath[j9]]),
                            r=[xin_b[j9]], w=[gath_b[j9]], inc=1, extra=xtoks[j9])
                    S.dma("sp", lambda: nc.sync.dma_start(out=ybuf[:, :, 0:32], in_=g0H), r=[gath_b[8]], w=[yhb])
                    S.op("dve", lambda: nc.vector.tensor_scalar(out=ybuf[:, :, 0:32], in0=ybuf[:, :, 0:32],
                                                                scalar1=self.hsc[:, 0:1], scalar2=None, op0=ALU.mult),
                         r=[yhb, self.cb], w=[yhb])
                for tt in range(NT):
                    sl = slice(tt * TT, (tt + 1) * TT)
                    rbufs = [ybb[tt]] + ([ybb[tt - 1]] if tt > 0 else [yhb])
                    for c in range(2):
                        for k in range(31):
                            src = ybuf[:, c, 32 + tt * TT - 30 + k:32 + tt * TT - 30 + k + TT]
                            wk = self.gv[:, GV_WDW + 2 * k + c:GV_WDW + 2 * k + c + 1]
                            if k == 0:
                                S.op("dve", lambda c=c, src=src, wk=wk: nc.vector.tensor_scalar(
                                    out=acc[:, c, :], in0=src, scalar1=wk, scalar2=self.gv[:, GV_BDW + c:GV_BDW + c + 1],
                                    op0=ALU.mult, op1=ALU.add), r=rbufs + [self.cb], w=[accb])
                            else:
                                S.op("dve", lambda c=c, src=src, wk=wk: nc.vector.scalar_tensor_tensor(
                                    out=acc[:, c, :], in0=src, scalar=wk, in1=acc[:, c, :], op0=ALU.mult, op1=ALU.add),
                                    r=rbufs + [self.cb, accb], w=[accb])
                    S.op("act", lambda: nc.scalar.copy(out=accq[:, 0, :, :], in_=acc[:, :, :]), r=[accb], w=[accqb])
                    S.op("act", lambda: nc.scalar.activation(out=accq[:, 1, :, :], in_=acc[:, :, :], func=AF.Square),
                         r=[accb], w=[accqb])
                    for c in range(2):
                        b1 = self.bank()
                        S.op("pe", lambda b1=b1, c=c: nc.tensor.matmul(self.ps[:, b1, :], lhsT=blk[:, :],
                                                                       rhs=accq[:, 0, c, :], start=True, stop=True),
                             r=[accqb, self.cb], w=[self.pb[b1]])
                        b2 = self.bank()
                        S.op("pe", lambda b2=b2, c=c: nc.tensor.matmul(self.ps[:, b2, :], lhsT=blk[:, :],
                                                                       rhs=accq[:, 1, c, :], start=True, stop=True),
                             r=[accqb, self.cb], w=[self.pb[b2]])
                        S.op("dve", lambda b1=b1: nc.vector.tensor_scalar(out=mean[:, :], in0=self.ps[:, b1, :],
                                                                          scalar1=1.0 / 64, scalar2=None, op0=ALU.mult),
                             r=[self.pb[b1]], w=[meanb])
                        S.op("dve", lambda: nc.vector.tensor_tensor(out=var[:, :], in0=mean[:, :], in1=mean[:, :],
                                                                    op=ALU.mult), r=[meanb], w=[varb])
                        S.op("dve", lambda b2=b2: nc.vector.scalar_tensor_tensor(
                            out=var[:, :], in0=self.ps[:, b2, :], scalar=1.0 / 64, in1=var[:, :], op0=ALU.mult,
                            op1=ALU.subtract), r=[self.pb[b2], varb], w=[varb])
                        S.op("act", lambda: nc.scalar.activation(out=var[:, :], in_=var[:, :], func=AF.Sqrt,
                                                                 bias=self.eps_t[:, 0:1], scale=1.0),
                             r=[varb, self.cb], w=[varb])
                        S.op("dve", lambda: nc.vector.reciprocal(out=var[:, :], in_=var[:, :]), r=[varb], w=[varb])
                        S.op("dve", lambda c=c: nc.vector.tensor_tensor(out=acc[:, c, :], in0=acc[:, c, :],
                                                                        in1=mean[:, :], op=ALU.subtract),
                             r=[accb, meanb], w=[accb])
                        S.op("dve", lambda c=c: nc.vector.tensor_tensor(out=acc[:, c, :], in0=acc[:, c, :],
                                                                        in1=var[:, :], op=ALU.mult),
                             r=[accb, varb], w=[accb])
                        S.op("act", lambda c=c, sl=sl: nc.scalar.activation(
                            out=ycT[:, c, sl], in_=acc[:, c, :], func=AF.Silu,
                            bias=self.gv[:, GV_GN_B + c:GV_GN_B + c + 1], scale=self.gv[:, GV_GN_G + c:GV_GN_G + c + 1]),
                            r=[accb, self.cb], w=[ycb[tt]])
            S.barrier()
            if self.stop_after == "p1":
                return self.debug_out(L, yaT, qT, ycT)
            with ExitStack() as les:
                ktp = self.sb("ktp", [128, SEQ], BF16, les)
                ktpb = S.buf("ktp")
                vpe = self.sb("vpe", [128, 32, 128], BF16, les)
                vpo = self.sb("vpo", [128, 32, 128], BF16, les)
                vpb = S.buf("vp")
                kbt = self.sb("kbt", [64, SEQ], BF16, les)
                cmask = self.sb("cmask", [128, 4, 512], BF16, les)
                mcb = S.buf("moba_consts")
                qbt = self.sb("qbt", [64, T], BF16, les)
                qbtb = [S.buf("qbt%d" % i) for i in range(NT)]
                ksf = self.sb("ksf", [128, 16], F32, les)
                ksh = self.sb("ksh", [128, 16], BF16, les)
                kmb = self.sb("kmb", [128, 16], BF16, les)
                kmbb = S.buf("kmb")
                gt = self.sb("gt", [128, 16], F32, les)
                gtb = S.buf("gt")
                m8 = self.sb("m8", [128, 8], F32, les)
                m8b = S.buf("m8")
                mbt = self.sb("mbt", [128, 64], F32, les)
                mbtb = S.buf("mbt")
                NPT = 4
                pt = self.sb("pt", [128, NPT, TT], BF16, les)
                ptb = [S.buf("pt%d" % i) for i in range(NPT)]
                rec = self.sb("rec", [128, TT], F32, les)
                recb = S.buf("rec")
                S.dma("sp", lambda: nc.sync.dma_start(out=kbt[:, :], in_=L["kbt_d"]), w=[mcb])
                S.dma("sp", lambda: nc.sync.dma_start(out=cmask[:, :, :], in_=L["cmask_d"]), w=[mcb])
                S.op("dve", lambda: nc.vector.memset(vpe[:, :, :], 0.0), w=[vpb])
                S.op("dve", lambda: nc.vector.memset(vpo[:, :, :], 0.0), w=[vpb])
                S.op("dve", lambda: nc.vector.memset(mbt[:, :], NEG), w=[mbtb])
                pti = 0
                for p in range(4):
                    if not fused:
                        S.dma("sp", lambda p=p: nc.sync.dma_start(out=ktp[:, :], in_=L["kt_d"][:, p, :]), w=[ktpb])
                        for g4 in range(4):
                            S.dma("sp", lambda p=p, g4=g4: nc.sync.dma_start(
                                out=vpe[:, 8 * g4:8 * g4 + 8, 0:64],
                                in_=L["v_d"][:, 8 * g4:8 * g4 + 8, 128 * p:128 * p + 64]), w=[vpb])
                            S.dma("sp", lambda p=p, g4=g4: nc.sync.dma_start(
                                out=vpo[:, 8 * g4:8 * g4 + 8, 64:128],
                                in_=L["v_d"][:, 8 * g4:8 * g4 + 8, 128 * p + 64:128 * p + 128]), w=[vpb])
                        S.dma("sp", lambda p=p: nc.sync.dma_start(out=ksf[:, :], in_=L["ks_d"][:, p, :]), w=[kmbb])
                        S.op("act", lambda: nc.scalar.activation(out=kmb[:, :], in_=ksf[:, :], func=AF.Copy,
                                                                 scale=1.0 / 32), r=[kmbb], w=[kmbb])
                    else:
                        S.dma("sp", lambda p=p: nc.sync.dma_start(out=ktp[:, 0:T], in_=g0K(p)),
                              r=[gath_b[p]], w=[ktpb])
                        S.dma("sp", lambda p=p: nc.sync.dma_start(out=ktp[:, T:SEQ], in_=xinK(p)),
                              r=[xin_b[p]], w=[ktpb])
                        for r4 in range(8):
                            if r4 < 4:
                                src, sb_ = g0V(r4), gath_b[4 + r4]
                            else:
                                src, sb_ = xinV(r4 - 4), xin_b[4 + r4 - 4]
                            S.dma("sp", lambda p=p, r4=r4, src=src: nc.sync.dma_start(
                                out=vpe[:, 4 * r4:4 * r4 + 4, 0:64], in_=src[:, :, 128 * p:128 * p + 64]),
                                r=[sb_], w=[vpb])
                            S.dma("sp", lambda p=p, r4=r4, src=src: nc.sync.dma_start(
                                out=vpo[:, 4 * r4:4 * r4 + 4, 64:128], in_=src[:, :, 128 * p + 64:128 * p + 128]),
                                r=[sb_], w=[vpb])
                        S.dma("sp", lambda p=p: nc.sync.dma_start(out=ksh[:, 0:8], in_=g0S[:, p, :]),
                              r=[gath_b[8]], w=[kmbb])
                        S.dma("sp", lambda p=p: nc.sync.dma_start(out=ksh[:, 8:16], in_=xinS[:, p, :]),
                              r=[xin_b[8]], w=[kmbb])
                        S.op("act", lambda: nc.scalar.activation(out=kmb[:, :], in_=ksh[:, :], func=AF.Copy,
                                                                 scale=1.0 / 32), r=[kmbb], w=[kmbb])
                    S.dma("sp", lambda p=p: nc.sync.dma_start(out=qbt[:, :], in_=L["qbs_d"][:, p, :]), w=qbtb)
                    for tt in range(NT):
                        bT = self.bank()
                        for s4 in range(4):
                            own = 8 + 2 * tt + (1 if s4 >= 2 else 0)
                            tsl = slice(tt * TT + 128 * s4, tt * TT + 128 * s4 + 128)
                            for e in range(2):
                                rs = slice(64 * e, 64 * e + 64)
                                off = 32 * e
                                bk = self.bank()
                                if bk == bT:
                                    bk = self.bank()
                                S.op("pe", lambda bk=bk, rs=rs, tsl=tsl, p=p: nc.tensor.matmul(
                                    self.ps[:, bk, 0:16], lhsT=qT[rs, p, tsl], rhs=kmb[rs, :], start=True, stop=True),
                                    r=[qb_[tt][p], kmbb], w=[self.pb[bk]])
                                S.op("dve", lambda bk=bk, own=own: nc.vector.tensor_tensor(
                                    out=gt[:, 0:own], in0=self.ps[:, bk, 0:own], in1=gb[:, 0:own], op=ALU.add),
                                    r=[self.pb[bk], self.cb], w=[gtb])
                                S.op("dve", lambda own=own: nc.vector.max(out=m8[:, :], in_=gt[:, 0:own]),
                                     r=[gtb], w=[m8b])
                                S.op("dve", lambda own=own, off=off: nc.vector.tensor_scalar(
                                    out=mbt[:, off:off + own], in0=gt[:, 0:own], scalar1=m8[:, 2:3], scalar2=NEG,
                                    op0=ALU.is_lt, op1=ALU.mult), r=[gtb, m8b], w=[mbtb])
                                S.op("dve", lambda own=own, off=off: nc.vector.tensor_tensor(
                                    out=mbt[:, off:off + own], in0=mbt[:, off:off + own], in1=gb[:, 0:own], op=ALU.add),
                                    r=[mbtb, self.cb], w=[mbtb])
                                S.op("dve", lambda own=own, off=off: nc.vector.memset(mbt[:, off + own:off + own + 1], 0.0),
                                     r=[], w=[mbtb])
                                if own + 1 < 16:
                                    S.op("dve", lambda own=own, off=off: nc.vector.memset(
                                        mbt[:, off + own + 1:off + 16], NEG), r=[], w=[mbtb])
                            S.op("pe", lambda bT=bT, s4=s4: nc.tensor.transpose(
                                self.ps[0:64, bT, 128 * s4:128 * s4 + 128], mbt[:, :], ident[:, :]),
                                r=[mbtb, self.cb], w=[self.pb[bT]])
                        sl = slice(tt * TT, (tt + 1) * TT)
                        S.op("act", lambda bT=bT, sl=sl: nc.scalar.copy(out=qbt[0:16, sl], in_=self.ps[0:16, bT, :]),
                             r=[self.pb[bT]], w=[qbtb[tt]])
                        S.op("act", lambda bT=bT, sl=sl: nc.scalar.copy(out=qbt[32:48, sl], in_=self.ps[32:48, bT, :]),
                             r=[self.pb[bT]], w=[qbtb[tt]])
                    self.nrot = 6
                    if self.bank_i >= 6:
                        self.bank_i = 0
                    for tt in range(NT):
                        sl = slice(tt * TT, (tt + 1) * TT)
                        nkt = 20 + 4 * tt
                        OB, DB = 6, 7
                        units = [(kt, e) for kt in range(nkt) for e in range(2)]
                        pend = None

                        def emit_pv(kt, e, pi, first, last):
                            vv = vpe if e == 0 else vpo
                            oo = onesE if e == 0 else onesO

                            def mm2(vv=vv, oo=oo, kt=kt, pi=pi, first=first, last=last):
                                nc.tensor.matmul(self.ps[:, OB, :], lhsT=vv[:, kt, :], rhs=pt[:, pi, :],
                                                 start=first, stop=last)
                                return nc.tensor.matmul(self.ps[:, DB, :], lhsT=oo[:, :], rhs=pt[:, pi, :],
                                                        start=first, stop=last)
                            S.op("pe", mm2, r=[vpb, ptb[pi], self.cb], w=[self.pb[OB], self.pb[DB]])

                        for ui, (kt, e) in enumerate(units):
                            ksl = slice(128 * kt, 128 * kt + 128)
                            d = kt - (16 + 4 * tt)
                            rs = slice(64 * e, 64 * e + 64)
                            bs = slice(32 * e, 32 * e + 20)
                            bk = self.bank()

                            def mm(bk=bk, rs=rs, bs=bs, ksl=ksl, sl=sl, p=p):
                                nc.tensor.matmul(self.ps[:, bk, :], lhsT=ktp[rs, ksl], rhs=qT[rs, p, sl],
                                                 start=True, stop=False)
                                return nc.tensor.matmul(self.ps[:, bk, :], lhsT=kbt[bs, ksl], rhs=qbt[bs, sl],
                                                        start=False, stop=True)
                            S.op("pe", mm, r=[ktpb, qb_[tt][p], mcb, qbtb[tt]], w=[self.pb[bk]])
                            pi = pti
                            pti = (pti + 1) % NPT
                            if d >= 0:
                                S.op("dve", lambda bk=bk, d=d: nc.vector.tensor_tensor(
                                    out=self.ps[:, bk, :], in0=self.ps[:, bk, :], in1=cmask[:, d, :], op=ALU.add),
                                    r=[self.pb[bk], mcb], w=[self.pb[bk]])
                            S.op("act", lambda bk=bk, pi=pi: nc.scalar.activation(
                                out=pt[:, pi, :], in_=self.ps[:, bk, :], func=AF.Exp), r=[self.pb[bk]], w=[ptb[pi]])
                            if pend is not None:
                                emit_pv(*pend)
                            pend = (kt, e, pi, ui == 0, ui == len(units) - 1)
                        emit_pv(*pend)
                        S.op("dve", lambda: nc.vector.reciprocal(out=rec[:, :], in_=self.ps[:, DB, :]),
                             r=[self.pb[DB]], w=[recb])
                        S.op("dve", lambda p=p, sl=sl: nc.vector.tensor_tensor(
                            out=qT[:, p, sl], in0=self.ps[:, OB, :], in1=rec[:, :], op=ALU.mult),
                            r=[self.pb[OB], recb], w=[qb_[tt][p]])
                    self.nrot = 8
            S.barrier()
            if self.stop_after == "p2a":
                return self.debug_out(L, yaT, qT, ycT)
            with ExitStack() as les:
                yt = self.sb("yt", [128, NCH, TT], F32, les)
                ytb = S.buf("yt")
                sq = self.sb("sq2", [128, NCH, TT], BF16, les)
                sqb = S.buf("sq2")
                rstd = self.sb("rstd2", [128, TT], F32, les)
                rstdb = S.buf("rstd2")
                wo = [self.ring_load(w_out_v[:, :, 512 * j:512 * j + 512], (8, 512)) for j in range(2)]
                for tt in range(NT):
                    sl = slice(tt * TT, (tt + 1) * TT)

                    def cat(k, sl=sl):
                        if k < 2:
                            return yaT[:, k, sl]
                        if k < 6:
                            return qT[:, k - 2, sl]
                        return ycT[:, k - 6, sl]
                    cbufs = [yab[tt], ycb[tt]] + qb_[tt]
                    for oc in range(8):
                        wv, wb = wo[oc // 4]
                        bk = self.proj_fm(wv, wb, 128 * (oc % 4), cat, cbufs, TT)
                        self.evac(bk, yt[:, oc, :], [ytb])
                    self.post_norm_res(yt[:, :, :], ytb, tt, GV_POST_MIX, sq, sqb, rstd, rstdb)
            S.barrier()
        if self.stop_after == "p2":
            return self.finish(x_o)
        with ExitStack() as les:
            h = self.sb("h3", [128, NCH, TT], BF16, les)
            hb = S.buf("h3")
            sq = self.sb("sq3", [128, NCH, TT], BF16, les)
            sqb = S.buf("sq3")
            rstd = self.sb("rstd3", [128, TT], F32, les)
            rstdb = S.buf("rstd3")
            mT = self.sb("mT", [128, NCH, 256], F32, les)
            mTb = S.buf("mT")
            mh = self.sb("mh", [128, NCH, 256], BF16, les)
            mhb = S.buf("mh")
            kx = self.sb("kx", [128, NCH, 256], BF16, les)
            kxb = S.buf("kx")
            vx = self.sb("vx", [128, 2, D], BF16, les)
            vxb = S.buf("vx")
            qx = self.sb("qx", [128, NCH, TT], BF16, les)
            qxb = S.buf("qx")
            px = self.sb("px", [128, 2, 2, TT], BF16, les)
            pxb = [S.buf("px0"), S.buf("px1")]
            rec = self.sb("rec3", [128, TT], F32, les)
            recb = S.buf("rec3")
            ox = self.sb("ox", [128, NCH, TT], BF16, les)
            oxb = S.buf("ox")
            yt = self.sb("yt3", [128, NCH, TT], F32, les)
            ytb = S.buf("yt3")
            S.dma("sp", lambda: nc.sync.dma_start(out=mT[:, :, :], in_=L["memT_d"]), w=[mTb])
            self.rms_rstd(mT[:, :, :], [mTb], 256, sq, sqb, rstd, rstdb)
            self.normalize(lambda c: mT[:, c, :], [mTb], GV_MEM, lambda c: mh[:, c, :], [mhb], rstd, rstdb, 256)
            mf = lambda lo=0, n=256: (lambda k: mh[:, k, lo:lo + n])
            wkv = [self.ring_load(w_xkv_v[:, :, 512 * j:512 * j + 512], (8, 512)) for j in range(4)]
            for oc in range(8):
                wv, wb = wkv[oc // 4]
                bk = self.proj_fm(wv, wb, 128 * (oc % 4), mf(), [mhb], 256)
                self.evac(bk, kx[:, oc, :], [kxb], n=256, scale=1.0 / 16)
            for mt in range(2):
                for hf2 in range(2):
                    wv, wb = wkv[2 + hf2]
                    bk = self.proj_tm(wv, wb, 0, 512, mf(128 * mt, 128), [mhb])
                    self.evac(bk, vx[:, mt, 512 * hf2:512 * hf2 + 512], [vxb])
            wq = [self.ring_load(w_xq_v[:, :, 512 * j:512 * j + 512], (8, 512)) for j in range(2)]
            wo = [self.ring_load(w_xo_v[:, :, 512 * j:512 * j + 512], (8, 512)) for j in range(2)]
            pxi = 0
            for tt in range(NT):
                sl = slice(tt * TT, (tt + 1) * TT)
                self.rms_rstd(self.x[:, :, sl], [self.xb[tt]], TT, sq, sqb, rstd, rstdb)
                self.normalize(lambda c: self.x[:, c, sl], [self.xb[tt]], GV_PRE_X, lambda c: h[:, c, :], [hb],
                               rstd, rstdb, TT)
                hf = lambda k: h[:, k, :]
                for oc in range(8):
                    wv, wb = wq[oc // 4]
                    bk = self.proj_fm(wv, wb, 128 * (oc % 4), hf, [hb], TT)
                    self.evac(bk, qx[:, oc, :], [qxb])
                for hd in range(4):
                    pi = pxi
                    pxi = 1 - pxi
                    for mt in range(2):
                        bk = self.bank()

                        def mm(bk=bk, hd=hd, mt=mt):
                            nc.tensor.matmul(self.ps[:, bk, :], lhsT=kx[:, 2 * hd, 128 * mt:128 * mt + 128],
                                             rhs=qx[:, 2 * hd, :], start=True, stop=False)
                            return nc.tensor.matmul(self.ps[:, bk, :], lhsT=kx[:, 2 * hd + 1, 128 * mt:128 * mt + 128],
                                                    rhs=qx[:, 2 * hd + 1, :], start=False, stop=True)
                        S.op("pe", mm, r=[kxb, qxb], w=[self.pb[bk]])
                        S.op("act", lambda bk=bk, pi=pi, mt=mt: nc.scalar.activation(
                            out=px[:, pi, mt, :], in_=self.ps[:, bk, :], func=AF.Exp), r=[self.pb[bk]], w=[pxb[pi]])
                    bd = self.bank()

                    def mmd(bd=bd, pi=pi):
                        nc.tensor.matmul(self.ps[:, bd, :], lhsT=self.ones_bf[:, :], rhs=px[:, pi, 0, :],
                                         start=True, stop=False)
                        return nc.tensor.matmul(self.ps[:, bd, :], lhsT=self.ones_bf[:, :], rhs=px[:, pi, 1, :],
                                                start=False, stop=True)
                    S.op("pe", mmd, r=[pxb[pi], self.cb], w=[self.pb[bd]])
                    S.op("dve", lambda bd=bd: nc.vector.reciprocal(out=rec[:, :], in_=self.ps[:, bd, :]),
                         r=[self.pb[bd]], w=[recb])
                    for c in range(2):
                        bo = self.bank()

                        def mmo(bo=bo, pi=pi, hd=hd, c=c):
                            cc = (2 * hd + c) * 128
                            nc.tensor.matmul(self.ps[:, bo, :], lhsT=vx[:, 0, cc:cc + 128], rhs=px[:, pi, 0, :],
                                             start=True, stop=False)
                            return nc.tensor.matmul(self.ps[:, bo, :], lhsT=vx[:, 1, cc:cc + 128], rhs=px[:, pi, 1, :],
                                                    start=False, stop=True)
                        S.op("pe", mmo, r=[vxb, pxb[pi]], w=[self.pb[bo]])
                        S.op("dve", lambda bo=bo, hd=hd, c=c: nc.vector.tensor_tensor(
                            out=ox[:, 2 * hd + c, :], in0=self.ps[:, bo, :], in1=rec[:, :], op=ALU.mult),
                            r=[self.pb[bo], recb], w=[oxb])
                of = lambda k: ox[:, k, :]
                for oc in range(8):
                    wv, wb = wo[oc // 4]
                    bk = self.proj_fm(wv, wb, 128 * (oc % 4), of, [oxb], TT)
                    self.evac(bk, yt[:, oc, :], [ytb])
                self.post_norm_res(yt[:, :, :], ytb, tt, GV_POST_X, sq, sqb, rstd, rstdb)
        S.barrier()
        if self.stop_after == "p3":
            return self.finish(x_o)
        with ExitStack() as les:
            hh = self.sb("hf", [128, NCH, 2 * TT], BF16, les)
            hhb = [S.buf("hf0"), S.buf("hf1")]
            sq = self.sb("sq4", [128, NCH, TT], BF16, les)
            sqb = S.buf("sq4")
            rstd = self.sb("rstd4", [128, TT], F32, les)
            rstdb = S.buf("rstd4")
            yacc = self.sb("yacc", [128, NCH, 2 * TT], F32, les)
            yaccb = [S.buf("yacc0"), S.buf("yacc1")]
            ft = self.sb("ft", [128, 2, NCH, TT], BF16, les)
            ftb = [S.buf("ft0"), S.buf("ft1")]
            fi = 0
            for half in range(2):
                for j in range(2):
                    tt = 2 * half + j
                    sl = slice(tt * TT, (tt + 1) * TT)
                    jl = slice(j * TT, (j + 1) * TT)
                    self.rms_rstd(self.x[:, :, sl], [self.xb[tt]], TT, sq, sqb, rstd, rstdb)
                    self.normalize(lambda c: self.x[:, c, sl], [self.xb[tt]], GV_PRE_FFN, lambda c: hh[:, c, jl],
                                   [hhb[j]], rstd, rstdb, TT)
                for qtr in range(4):
                    w1 = [self.ring_load(w_ff1_v[:, :, 1024 * qtr + 512 * i:1024 * qtr + 512 * i + 512], (8, 512))
                          for i in range(2)]
                    w2 = [self.ring_load(w_ff2_v[:, 8 * qtr + 4 * i:8 * qtr + 4 * i + 4, :], (4, 1024))
                          for i in range(2)]
                    for j in range(2):
                        jl = slice(j * TT, (j + 1) * TT)
                        hf = lambda k, jl=jl: hh[:, k, jl]
                        f = fi
                        fi = 1 - fi
                        for fc in range(8):
                            wv, wb = w1[fc // 4]
                            bk = self.proj_fm(wv, wb, 128 * (fc % 4), hf, [hhb[j]], TT)
                            self.evac(bk, ft[:, f, fc, :], [ftb[f]], func=AF.Relu)
                        S.op("dve", lambda f=f: nc.vector.tensor_tensor(out=ft[:, f, :, :], in0=ft[:, f, :, :],
                                                                        in1=ft[:, f, :, :], op=ALU.mult),
                             r=[ftb[f]], w=[ftb[f]])
                        for oc in range(8):
                            bk = self.bank()

                            def mm(bk=bk, oc=oc, f=f, w2=w2):
                                for kc in range(8):
                                    wv, _ = w2[kc // 4]
                                    ins = nc.tensor.matmul(self.ps[:, bk, :], lhsT=wv[:, kc % 4, 128 * oc:128 * oc + 128],
                                                           rhs=ft[:, f, kc, :], start=(kc == 0), stop=(kc == 7))
                                return ins
                            S.op("pe", mm, r=[w2[0][1], w2[1][1], ftb[f]], w=[self.pb[bk]])
                            if qtr == 0:
                                self.evac(bk, yacc[:, oc, jl], [yaccb[j]])
                            else:
                                S.op("dve", lambda bk=bk, oc=oc, jl=jl: nc.vector.tensor_tensor(
                                    out=yacc[:, oc, jl], in0=self.ps[:, bk, :], in1=yacc[:, oc, jl], op=ALU.add),
                                    r=[self.pb[bk], yaccb[j]], w=[yaccb[j]])
                for j in range(2):
                    tt = 2 * half + j
                    self.post_norm_res(yacc[:, :, j * TT:(j + 1) * TT], yaccb[j], tt, GV_POST_FFN, sq, sqb, rstd, rstdb)
        S.barrier()
        if fused and lyr < L["nlayers"] - 1:
            return
        return self.finish(x_o)

    def finish(self, x_o):
        nc, S = self.nc, self.S
        outs = []
        for tt in range(NT):
            outs.append(S.dma("sp", lambda tt=tt: nc.sync.dma_start(out=x_o[:, :, tt * TT:(tt + 1) * TT],
                                                                    in_=self.x[:, :, tt * TT:(tt + 1) * TT]),
                              r=[self.xb[tt]], semkey=self.xb[tt]))
        S.final_wait("sp", outs)

    def debug_out(self, L, yaT, qT, ycT):
        nc, S = self.nc, self.S
        dbg = L["dbg_o"]
        kb = Buf("dbg")
        t1 = S.dma("sp", lambda: nc.sync.dma_start(out=dbg[:, 0:2, :], in_=yaT[:, :, :]), semkey=kb)
        t2 = S.dma("sp", lambda: nc.sync.dma_start(out=dbg[:, 2:6, :], in_=qT[:, :, :]), semkey=kb)
        t3 = S.dma("sp", lambda: nc.sync.dma_start(out=dbg[:, 6:8, :], in_=ycT[:, :, :]), semkey=kb)
        S.final_wait("sp", [t3])


_PROGS = {}


def get_prog(mode):
    if mode not in _PROGS:
        _PROGS[mode] = Builder(mode).build()
    return _PROGS[mode]


def to_fm(a):
    t, f = a.shape
    return np.ascontiguousarray(a.reshape(t, f // 128, 128).transpose(2, 1, 0))


def from_fm(a):
    p, c, t = a.shape
    return np.ascontiguousarray(a.transpose(2, 1, 0).reshape(t, c * p))


def col(v):
    return np.ascontiguousarray(v.reshape(-1, 128).T)


def make_gv(inp, l):
    gv = np.zeros((128, NGV), np.float32)
    for off, key in ((GV_PRE_MIX, "pre_mix_g"), (GV_POST_MIX, "post_mix_g"), (GV_PRE_X, "pre_x_g"),
                     (GV_MEM, "mem_g"), (GV_POST_X, "post_x_g"), (GV_PRE_FFN, "pre_ffn_g"),
                     (GV_POST_FFN, "post_ffn_g")):
        gv[:, off:off + 8] = col(inp[key][l])
    for off, key in ((GV_LN_G, "gate_ln_g"), (GV_LN_B, "gate_ln_b"), (GV_BDW, "b_dw"), (GV_GN_G, "conv_gn_g"),
                     (GV_GN_B, "conv_gn_b")):
        gv[:, off:off + 2] = col(inp[key][l])
    wdw = inp["w_dw"][l]
    for k in range(31):
        gv[:, GV_WDW + 2 * k:GV_WDW + 2 * k + 2] = col(wdw[k])
    return gv


def run_kv(inp, l, xs):
    nc = get_prog("kv")
    ones = np.ones((128, 128), ml_dtypes.bfloat16)
    gv = make_gv(inp, l)
    w_in = np.ascontiguousarray(inp["w_in"][l])
    in_maps = [{"xT": xs[c], "gv": gv, "w_in": w_in, "ones_bf": ones} for c in range(8)]
    res = run_bass_kernel_spmd(nc, in_maps, core_ids=list(range(8)))
    return res.results


BF = ml_dtypes.bfloat16


def static_tables():
    t = {}
    t["ones_bf"] = np.ones((128, 128), BF)
    t["ident"] = np.eye(128, dtype=np.float32)
    blk = np.zeros((128, 128), np.float32)
    blk[:64, :64] = 1
    blk[64:, 64:] = 1
    t["blkones"] = blk.astype(BF)
    s_ = np.arange(128)[:, None]
    t_ = np.arange(128)[None, :]
    t["trilT"] = (s_ <= t_).astype(np.float32)
    k_ = np.arange(128)[:, None, None]
    d_ = np.arange(4)[None, :, None]
    q_ = np.arange(512)[None, None, :]
    t["cmask"] = np.where((128 * d_ + k_) <= q_, 0.0, NEG).astype(np.float32).astype(BF)
    s = np.arange(SEQ)
    kb = np.zeros((64, SEQ), np.float32)
    for e in range(2):
        o = 32 * e
        kb[o + (s // 256), s] = 1.0
        kb[o + 16] = (s // 64) * 64
        kb[o + 17] = s % 64
        kb[o + 18] = 1.0
        kb[o + 19] = 1.0
    t["kbt"] = kb.astype(BF)
    tq = 2048 + np.arange(T)
    qb = np.zeros((64, 4, T), np.float32)
    for p in range(4):
        for e in range(2):
            hh = 2 * p + e
            slope = 2.0 ** (-(hh + 1))
            o = 32 * e
            qb[o + 16, p] = slope
            qb[o + 17, p] = slope
            qb[o + 18, p] = -slope * ((tq // 64) * 64)
            qb[o + 19, p] = -slope * (tq % 64)
    t["qbs"] = qb.astype(BF)
    return t


def run_main(inp, l, xs, kvres, stop_after=None):
    key = "main" if stop_after is None else "main_" + stop_after
    if key not in _PROGS:
        _PROGS[key] = Builder("main", stop_after).build()
    nc = _PROGS[key]
    st = static_tables()
    gv = make_gv(inp, l)
    wsT = np.ascontiguousarray(inp["w_s"][l].transpose(2, 0, 1))
    bsb = np.ascontiguousarray(np.broadcast_to(inp["b_s"][l][None], (128, 4, 128))).astype(np.float32)
    common = {"gv": gv, "ones_bf": st["ones_bf"], "ident": st["ident"], "blkones": st["blkones"],
              "trilT": st["trilT"], "cmask": st["cmask"], "kbt": st["kbt"], "qbs": st["qbs"],
              "wsT": wsT, "bsb": bsb}
    for k_ in ("w_in", "w_out", "w_xq", "w_xkv", "w_xo", "w_ff1", "w_ff2"):
        common[k_] = np.ascontiguousarray(inp[k_][l])
    in_maps = []
    for c in range(8):
        b, half = c // 2, c % 2
        ra, rb = kvres[2 * b], kvres[2 * b + 1]
        kt = np.zeros((128, 4, SEQ), BF)
        vr = np.zeros((128, 32, 512), BF)
        ks = np.zeros((128, 4, 16), np.float32)
        gb = np.zeros((128, 16), np.float32)
        yh = np.zeros((128, 2, 32), BF)
        if half == 1:
            kt[:, :, :T] = ra["kt_o"]
            kt[:, :, T:] = rb["kt_o"]
            vr[:, :16] = ra["v_o"]
            vr[:, 16:] = rb["v_o"]
            ks[:, :, :8] = ra["ks_o"]
            ks[:, :, 8:] = rb["ks_o"]
            yh[:] = ra["yh_o"]
        else:
            kt[:, :, T:] = ra["kt_o"]
            vr[:, 16:] = ra["v_o"]
            ks[:, :, 8:] = ra["ks_o"]
            gb[:, :8] = -1e30
        m = dict(common)
        m.update({"xT": xs[c], "memT": to_fm(inp["mem"][b]), "kt_rel": kt, "v_rel": vr, "ks_rel": ks, "gb": gb,
                  "yh_in": yh})
        in_maps.append(m)
    return nc, in_maps


def kernel_unfused(**inp):
    inp = {k: np.asarray(v) for k, v in inp.items()}
    x = inp["x"]
    xs = [to_fm(x[c // 2, (c % 2) * T:(c % 2 + 1) * T]) for c in range(8)]
    for l in range(DEPTH):
        kvres = run_kv(inp, l, xs)
        nc, in_maps = run_main(inp, l, xs, kvres)
        res = run_bass_kernel_spmd(nc, in_maps, core_ids=list(range(8)))
        xs = [np.asarray(res.results[c]["x_o"]) for c in range(8)]
    out = np.zeros_like(x)
    for c in range(8):
        out[c // 2, (c % 2) * T:(c % 2 + 1) * T] = from_fm(xs[c])
    return out


def fused_inputs(inp, cores=range(8)):
    st = static_tables()
    x = inp["x"]
    gv = np.ascontiguousarray(np.stack([make_gv(inp, l) for l in range(DEPTH)], axis=1))
    wsT = np.ascontiguousarray(inp["w_s"].transpose(0, 3, 1, 2))
    bsb = np.ascontiguousarray(np.broadcast_to(inp["b_s"][:, None], (DEPTH, 128, 4, 128))).astype(np.float32)
    common = {"gv": gv, "ones_bf": st["ones_bf"], "ident": st["ident"], "blkones": st["blkones"],
              "trilT": st["trilT"], "cmask": st["cmask"], "kbt": st["kbt"], "qbs": st["qbs"], "wsT": wsT, "bsb": bsb}
    for k_ in ("w_in", "w_out", "w_xq", "w_xkv", "w_xo", "w_ff1", "w_ff2"):
        common[k_] = np.ascontiguousarray(inp[k_])
    in_maps = []
    for c in cores:
        b, half = c // 2, c % 2
        gb = np.zeros((128, 16), np.float32)
        if half == 0:
            gb[:, :8] = -1e30
        hsc = np.full((128, 1), float(half), np.float32)
        m = dict(common)
        m.update({"xT": to_fm(x[b, half * T:(half + 1) * T]), "memT": to_fm(inp["mem"][b]), "gb": gb, "hsc": hsc})
        in_maps.append(m)
    return in_maps


def kernel_fused(**inp):
    inp = {k: np.asarray(v) for k, v in inp.items()}
    if "fused" not in _PROGS:
        b = Builder("fused")
        _PROGS["fused"] = b.build_fused()
    nc = _PROGS["fused"]
    in_maps = fused_inputs(inp)
    res = run_bass_kernel_spmd(nc, in_maps, core_ids=list(range(8)))
    x = inp["x"]
    out = np.zeros_like(x)
    for c in range(8):
        out[c // 2, (c % 2) * T:(c % 2 + 1) * T] = from_fm(np.asarray(res.results[c]["x_o"]))
    return out


def kernel(**inp):
    return kernel_fused(**inp)
```

```python
import numpy as np
import ml_dtypes
from contextlib import ExitStack
import concourse.bass as bass
import concourse.mybir as mybir
from concourse.bass_utils import run_bass_kernel_spmd

F32 = mybir.dt.float32
BF16 = mybir.dt.bfloat16
AF = mybir.ActivationFunctionType
ALU = mybir.AluOpType
AX = mybir.AxisListType

D = 1024
NCH = 8
T = 2048
NT = 4
TT = 512
SEQ = 4096
DEPTH = 4
EPS = 1e-6
NEG = -30000.0
NSLOT = 6

GV_PRE_MIX, GV_POST_MIX, GV_PRE_X, GV_MEM, GV_POST_X, GV_PRE_FFN, GV_POST_FFN = 0, 8, 16, 24, 32, 40, 48
GV_LN_G, GV_LN_B, GV_BDW, GV_GN_G, GV_GN_B, GV_WDW = 56, 58, 60, 62, 64, 66
NGV = 66 + 62


class Buf:
    __slots__ = ("name", "w", "rd", "sem", "cnt")

    def __init__(self, name):
        self.name = name
        self.w = None
        self.rd = {}
        self.sem = None
        self.cnt = 0


class Sched:
    def __init__(self, nc, es):
        self.nc = nc
        self.es = es
        self.engs = {"pe": nc.tensor, "act": nc.scalar, "dve": nc.vector, "pool": nc.gpsimd, "sp": nc.sync}
        self.esem = {}
        self.ecnt = {}
        for e in ("pe", "act", "dve", "pool"):
            self.esem[e] = es.enter_context(nc.semaphore("sem_" + e))
            self.ecnt[e] = 0
        self.known = {e: {} for e in self.engs}
        self.dma_sems = {}
        self.nsem = 0
        import os
        self.nops = 0
        self.limit = int(os.environ.get('OPCUT', '0'))

    def buf(self, name):
        return Buf(name)

    def _collect(self, eng, r, w):
        need = {}

        def add(tok):
            if tok is None:
                return
            s, v, src = tok
            if src == "pe" and eng == "pe":
                return
            if need.get(s, 0) < v:
                need[s] = v

        for b in r:
            add(b.w)
        for b in w:
            add(b.w)
            for s, (v, src) in b.rd.items():
                add((s, v, src))
        return need

    def _wait(self, eng, need):
        kn = self.known[eng]
        e = self.engs[eng]
        for s, v in need.items():
            if kn.get(s, 0) < v:
                e.wait_ge(s, v)
                kn[s] = v

    def _record(self, tok, r, w):
        s, v, src = tok
        for b in r:
            old = b.rd.get(s)
            if old is None or old[0] < v:
                b.rd[s] = (v, src)
        for b in w:
            b.w = tok
            b.rd = {}

    def op(self, eng, fn, r=(), w=()):
        self.nops += 1
        if self.limit and self.nops > self.limit:
            return None
        need = self._collect(eng, r, w)
        self._wait(eng, need)
        ins = fn()
        self.ecnt[eng] += 1
        s = self.esem[eng]
        ins.then_inc(s, 1)
        tok = (s, self.ecnt[eng], eng)
        self._record(tok, r, w)
        return tok

    def dma(self, queue, fn, r=(), w=(), semkey=None, persistent=False, inc=16, extra=()):
        self.nops += 1
        if self.limit and self.nops > self.limit:
            return None
        need = self._collect("dma_" + queue, r, w)
        for tk in extra:
            if tk is not None and need.get(tk[0], 0) < tk[1]:
                need[tk[0]] = tk[1]
        self._wait(queue, need)
        kb = semkey if semkey is not None else w[0]
        if kb.sem is None:
            kb.sem = self.es.enter_context(self.nc.semaphore("dsem%d" % self.nsem))
            self.nsem += 1
            self.dma_sems[kb.sem] = [0, persistent]
        ins = fn()
        kb.cnt += inc
        ins.then_inc(kb.sem, inc)
        self.dma_sems[kb.sem][0] = kb.cnt
        tok = (kb.sem, kb.cnt, "dma")
        self._record(tok, r, w)
        return tok

    def barrier(self):
        need = {}
        for e in ("pe", "act", "dve", "pool"):
            if self.ecnt[e] > 0:
                need[self.esem[e]] = self.ecnt[e]
        for s, (c, pers) in self.dma_sems.items():
            if c > 0 and not pers:
                need[s] = c
        for e in ("pe", "act", "dve", "sp"):
            self._wait(e, need)

    def final_wait(self, eng, toks):
        need = {}
        toks = [t for t in toks if t is not None]
        for (s, v, _) in toks:
            need[s] = max(need.get(s, 0), v)
        self._wait(eng, need)


class GV:
    def __init__(self, t, l):
        self.t, self.l = t, l

    def __getitem__(self, idx):
        rows, cols = idx
        return self.t[rows, self.l, cols]


class StopBuild(Exception):
    pass


class Builder:
    def __init__(self, mode, stop_after=None):
        self.stop_after = stop_after
        self.mode = mode
        self.nc = bass.Bass("TRN2", target_bir_lowering=False)
        self.es = ExitStack()

    def sb(self, name, shape, dt, es=None):
        self.nsb = getattr(self, "nsb", 0) + 1
        return (es or self.es).enter_context(self.nc.sbuf_tensor("s_%s_%d" % (name, self.nsb), shape, dt))

    def din(self, name, shape, dt=F32):
        return self.nc.dram_tensor(name, shape, dt, kind="ExternalInput").ap()

    def dout(self, name, shape, dt=F32):
        return self.nc.dram_tensor(name, shape, dt, kind="ExternalOutput").ap()

    def bank(self):
        i = self.bank_i
        self.bank_i = (i + 1) % self.nrot
        return i

    def ring_load(self, src_ap, shape3):
        nc, S = self.nc, self.S
        i = self.ring_i
        self.ring_i = (i + 1) % NSLOT
        a, b = shape3
        view = self.ring[:, i, 0:a * b].rearrange("p (a b) -> p a b", a=a)
        buf = self.ring_b[i]
        if self.mode != "fused":
            S.dma("pool", lambda: nc.gpsimd.dma_start(out=view, in_=src_ap), w=[buf], persistent=True)
            return view, buf
        for k8 in range(8):
            j = self.stg_i
            self.stg_i = 1 - j
            row, col = (k8 * 512) // b, (k8 * 512) % b
            S.dma("sp", lambda j=j, row=row, col=col: nc.sync.dma_start(out=self.stg[:, j, :],
                                                                        in_=src_ap[:, row, col:col + 512]),
                  w=[self.stgb[j]], persistent=True)
            S.op("pool", lambda j=j, k8=k8, i=i: nc.gpsimd.tensor_copy(out=self.ring[:, i, k8 * 512:(k8 + 1) * 512],
                                                                       in_=self.stg[:, j, :]),
                 r=[self.stgb[j]], w=[buf])
        return view, buf

    def rms_rstd(self, src_ap, src_bufs, n, sq_t, sq_b, rstd_t, rstd_b, nch=NCH, scale=1.0 / D):
        nc, S = self.nc, self.S
        for c in range(nch):
            S.op("act", lambda c=c: nc.scalar.activation(out=sq_t[:, c, 0:n], in_=src_ap[:, c, :], func=AF.Square),
                 r=src_bufs, w=[sq_b])
        bk = self.bank()
        ps = self.ps[:, bk, 0:n]

        def mm():
            for c in range(nch):
                ins = nc.tensor.matmul(ps, lhsT=self.ones_bf[:, :], rhs=sq_t[:, c, 0:n],
                                       start=(c == 0), stop=(c == nch - 1))
            return ins
        S.op("pe", mm, r=[sq_b, self.cb], w=[self.pb[bk]])
        S.op("dve", lambda: nc.vector.tensor_scalar(out=rstd_t[:, 0:n], in0=ps, scalar1=scale, scalar2=EPS,
                                                    op0=ALU.mult, op1=ALU.add), r=[self.pb[bk]], w=[rstd_b])
        S.op("act", lambda: nc.scalar.activation(out=rstd_t[:, 0:n], in_=rstd_t[:, 0:n], func=AF.Sqrt),
             r=[rstd_b], w=[rstd_b])
        import os
        if os.environ.get('KVCUT') == '3':
            raise StopBuild()
        S.op("dve", lambda: nc.vector.reciprocal(out=rstd_t[:, 0:n], in_=rstd_t[:, 0:n]), r=[rstd_b], w=[rstd_b])
        if os.environ.get('KVCUT') == '4':
            raise StopBuild()

    def normalize(self, src_fn, src_bufs, gcol, out_fn, out_bufs, rstd_t, rstd_b, n, nch=NCH):
        nc, S = self.nc, self.S
        for c in range(nch):
            S.op("dve", lambda c=c: nc.vector.scalar_tensor_tensor(
                out=out_fn(c), in0=src_fn(c), scalar=self.gv[:, gcol + c:gcol + c + 1], in1=rstd_t[:, 0:n],
                op0=ALU.mult, op1=ALU.mult), r=list(src_bufs) + [rstd_b, self.cb], w=out_bufs)

    def post_norm_residual(self, y_t, y_b, tt, gcol, sq_t, sq_b, rstd_t, rstd_b):
        nc, S = self.nc, self.S
        self.rms_rstd(y_t[:, :, :], [y_b], TT, sq_t, sq_b, rstd_t, rstd_b)
        sl = slice(tt * TT, (tt + 1) * TT)
        for c in range(NCH):
            S.op("dve", lambda c=c: nc.vector.tensor_tensor(out=y_t[:, c, :], in0=y_t[:, c, :], in1=rstd_t[:, :],
                                                            op=ALU.mult), r=[y_b, rstd_b], w=[y_b])
            S.op("dve", lambda c=c: nc.vector.scalar_tensor_tensor(
                out=self.x[:, c, sl], in0=y_t[:, c, :], scalar=self.gv[:, gcol + c:gcol + c + 1],
                in1=self.x[:, c, sl], op0=ALU.mult, op1=ALU.add), r=[y_b, self.cb, self.xb[tt]], w=[self.xb[tt]])

    def proj_fm(self, w_view, w_buf, col0, h_fn, h_bufs, n, kch=NCH):
        nc, S = self.nc, self.S
        bk = self.bank()
        ps = self.ps[:, bk, 0:n]

        def mm():
            for k in range(kch):
                ins = nc.tensor.matmul(ps, lhsT=w_view[:, k, col0:col0 + 128], rhs=h_fn(k),
                                       start=(k == 0), stop=(k == kch - 1))
            return ins
        S.op("pe", mm, r=[w_buf] + list(h_bufs), w=[self.pb[bk]])
        return bk

    def proj_tm(self, w_view, w_buf, col0, ncols, h_fn, h_bufs, kch=NCH):
        nc, S = self.nc, self.S
        bk = self.bank()
        ps = self.ps[:, bk, 0:ncols]

        def mm():
            for k in range(kch):
                ins = nc.tensor.matmul(ps, lhsT=h_fn(k), rhs=w_view[:, k, col0:col0 + ncols],
                                       start=(k == 0), stop=(k == kch - 1))
            return ins
        S.op("pe", mm, r=[w_buf] + list(h_bufs), w=[self.pb[bk]])
        return bk

    def build_fused(self, nlayers=DEPTH, ncores=8):
        nc, es = self.nc, self.es
        with es:
            self.S = S = Sched(nc, es)
            self.bank_i = 0
            self.nrot = 8
            self.ring_i = 0
            NL = nlayers
            xT_d = self.din("xT", [128, NCH, T])
            memT_d = self.din("memT", [128, NCH, 256])
            gv_d = self.din("gv", [128, DEPTH, NGV])
            ones_d = self.din("ones_bf", [128, 128], BF16)
            gb_d = self.din("gb", [128, 16])
            hsc_d = self.din("hsc", [128, 1])
            wsT_d = self.din("wsT", [DEPTH, 128, 4, 128])
            bsb_d = self.din("bsb", [DEPTH, 128, 4, 128])
            w_in_d = self.din("w_in", [DEPTH, D, 2560])
            w_out_d = self.din("w_out", [DEPTH, D, D])
            w_xq_d = self.din("w_xq", [DEPTH, D, D])
            w_xkv_d = self.din("w_xkv", [DEPTH, D, 2 * D])
            w_xo_d = self.din("w_xo", [DEPTH, D, D])
            w_ff1_d = self.din("w_ff1", [DEPTH, D, 4 * D])
            w_ff2_d = self.din("w_ff2", [DEPTH, 4 * D, D])
            ident_d = self.din("ident", [128, 128])
            blk_d = self.din("blkones", [128, 128], BF16)
            tril_d = self.din("trilT", [128, 128])
            cmask_d = self.din("cmask", [128, 4, 512], BF16)
            kbt_d = self.din("kbt", [64, SEQ], BF16)
            qbs_d = self.din("qbs", [64, 4, T], BF16)
            x_o = self.dout("x_o", [128, NCH, T])
            CW = [1024] * 8 + [48]
            xin = [[nc.dram_tensor("xin%d_%d" % (i, j), [128, CW[j]], F32).ap() for j in range(9)] for i in range(2)]
            gath = [[nc.dram_tensor("gath%d_%d" % (i, j), [256, CW[j]], F32).ap() for j in range(9)] for i in range(2)]
            xin_b = [[S.buf("xin%d_%d" % (i, j)) for j in range(9)] for i in range(2)]
            gath_b = [[S.buf("gath%d_%d" % (i, j)) for j in range(9)] for i in range(2)]
            groups = [[2 * i, 2 * i + 1] for i in range(ncores // 2)]

            self.x = self.sb("x", [128, NCH, T], F32)
            self.xb = [S.buf("x%d" % i) for i in range(NT)]
            gvall = self.sb("gvall", [128, DEPTH, NGV], F32)
            self.ones_bf = self.sb("ones", [128, 128], BF16)
            self.eps_t = self.sb("eps", [128, 1], F32)
            self.cb = S.buf("consts")
            self.ring = self.sb("ring", [128, NSLOT, 4096], BF16)
            self.ring_b = [S.buf("ring%d" % i) for i in range(NSLOT)]
            self.stg = self.sb("stg", [128, 2, 512], F32)
            self.stgb = [S.buf("stg0"), S.buf("stg1")]
            self.stg_i = 0
            self.ps = es.enter_context(nc.psum_tensor("ps", [128, 8, 512], F32))
            self.pb = [S.buf("bank%d" % i) for i in range(8)]
            for tt in range(NT):
                S.dma("sp", lambda tt=tt: nc.sync.dma_start(out=self.x[:, :, tt * TT:(tt + 1) * TT],
                                                            in_=xT_d[:, :, tt * TT:(tt + 1) * TT]), w=[self.xb[tt]])
            S.dma("sp", lambda: nc.sync.dma_start(out=gvall[:, :, :], in_=gv_d), w=[self.cb])
            S.dma("sp", lambda: nc.sync.dma_start(out=self.ones_bf[:, :], in_=ones_d), w=[self.cb])
            S.op("dve", lambda: nc.vector.memset(self.eps_t[:, :], EPS), w=[self.cb])
            for l in range(NL):
                L = {"layer": l, "nlayers": NL, "x_o": x_o, "gv": GV(gvall, l),
                     "w_in_v": w_in_d[l].rearrange("(k p) n -> p k n", p=128),
                     "w_out_d": w_out_d[l], "w_xq_d": w_xq_d[l], "w_xkv_d": w_xkv_d[l], "w_xo_d": w_xo_d[l],
                     "w_ff1_d": w_ff1_d[l], "w_ff2_d": w_ff2_d[l], "wsT_d": wsT_d[l], "bsb_d": bsb_d[l],
                     "ident_d": ident_d, "blk_d": blk_d, "tril_d": tril_d, "cmask_d": cmask_d, "kbt_d": kbt_d,
                     "qbs_d": qbs_d, "gb_d": gb_d, "hsc_d": hsc_d, "memT_d": memT_d,
                     "xin": xin, "gath": gath, "xin_b": xin_b, "gath_b": gath_b, "groups": groups}
                self.build_main(L)
            print("fused nops", S.nops, "nsem", S.nsem)
        return nc

    def build(self):
        nc, es = self.nc, self.es
        mode = self.mode
        with es:
            self.S = S = Sched(nc, es)
            import os
            self.bank_i = int(os.environ.get('BANKSHIFT', '0'))
            self.nrot = 8
            self.ring_i = 0
            self.okey = None
            xT_d = self.din("xT", [128, NCH, T])
            gv_d = self.din("gv", [128, NGV])
            w_in_d = self.din("w_in", [D, 2560])
            ones_d = self.din("ones_bf", [128, 128], BF16)
            if mode == "kv":
                kt_o = self.dout("kt_o", [128, 4, T], BF16)
                v_o = self.dout("v_o", [128, 16, 512], BF16)
                ks_o = self.dout("ks_o", [128, 4, 8])
                yh_o = self.dout("yh_o", [128, 2, 32], BF16)
            else:
                memT_d = self.din("memT", [128, NCH, 256])
                kt_d = self.din("kt_rel", [128, 4, SEQ], BF16)
                v_d = self.din("v_rel", [128, 32, 512], BF16)
                ks_d = self.din("ks_rel", [128, 4, 16])
                yh_d = self.din("yh_in", [128, 2, 32], BF16)
                gb_d = self.din("gb", [128, 16])
                wsT_d = self.din("wsT", [128, 4, 128])
                bsb_d = self.din("bsb", [128, 4, 128])
                w_out_d = self.din("w_out", [D, D])
                w_xq_d = self.din("w_xq", [D, D])
                w_xkv_d = self.din("w_xkv", [D, 2 * D])
                w_xo_d = self.din("w_xo", [D, D])
                w_ff1_d = self.din("w_ff1", [D, 4 * D])
                w_ff2_d = self.din("w_ff2", [4 * D, D])
                ident_d = self.din("ident", [128, 128])
                blk_d = self.din("blkones", [128, 128], BF16)
                tril_d = self.din("trilT", [128, 128])
                cmask_d = self.din("cmask", [128, 4, 512], BF16)
                kbt_d = self.din("kbt", [64, SEQ], BF16)
                qbs_d = self.din("qbs", [64, 4, T], BF16)
                x_o = self.dout("x_o", [128, NCH, T])
                if self.stop_after in ("p1", "p2a"):
                    dbg_o = self.dout("dbg_o", [128, 8, T], BF16)

            self.x = self.sb("x", [128, NCH, T], F32)
            self.xb = [S.buf("x%d" % i) for i in range(NT)]
            self.gv = self.sb("gv", [128, NGV], F32)
            self.ones_bf = self.sb("ones", [128, 128], BF16)
            self.eps_t = self.sb("eps", [128, 1], F32)
            self.cb = S.buf("consts")
            self.ring = self.sb("ring", [128, NSLOT, 4096], BF16)
            self.ring_b = [S.buf("ring%d" % i) for i in range(NSLOT)]
            self.ps = es.enter_context(nc.psum_tensor("ps", [128, 8, 512], F32))
            self.pb = [S.buf("bank%d" % i) for i in range(8)]

            for tt in range(NT):
                S.dma("sp", lambda tt=tt: nc.sync.dma_start(out=self.x[:, :, tt * TT:(tt + 1) * TT],
                                                            in_=xT_d[:, :, tt * TT:(tt + 1) * TT]), w=[self.xb[tt]])
            S.dma("sp", lambda: nc.sync.dma_start(out=self.gv[:, :], in_=gv_d), w=[self.cb])
            S.dma("sp", lambda: nc.sync.dma_start(out=self.ones_bf[:, :], in_=ones_d), w=[self.cb])
            S.op("dve", lambda: nc.vector.memset(self.eps_t[:, :], EPS), w=[self.cb])
            w_in_v = w_in_d.rearrange("(k p) n -> p k n", p=128)
            import os
            if os.environ.get('KVCUT') == '2':
                S.barrier()
                return nc

            if mode == "kv":
                try:
                    self.build_kv(w_in_v, kt_o, v_o, ks_o, yh_o)
                except StopBuild:
                    S.barrier()
            else:
                self.build_main(locals())
        return nc

    def p1_norm(self, les, gcol):
        nc, S = self.nc, self.S
        self.hT = self.sb("hT", [128, NCH, T], BF16, les)
        self.hb = [S.buf("h%d" % i) for i in range(NT)]
        self.sq = self.sb("sq", [128, NCH, TT], BF16, les)
        self.sqb = S.buf("sq")
        self.rstd = self.sb("rstd", [128, TT], F32, les)
        self.rstdb = S.buf("rstd")
        for tt in range(NT):
            sl = slice(tt * TT, (tt + 1) * TT)
            self.rms_rstd(self.x[:, :, sl], [self.xb[tt]], TT, self.sq, self.sqb, self.rstd, self.rstdb)
            self.normalize(lambda c: self.x[:, c, sl], [self.xb[tt]], gcol, lambda c: self.hT[:, c, sl],
                           [self.hb[tt]], self.rstd, self.rstdb, TT)

    def h_fn(self, tt, lo=0, n=TT):
        return lambda k: self.hT[:, k, tt * TT + lo:tt * TT + lo + n]

    def glu_block(self, wv, wb, tt, lo, n, y_out_ap, y_bufs, sig_t, sig_b):
        nc, S = self.nc, self.S
        for c in range(2):
            bg = self.proj_fm(wv, wb, 256 + 128 * c, self.h_fn(tt, lo, n), [self.hb[tt]], n)
            S.op("act", lambda bg=bg: nc.scalar.activation(out=sig_t[:, 0:n], in_=self.ps[:, bg, 0:n],
                                                           func=AF.Sigmoid), r=[self.pb[bg]], w=[sig_b])
            ba = self.proj_fm(wv, wb, 128 * c, self.h_fn(tt, lo, n), [self.hb[tt]], n)
            S.op("dve", lambda ba=ba, c=c: nc.vector.tensor_tensor(out=y_out_ap(c), in0=self.ps[:, ba, 0:n],
                                                                   in1=sig_t[:, 0:n], op=ALU.mult),
                 r=[self.pb[ba], sig_b], w=y_bufs)

    def build_kv(self, w_in_v, kt_o, v_o, ks_o, yh_o):
        nc, S = self.nc, self.S
        with ExitStack() as les:
            self.p1_norm(les, GV_PRE_MIX)
            import os
            if os.environ.get('KVCUT') == '1':
                S.barrier()
                return
            kst = self.sb("kst", [128, 4, TT], BF16, les)
            kstb = S.buf("kst")
            ksum = self.sb("ksum", [128, 4, 8], F32, les)
            ksumb = S.buf("ksum")
            vst = self.sb("vst", [128, 4, 512], BF16, les)
            vstb = S.buf("vst")
            sig = self.sb("sig", [128, TT], F32, les)
            sigb = S.buf("sig")
            yh = self.sb("yh", [128, 2, 32], BF16, les)
            yhb = S.buf("yh")
            outs = []
            wv, wb = self.ring_load(w_in_v[:, :, 1024:1536], (8, 512))
            for tt in range(NT):
                for c in range(4):
                    bk = self.proj_fm(wv, wb, 128 * c, self.h_fn(tt), [self.hb[tt]], TT)
                    S.op("act", lambda bk=bk, c=c: nc.scalar.copy(out=kst[:, c, :], in_=self.ps[:, bk, :]),
                         r=[self.pb[bk]], w=[kstb, self.pb[bk]])
                    if os.environ.get('KVCUT') == '5':
                        S.barrier()
                        return
                    S.op("dve", lambda bk=bk, c=c, tt=tt: nc.vector.tensor_reduce(
                        out=ksum[:, c, 2 * tt:2 * tt + 2], in_=self.ps[:, bk, :].rearrange("p (a b) -> p a b", a=2),
                        axis=AX.X, op=ALU.add), r=[self.pb[bk]], w=[ksumb])
                outs.append(S.dma("sp", lambda tt=tt: nc.sync.dma_start(out=kt_o[:, :, tt * TT:(tt + 1) * TT],
                                                                        in_=kst[:, :, :]), r=[kstb],
                                  semkey=kstb))
            outs.append(S.dma("sp", lambda: nc.sync.dma_start(out=ks_o, in_=ksum[:, :, :]), r=[ksumb],
                              semkey=ksumb))
            wv, wb = self.ring_load(w_in_v[:, :, 1536:2048], (8, 512))
            for tt in range(NT):
                for s in range(4):
                    bk = self.proj_tm(wv, wb, 0, 512, self.h_fn(tt, 128 * s, 128), [self.hb[tt]])
                    S.op("act", lambda bk=bk, s=s: nc.scalar.copy(out=vst[:, s, :], in_=self.ps[:, bk, :]),
                         r=[self.pb[bk]], w=[vstb])
                outs.append(S.dma("sp", lambda tt=tt: nc.sync.dma_start(out=v_o[:, 4 * tt:4 * tt + 4, :],
                                                                        in_=vst[:, :, :]), r=[vstb],
                                  semkey=vstb))
            wv, wb = self.ring_load(w_in_v[:, :, 2048:2560], (8, 512))
            self.glu_block(wv, wb, 3, TT - 32, 32, lambda c: yh[:, c, :], [yhb], sig, sigb)
            outs.append(S.dma("sp", lambda: nc.sync.dma_start(out=yh_o, in_=yh[:, :, :]), r=[yhb], semkey=yhb))
            S.final_wait("sp", outs)
            if S.limit:
                S.barrier()
            print('KV nops', S.nops)


    def post_norm_res(self, y3, y_b, tt, gcol, sq_t, sq_b, rstd_t, rstd_b):
        nc, S = self.nc, self.S
        self.rms_rstd(y3, [y_b], TT, sq_t, sq_b, rstd_t, rstd_b)
        sl = slice(tt * TT, (tt + 1) * TT)
        for c in range(NCH):
            S.op("dve", lambda c=c: nc.vector.tensor_tensor(out=y3[:, c, :], in0=y3[:, c, :], in1=rstd_t[:, :],
                                                            op=ALU.mult), r=[y_b, rstd_b], w=[y_b])
            S.op("dve", lambda c=c: nc.vector.scalar_tensor_tensor(
                out=self.x[:, c, sl], in0=y3[:, c, :], scalar=self.gv[:, gcol + c:gcol + c + 1],
                in1=self.x[:, c, sl], op0=ALU.mult, op1=ALU.add), r=[y_b, self.cb, self.xb[tt]], w=[self.xb[tt]])

    def evac(self, bk, out_ap, out_bufs, n=TT, func=None, scale=1.0, eng="act"):
        nc, S = self.nc, self.S
        if eng == "act":
            S.op("act", lambda: nc.scalar.activation(out=out_ap, in_=self.ps[:, bk, 0:n],
                                                     func=(func or AF.Copy), scale=scale),
                 r=[self.pb[bk]], w=out_bufs)
        else:
            S.op("dve", lambda: nc.vector.tensor_copy(out=out_ap, in_=self.ps[:, bk, 0:n]),
                 r=[self.pb[bk]], w=out_bufs)

    def build_main(self, L):
        nc, S = self.nc, self.S
        x_o = L["x_o"]
        w_in_v = L["w_in_v"]
        if "gv" in L:
            self.gv = L["gv"]
        vw = lambda d: d.rearrange("(k p) n -> p k n", p=128)
        w_out_v, w_xq_v, w_xkv_v, w_xo_v = vw(L["w_out_d"]), vw(L["w_xq_d"]), vw(L["w_xkv_d"]), vw(L["w_xo_d"])
        w_ff1_v, w_ff2_v = vw(L["w_ff1_d"]), vw(L["w_ff2_d"])
        fused = self.mode == "fused"
        lyr = L.get("layer", 0)
        if not getattr(self, "consts_done", False):
            self.consts_done = True
            self.ident = self.sb("ident", [128, 128], F32)
            self.blk = self.sb("blk", [128, 128], BF16)
            self.onesE = self.sb("onesE", [128, 128], BF16)
            self.onesO = self.sb("onesO", [128, 128], BF16)
            self.gbt = self.sb("gb", [128, 16], F32)
            self.hsc = self.sb("hsc", [128, 1], F32)
            S.dma("sp", lambda: nc.sync.dma_start(out=self.ident[:, :], in_=L["ident_d"]), w=[self.cb])
            S.dma("sp", lambda: nc.sync.dma_start(out=self.blk[:, :], in_=L["blk_d"]), w=[self.cb])
            S.dma("sp", lambda: nc.sync.dma_start(out=self.gbt[:, :], in_=L["gb_d"]), w=[self.cb])
            if fused:
                S.dma("sp", lambda: nc.sync.dma_start(out=self.hsc[:, :], in_=L["hsc_d"]), w=[self.cb])
            S.op("dve", lambda: nc.vector.memset(self.onesE[:, :], 0.0), w=[self.cb])
            S.op("dve", lambda: nc.vector.memset(self.onesO[:, :], 0.0), w=[self.cb])
            S.op("dve", lambda: nc.vector.memset(self.onesE[:, 0:64], 1.0), w=[self.cb])
            S.op("dve", lambda: nc.vector.memset(self.onesO[:, 64:128], 1.0), w=[self.cb])
        ident, blk, onesE, onesO, gb = self.ident, self.blk, self.onesE, self.onesO, self.gbt
        if fused:
            par = lyr % 2
            xin, gath = L["xin"][par], L["gath"][par]
            xin_b, gath_b = L["xin_b"][par], L["gath_b"][par]
            xb16 = [a.bitcast(BF16) for a in xin]
            gb16 = [a.bitcast(BF16)[0:128, :] for a in gath]
            xinK = lambda c: xb16[c]
            xinV = lambda t_: xb16[4 + t_].rearrange("p (s f) -> p s f", s=4)
            xinS = xb16[8][:, 0:32].rearrange("p (c b) -> p c b", c=4)
            xinH = xb16[8][:, 32:96].rearrange("p (c j) -> p c j", c=2)
            g0K = lambda c: gb16[c]
            g0V = lambda t_: gb16[4 + t_].rearrange("p (s f) -> p s f", s=4)
            g0S = gb16[8][:, 0:32].rearrange("p (c b) -> p c b", c=4)
            g0H = gb16[8][:, 32:96].rearrange("p (c j) -> p c j", c=2)

        with ExitStack() as lay:
            yaT = self.sb("yaT", [128, 2, T], BF16, lay)
            qT = self.sb("qT", [128, 4, T], BF16, lay)
            ycT = self.sb("ycT", [128, 2, T], BF16, lay)
            yab = [S.buf("ya%d" % i) for i in range(NT)]
            qb_ = [[S.buf("q%d_%d" % (i, p)) for p in range(4)] for i in range(NT)]
            ycb = [S.buf("yc%d" % i) for i in range(NT)]
            with ExitStack() as les:
                h = self.sb("h", [128, NCH, TT], BF16, les)
                hb = S.buf("h")
                sq = self.sb("sq", [128, NCH, TT], BF16, les)
                sqb = S.buf("sq")
                rstd = self.sb("rstd", [128, TT], F32, les)
                rstdb = S.buf("rstd")
                sig = self.sb("sig", [128, TT], F32, les)
                sigb = S.buf("sig")
                ybuf = self.sb("ybuf", [128, 2, 32 + T], BF16, les)
                ybb = [S.buf("yb%d" % i) for i in range(NT)]
                yhb = S.buf("yhalo")
                acc = self.sb("acc", [128, 2, TT], F32, les)
                accb = S.buf("acc")
                accq = self.sb("accq", [128, 2, 2, TT], BF16, les)
                accqb = S.buf("accq")
                mean = self.sb("mean", [128, TT], F32, les)
                meanb = S.buf("mean")
                var = self.sb("var", [128, TT], F32, les)
                varb = S.buf("var")
                u = self.sb("u", [128, 2, TT], BF16, les)
                ub = S.buf("u")
                vg = self.sb("vg", [128, 256], F32, les)
                vgb = S.buf("vg")
                vsq = self.sb("vsq", [128, 256], F32, les)
                vsqb = S.buf("vsq")
                vhat = self.sb("vhat", [128, 256], BF16, les)
                vhatb = S.buf("vhat")
                st = self.sb("st", [128, 8], F32, les)
                stb = S.buf("st")
                mixt = self.sb("mixt", [128, 128], F32, les)
                mixb = S.buf("mixt")
                wsf = self.sb("wsf", [128, 4, 128], F32, les)
                wsb = self.sb("wsb", [128, 4, 128], BF16, les)
                trilT = self.sb("trilT", [128, 128], F32, les)
                bsb = self.sb("bsb", [128, 4, 128], F32, les)
                cbias = self.sb("cbias", [128, 2, 128], F32, les)
                gmb = S.buf("gmlp_consts")
                S.dma("sp", lambda: nc.sync.dma_start(out=wsf[:, :, :], in_=L["wsT_d"]), w=[gmb])
                S.dma("sp", lambda: nc.sync.dma_start(out=trilT[:, :], in_=L["tril_d"]), w=[gmb])
                S.dma("sp", lambda: nc.sync.dma_start(out=bsb[:, :, :], in_=L["bsb_d"]), w=[gmb])
                if not fused:
                    S.dma("sp", lambda: nc.sync.dma_start(out=ybuf[:, :, 0:32], in_=L["yh_d"]), w=[yhb])
                else:
                    kst = self.sb("kst", [128, 4, TT], BF16, les)
                    kstb = S.buf("kst")
                    vst = kst
                    vstb = kstb
                    ksum = self.sb("ksum", [128, 4, 8], F32, les)
                    ksumb = S.buf("ksum")
                    ksb16 = self.sb("ksb16", [128, 4, 8], BF16, les)
                    ksb16b = S.buf("ksb16")
                    xtoks = [[] for _ in range(9)]
                    kstk = [S.buf("kstk%d" % c) for c in range(4)]
                for hh in range(4):
                    S.op("dve", lambda hh=hh: nc.vector.tensor_tensor(out=wsb[:, hh, :], in0=wsf[:, hh, :],
                                                                      in1=trilT[:, :], op=ALU.mult), r=[gmb], w=[gmb])
                for hh in range(4):
                    bk = self.bank()
                    S.op("pe", lambda hh=hh, bk=bk: nc.tensor.matmul(self.ps[:, bk, 0:128], lhsT=self.ones_bf[:, :],
                                                                     rhs=wsb[:, hh, :], start=True, stop=True),
                         r=[gmb, self.cb], w=[self.pb[bk]])
                    c, e = hh // 2, hh % 2
                    rs = slice(64 * e, 64 * e + 64)
                    S.op("dve", lambda hh=hh, bk=bk, c=c, rs=rs: nc.vector.scalar_tensor_tensor(
                        out=cbias[rs, c, :], in0=self.ps[rs, bk, 0:128], scalar=self.gv[rs, GV_LN_B + c:GV_LN_B + c + 1],
                        in1=bsb[rs, hh, :], op0=ALU.mult, op1=ALU.add), r=[self.pb[bk], gmb, self.cb], w=[gmb])

                wq_v, wq_b = self.ring_load(w_in_v[:, :, 512:1024], (8, 512))
                wu_v, wu_b = self.ring_load(w_in_v[:, :, 0:512], (8, 512))
                wc_v, wc_b = self.ring_load(w_in_v[:, :, 2048:2560], (8, 512))
                if fused:
                    wk_v, wk_b = self.ring_load(w_in_v[:, :, 1024:1536], (8, 512))
                    wvv_v, wvv_b = self.ring_load(w_in_v[:, :, 1536:2048], (8, 512))
                hf = lambda lo=0, n=TT: (lambda k: h[:, k, lo:lo + n])
                for tt in range(NT):
                    sl = slice(tt * TT, (tt + 1) * TT)
                    self.rms_rstd(self.x[:, :, sl], [self.xb[tt]], TT, sq, sqb, rstd, rstdb)
                    self.normalize(lambda c: self.x[:, c, sl], [self.xb[tt]], GV_PRE_MIX, lambda c: h[:, c, :], [hb],
                                   rstd, rstdb, TT)
                    for c in range(4):
                        bk = self.proj_fm(wq_v, wq_b, 128 * c, hf(), [hb], TT)
                        self.evac(bk, qT[:, c, sl], [qb_[tt][c]], scale=0.125)
                    if fused:
                        for c in range(4):
                            bk = self.proj_fm(wk_v, wk_b, 128 * c, hf(), [hb], TT)
                            S.op("act", lambda bk=bk, c=c: nc.scalar.copy(out=kst[:, c, :], in_=self.ps[:, bk, :]),
                                 r=[self.pb[bk]], w=[kstb, self.pb[bk]])
                            S.op("dve", lambda bk=bk, c=c, tt=tt: nc.vector.tensor_reduce(
                                out=ksum[:, c, 2 * tt:2 * tt + 2],
                                in_=self.ps[:, bk, :].rearrange("p (a b) -> p a b", a=2), axis=AX.X, op=ALU.add),
                                r=[self.pb[bk]], w=[ksumb])
                        for c in range(4):
                            xtoks[c].append(S.dma("sp", lambda sl=sl, c=c: nc.sync.dma_start(out=xinK(c)[:, sl],
                                                                                             in_=kst[:, c, :]),
                                                  r=[kstb], w=[xin_b[c]], semkey=kstk[c]))
                        for s4 in range(4):
                            bk = self.proj_tm(wvv_v, wvv_b, 0, 512, hf(128 * s4, 128), [hb])
                            self.evac(bk, vst[:, s4, :], [vstb])
                        xtoks[4 + tt].append(S.dma("sp", lambda tt=tt: nc.sync.dma_start(out=xinV(tt), in_=vst[:, :, :]),
                                                   r=[vstb], w=[xin_b[4 + tt]], semkey=vstb))
                    for c in range(2):
                        bk = self.proj_fm(wu_v, wu_b, 128 * c, hf(), [hb], TT)
                        self.evac(bk, u[:, c, :], [ub], func=AF.Gelu)
                    for s4 in range(4):
                        bk = self.proj_tm(wu_v, wu_b, 256, 256, hf(128 * s4, 128), [hb])
                        S.op("act", lambda bk=bk: nc.scalar.activation(out=vg[:, :], in_=self.ps[:, bk, 0:256],
                                                                       func=AF.Gelu), r=[self.pb[bk]], w=[vgb])
                        S.op("dve", lambda: nc.vector.tensor_reduce(out=st[:, 0:1], in_=vg[:, :], axis=AX.X, op=ALU.add),
                             r=[vgb], w=[stb])
                        S.op("dve", lambda: nc.vector.tensor_tensor(out=vsq[:, :], in0=vg[:, :], in1=vg[:, :],
                                                                    op=ALU.mult), r=[vgb], w=[vsqb])
                        S.op("dve", lambda: nc.vector.tensor_reduce(out=st[:, 1:2], in_=vsq[:, :], axis=AX.X, op=ALU.add),
                             r=[vsqb, stb], w=[stb])
                        S.op("dve", lambda: nc.vector.tensor_scalar(out=st[:, 2:3], in0=st[:, 0:1], scalar1=1.0 / 256,
                                                                    scalar2=None, op0=ALU.mult), r=[stb], w=[stb])
                        S.op("dve", lambda: nc.vector.tensor_tensor(out=st[:, 3:4], in0=st[:, 2:3], in1=st[:, 2:3],
                                                                    op=ALU.mult), r=[stb], w=[stb])
                        S.op("dve", lambda: nc.vector.scalar_tensor_tensor(out=st[:, 4:5], in0=st[:, 1:2],
                                                                           scalar=1.0 / 256, in1=st[:, 3:4],
                                                                           op0=ALU.mult, op1=ALU.subtract),
                             r=[stb], w=[stb])
                        S.op("act", lambda: nc.scalar.activation(out=st[:, 5:6], in_=st[:, 4:5], func=AF.Sqrt,
                                                                 bias=self.eps_t[:, 0:1], scale=1.0),
                             r=[stb, self.cb], w=[stb])
                        S.op("dve", lambda: nc.vector.reciprocal(out=st[:, 6:7], in_=st[:, 5:6]), r=[stb], w=[stb])
                        S.op("dve", lambda: nc.vector.tensor_scalar(out=vhat[:, :], in0=vg[:, :], scalar1=st[:, 2:3],
                                                                    scalar2=st[:, 6:7], op0=ALU.subtract, op1=ALU.mult),
                             r=[vgb, stb], w=[vhatb])
                        for c in range(2):
                            for e in range(2):
                                hh = 2 * c + e
                                rs = slice(64 * e, 64 * e + 64)
                                bk = self.bank()
                                S.op("pe", lambda bk=bk, c=c, hh=hh: nc.tensor.matmul(
                                    self.ps[:, bk, 0:128], lhsT=vhat[:, 128 * c:128 * c + 128], rhs=wsb[:, hh, :],
                                    start=True, stop=True), r=[vhatb, gmb], w=[self.pb[bk]])
                                S.op("dve", lambda bk=bk, c=c, rs=rs: nc.vector.scalar_tensor_tensor(
                                    out=mixt[rs, :], in0=self.ps[rs, bk, 0:128],
                                    scalar=self.gv[rs, GV_LN_G + c:GV_LN_G + c + 1], in1=cbias[rs, c, :],
                                    op0=ALU.mult, op1=ALU.add), r=[self.pb[bk], gmb, self.cb], w=[mixb])
                            tsl = slice(tt * TT + 128 * s4, tt * TT + 128 * s4 + 128)
                            S.op("dve", lambda c=c, tsl=tsl, s4=s4: nc.vector.tensor_tensor(
                                out=yaT[:, c, tsl], in0=u[:, c, 128 * s4:128 * s4 + 128], in1=mixt[:, :], op=ALU.mult),
                                r=[ub, mixb], w=[yab[tt]])
                    self.hT = h
                    self.hb = {tt: hb}
                    self.h_fn = lambda tt_, lo=0, n=TT: (lambda k: h[:, k, lo:lo + n])
                    self.glu_block(wc_v, wc_b, tt, 0, TT, lambda c: ybuf[:, c, 32 + tt * TT:32 + (tt + 1) * TT],
                                   [ybb[tt]], sig, sigb)
                if fused:
                    S.op("act", lambda: nc.scalar.copy(out=ksb16[:, :, :], in_=ksum[:, :, :]), r=[ksumb], w=[ksb16b])
                    xtoks[8].append(S.dma("sp", lambda: nc.sync.dma_start(out=xinS, in_=ksb16[:, :, :]),
                                          r=[ksb16b], w=[xin_b[8]], semkey=ksb16b))
                    xtoks[8].append(S.dma("sp", lambda: nc.sync.dma_start(out=xinH, in_=ybuf[:, :, T:T + 32]),
                                          r=[ybb[3]], w=[xin_b[8]], semkey=yhb))
                    for j9 in (8, 0, 1, 2, 3, 4, 5, 6, 7):
                        S.dma("pool", lambda j9=j9: nc.gpsimd.collective_compute(
                            "AllGather", ALU.bypass, replica_groups=L["groups"], ins=[xin[j9]], outs=[gath[j9]]),
                            r=[xin_b[j9]], w=[gath_b[j9]], inc=1, extra=xtoks[j9])
                    S.dma("sp", lambda: nc.sync.dma_start(out=ybuf[:, :, 0:32], in_=g0H), r=[gath_b[8]], w=[yhb])
                    S.op("dve", lambda: nc.vector.tensor_scalar(out=ybuf[:, :, 0:32], in0=ybuf[:, :, 0:32],
                                                                scalar1=self.hsc[:, 0:1], scalar2=None, op0=ALU.mult),
                         r=[yhb, self.cb], w=[yhb])
                for tt in range(NT):
                    sl = slice(tt * TT, (tt + 1) * TT)
                    rbufs = [ybb[tt]] + ([ybb[tt - 1]] if tt > 0 else [yhb])
                    for c in range(2):
                        for k in range(31):
                            src = ybuf[:, c, 32 + tt * TT - 30 + k:32 + tt * TT - 30 + k + TT]
                            wk = self.gv[:, GV_WDW + 2 * k + c:GV_WDW + 2 * k + c + 1]
                            if k == 0:
                                S.op("dve", lambda c=c, src=src, wk=wk: nc.vector.tensor_scalar(
                                    out=acc[:, c, :], in0=src, scalar1=wk, scalar2=self.gv[:, GV_BDW + c:GV_BDW + c + 1],
                                    op0=ALU.mult, op1=ALU.add), r=rbufs + [self.cb], w=[accb])
                            else:
                                S.op("dve", lambda c=c, src=src, wk=wk: nc.vector.scalar_tensor_tensor(
                                    out=acc[:, c, :], in0=src, scalar=wk, in1=acc[:, c, :], op0=ALU.mult, op1=ALU.add),
                                    r=rbufs + [self.cb, accb], w=[accb])
                    S.op("act", lambda: nc.scalar.copy(out=accq[:, 0, :, :], in_=acc[:, :, :]), r=[accb], w=[accqb])
                    S.op("act", lambda: nc.scalar.activation(out=accq[:, 1, :, :], in_=acc[:, :, :], func=AF.Square),
                         r=[accb], w=[accqb])
                    for c in range(2):
                        b1 = self.bank()
                        S.op("pe", lambda b1=b1, c=c: nc.tensor.matmul(self.ps[:, b1, :], lhsT=blk[:, :],
                                                                       rhs=accq[:, 0, c, :], start=True, stop=True),
                             r=[accqb, self.cb], w=[self.pb[b1]])
                        b2 = self.bank()
                        S.op("pe", lambda b2=b2, c=c: nc.tensor.matmul(self.ps[:, b2, :], lhsT=blk[:, :],
                                                                       rhs=accq[:, 1, c, :], start=True, stop=True),
                             r=[accqb, self.cb], w=[self.pb[b2]])
                        S.op("dve", lambda b1=b1: nc.vector.tensor_scalar(out=mean[:, :], in0=self.ps[:, b1, :],
                                                                          scalar1=1.0 / 64, scalar2=None, op0=ALU.mult),
                             r=[self.pb[b1]], w=[meanb])
                        S.op("dve", lambda: nc.vector.tensor_tensor(out=var[:, :], in0=mean[:, :], in1=mean[:, :],
                                                                    op=ALU.mult), r=[meanb], w=[varb])
                        S.op("dve", lambda b2=b2: nc.vector.scalar_tensor_tensor(
                            out=var[:, :], in0=self.ps[:, b2, :], scalar=1.0 / 64, in1=var[:, :], op0=ALU.mult,
                            op1=ALU.subtract), r=[self.pb[b2], varb], w=[varb])
                        S.op("act", lambda: nc.scalar.activation(out=var[:, :], in_=var[:, :], func=AF.Sqrt,
                                                                 bias=self.eps_t[:, 0:1], scale=1.0),
                             r=[varb, self.cb], w=[varb])
                        S.op("dve", lambda: nc.vector.reciprocal(out=var[:, :], in_=var[:, :]), r=[varb], w=[varb])
                        S.op("dve", lambda c=c: nc.vector.tensor_tensor(out=acc[:, c, :], in0=acc[:, c, :],
                                                                        in1=mean[:, :], op=ALU.subtract),
                             r=[accb, meanb], w=[accb])
                        S.op("dve", lambda c=c: nc.vector.tensor_tensor(out=acc[:, c, :], in0=acc[:, c, :],
                                                                        in1=var[:, :], op=ALU.mult),
                             r=[accb, varb], w=[accb])
                        S.op("act", lambda c=c, sl=sl: nc.scalar.activation(
                            out=ycT[:, c, sl], in_=acc[:, c, :], func=AF.Silu,
                            bias=self.gv[:, GV_GN_B + c:GV_GN_B + c + 1], scale=self.gv[:, GV_GN_G + c:GV_GN_G + c + 1]),
                            r=[accb, self.cb], w=[ycb[tt]])
            S.barrier()
            if self.stop_after == "p1":
                return self.debug_out(L, yaT, qT, ycT)
            with ExitStack() as les:
                ktp = self.sb("ktp", [128, SEQ], BF16, les)
                ktpb = S.buf("ktp")
                vpe = self.sb("vpe", [128, 32, 128], BF16, les)
                vpo = self.sb("vpo", [128, 32, 128], BF16, les)
                vpb = S.buf("vp")
                kbt = self.sb("kbt", [64, SEQ], BF16, les)
                cmask = self.sb("cmask", [128, 4, 512], BF16, les)
                mcb = S.buf("moba_consts")
                qbt = self.sb("qbt", [64, T], BF16, les)
                qbtb = [S.buf("qbt%d" % i) for i in range(NT)]
                ksf = self.sb("ksf", [128, 16], F32, les)
                ksh = self.sb("ksh", [128, 16], BF16, les)
                kmb = self.sb("kmb", [128, 16], BF16, les)
                kmbb = S.buf("kmb")
                gt = self.sb("gt", [128, 16], F32, les)
                gtb = S.buf("gt")
                m8 = self.sb("m8", [128, 8], F32, les)
                m8b = S.buf("m8")
                mbt = self.sb("mbt", [128, 64], F32, les)
                mbtb = S.buf("mbt")
                NPT = 4
                pt = self.sb("pt", [128, NPT, TT], BF16, les)
                ptb = [S.buf("pt%d" % i) for i in range(NPT)]
                rec = self.sb("rec", [128, TT], F32, les)
                recb = S.buf("rec")
                S.dma("sp", lambda: nc.sync.dma_start(out=kbt[:, :], in_=L["kbt_d"]), w=[mcb])
                S.dma("sp", lambda: nc.sync.dma_start(out=cmask[:, :, :], in_=L["cmask_d"]), w=[mcb])
                S.op("dve", lambda: nc.vector.memset(vpe[:, :, :], 0.0), w=[vpb])
                S.op("dve", lambda: nc.vector.memset(vpo[:, :, :], 0.0), w=[vpb])
                S.op("dve", lambda: nc.vector.memset(mbt[:, :], NEG), w=[mbtb])
                pti = 0
                for p in range(4):
                    if not fused:
                        S.dma("sp", lambda p=p: nc.sync.dma_start(out=ktp[:, :], in_=L["kt_d"][:, p, :]), w=[ktpb])
                        for g4 in range(4):
                            S.dma("sp", lambda p=p, g4=g4: nc.sync.dma_start(
                                out=vpe[:, 8 * g4:8 * g4 + 8, 0:64],
                                in_=L["v_d"][:, 8 * g4:8 * g4 + 8, 128 * p:128 * p + 64]), w=[vpb])
                            S.dma("sp", lambda p=p, g4=g4: nc.sync.dma_start(
                                out=vpo[:, 8 * g4:8 * g4 + 8, 64:128],
                                in_=L["v_d"][:, 8 * g4:8 * g4 + 8, 128 * p + 64:128 * p + 128]), w=[vpb])
                        S.dma("sp", lambda p=p: nc.sync.dma_start(out=ksf[:, :], in_=L["ks_d"][:, p, :]), w=[kmbb])
                        S.op("act", lambda: nc.scalar.activation(out=kmb[:, :], in_=ksf[:, :], func=AF.Copy,
                                                                 scale=1.0 / 32), r=[kmbb], w=[kmbb])
                    else:
                        S.dma("sp", lambda p=p: nc.sync.dma_start(out=ktp[:, 0:T], in_=g0K(p)),
                              r=[gath_b[p]], w=[ktpb])
                        S.dma("sp", lambda p=p: nc.sync.dma_start(out=ktp[:, T:SEQ], in_=xinK(p)),
                              r=[xin_b[p]], w=[ktpb])
                        for r4 in range(8):
                            if r4 < 4:
                                src, sb_ = g0V(r4), gath_b[4 + r4]
                            else:
                                src, sb_ = xinV(r4 - 4), xin_b[4 + r4 - 4]
                            S.dma("sp", lambda p=p, r4=r4, src=src: nc.sync.dma_start(
                                out=vpe[:, 4 * r4:4 * r4 + 4, 0:64], in_=src[:, :, 128 * p:128 * p + 64]),
                                r=[sb_], w=[vpb])
                            S.dma("sp", lambda p=p, r4=r4, src=src: nc.sync.dma_start(
                                out=vpo[:, 4 * r4:4 * r4 + 4, 64:128], in_=src[:, :, 128 * p + 64:128 * p + 128]),
                                r=[sb_], w=[vpb])
                        S.dma("sp", lambda p=p: nc.sync.dma_start(out=ksh[:, 0:8], in_=g0S[:, p, :]),
                              r=[gath_b[8]], w=[kmbb])
                        S.dma("sp", lambda p=p: nc.sync.dma_start(out=ksh[:, 8:16], in_=xinS[:, p, :]),
                              r=[xin_b[8]], w=[kmbb])
                        S.op("act", lambda: nc.scalar.activation(out=kmb[:, :], in_=ksh[:, :], func=AF.Copy,
                                                                 scale=1.0 / 32), r=[kmbb], w=[kmbb])
                    S.dma("sp", lambda p=p: nc.sync.dma_start(out=qbt[:, :], in_=L["qbs_d"][:, p, :]), w=qbtb)
                    for tt in range(NT):
                        bT = self.bank()
                        for s4 in range(4):
                            own = 8 + 2 * tt + (1 if s4 >= 2 else 0)
                            tsl = slice(tt * TT + 128 * s4, tt * TT + 128 * s4 + 128)
                            for e in range(2):
                                rs = slice(64 * e, 64 * e + 64)
                                off = 32 * e
                                bk = self.bank()
                                if bk == bT:
                                    bk = self.bank()
                                S.op("pe", lambda bk=bk, rs=rs, tsl=tsl, p=p: nc.tensor.matmul(
                                    self.ps[:, bk, 0:16], lhsT=qT[rs, p, tsl], rhs=kmb[rs, :], start=True, stop=True),
                                    r=[qb_[tt][p], kmbb], w=[self.pb[bk]])
                                S.op("dve", lambda bk=bk, own=own: nc.vector.tensor_tensor(
                                    out=gt[:, 0:own], in0=self.ps[:, bk, 0:own], in1=gb[:, 0:own], op=ALU.add),
                                    r=[self.pb[bk], self.cb], w=[gtb])
                                S.op("dve", lambda own=own: nc.vector.max(out=m8[:, :], in_=gt[:, 0:own]),
                                     r=[gtb], w=[m8b])
                                S.op("dve", lambda own=own, off=off: nc.vector.tensor_scalar(
                                    out=mbt[:, off:off + own], in0=gt[:, 0:own], scalar1=m8[:, 2:3], scalar2=NEG,
                                    op0=ALU.is_lt, op1=ALU.mult), r=[gtb, m8b], w=[mbtb])
                                S.op("dve", lambda own=own, off=off: nc.vector.tensor_tensor(
                                    out=mbt[:, off:off + own], in0=mbt[:, off:off + own], in1=gb[:, 0:own], op=ALU.add),
                                    r=[mbtb, self.cb], w=[mbtb])
                                S.op("dve", lambda own=own, off=off: nc.vector.memset(mbt[:, off + own:off + own + 1], 0.0),
                                     r=[], w=[mbtb])
                                if own + 1 < 16:
                                    S.op("dve", lambda own=own, off=off: nc.vector.memset(
                                        mbt[:, off + own + 1:off + 16], NEG), r=[], w=[mbtb])
                            S.op("pe", lambda bT=bT, s4=s4: nc.tensor.transpose(
                                self.ps[0:64, bT, 128 * s4:128 * s4 + 128], mbt[:, :], ident[:, :]),
                                r=[mbtb, self.cb], w=[self.pb[bT]])
                        sl = slice(tt * TT, (tt + 1) * TT)
                        S.op("act", lambda bT=bT, sl=sl: nc.scalar.copy(out=qbt[0:16, sl], in_=self.ps[0:16, bT, :]),
                             r=[self.pb[bT]], w=[qbtb[tt]])
                        S.op("act", lambda bT=bT, sl=sl: nc.scalar.copy(out=qbt[32:48, sl], in_=self.ps[32:48, bT, :]),
                             r=[self.pb[bT]], w=[qbtb[tt]])
                    self.nrot = 6
                    if self.bank_i >= 6:
                        self.bank_i = 0
                    for tt in range(NT):
                        sl = slice(tt * TT, (tt + 1) * TT)
                        nkt = 20 + 4 * tt
                        OB, DB = 6, 7
                        units = [(kt, e) for kt in range(nkt) for e in range(2)]
                        pend = None

                        def emit_pv(kt, e, pi, first, last):
                            vv = vpe if e == 0 else vpo
                            oo = onesE if e == 0 else onesO

                            def mm2(vv=vv, oo=oo, kt=kt, pi=pi, first=first, last=last):
                                nc.tensor.matmul(self.ps[:, OB, :], lhsT=vv[:, kt, :], rhs=pt[:, pi, :],
                                                 start=first, stop=last)
                                return nc.tensor.matmul(self.ps[:, DB, :], lhsT=oo[:, :], rhs=pt[:, pi, :],
                                                        start=first, stop=last)
                            S.op("pe", mm2, r=[vpb, ptb[pi], self.cb], w=[self.pb[OB], self.pb[DB]])

                        for ui, (kt, e) in enumerate(units):
                            ksl = slice(128 * kt, 128 * kt + 128)
                            d = kt - (16 + 4 * tt)
                            rs = slice(64 * e, 64 * e + 64)
                            bs = slice(32 * e, 32 * e + 20)
                            bk = self.bank()

                            def mm(bk=bk, rs=rs, bs=bs, ksl=ksl, sl=sl, p=p):
                                nc.tensor.matmul(self.ps[:, bk, :], lhsT=ktp[rs, ksl], rhs=qT[rs, p, sl],
                                                 start=True, stop=False)
                                return nc.tensor.matmul(self.ps[:, bk, :], lhsT=kbt[bs, ksl], rhs=qbt[bs, sl],
                                                        start=False, stop=True)
                            S.op("pe", mm, r=[ktpb, qb_[tt][p], mcb, qbtb[tt]], w=[self.pb[bk]])
                            pi = pti
                            pti = (pti + 1) % NPT
                            if d >= 0:
                                S.op("dve", lambda bk=bk, d=d: nc.vector.tensor_tensor(
                                    out=self.ps[:, bk, :], in0=self.ps[:, bk, :], in1=cmask[:, d, :], op=ALU.add),
                                    r=[self.pb[bk], mcb], w=[self.pb[bk]])
                            S.op("act", lambda bk=bk, pi=pi: nc.scalar.activation(
                                out=pt[:, pi, :], in_=self.ps[:, bk, :], func=AF.Exp), r=[self.pb[bk]], w=[ptb[pi]])
                            if pend is not None:
                                emit_pv(*pend)
                            pend = (kt, e, pi, ui == 0, ui == len(units) - 1)
                        emit_pv(*pend)
                        S.op("dve", lambda: nc.vector.reciprocal(out=rec[:, :], in_=self.ps[:, DB, :]),
                             r=[self.pb[DB]], w=[recb])
                        S.op("dve", lambda p=p, sl=sl: nc.vector.tensor_tensor(
                            out=qT[:, p, sl], in0=self.ps[:, OB, :], in1=rec[:, :], op=ALU.mult),
                            r=[self.pb[OB], recb], w=[qb_[tt][p]])
                    self.nrot = 8
            S.barrier()
            if self.stop_after == "p2a":
                return self.debug_out(L, yaT, qT, ycT)
            with ExitStack() as les:
                yt = self.sb("yt", [128, NCH, TT], F32, les)
                ytb = S.buf("yt")
                sq = self.sb("sq2", [128, NCH, TT], BF16, les)
                sqb = S.buf("sq2")
                rstd = self.sb("rstd2", [128, TT], F32, les)
                rstdb = S.buf("rstd2")
                wo = [self.ring_load(w_out_v[:, :, 512 * j:512 * j + 512], (8, 512)) for j in range(2)]
                for tt in range(NT):
                    sl = slice(tt * TT, (tt + 1) * TT)

                    def cat(k, sl=sl):
                        if k < 2:
                            return yaT[:, k, sl]
                        if k < 6:
                            return qT[:, k - 2, sl]
                        return ycT[:, k - 6, sl]
                    cbufs = [yab[tt], ycb[tt]] + qb_[tt]
                    for oc in range(8):
                        wv, wb = wo[oc // 4]
                        bk = self.proj_fm(wv, wb, 128 * (oc % 4), cat, cbufs, TT)
                        self.evac(bk, yt[:, oc, :], [ytb])
                    self.post_norm_res(yt[:, :, :], ytb, tt, GV_POST_MIX, sq, sqb, rstd, rstdb)
            S.barrier()
        if self.stop_after == "p2":
            return self.finish(x_o)
        with ExitStack() as les:
            h = self.sb("h3", [128, NCH, TT], BF16, les)
            hb = S.buf("h3")
            sq = self.sb("sq3", [128, NCH, TT], BF16, les)
            sqb = S.buf("sq3")
            rstd = self.sb("rstd3", [128, TT], F32, les)
            rstdb = S.buf("rstd3")
            mT = self.sb("mT", [128, NCH, 256], F32, les)
            mTb = S.buf("mT")
            mh = self.sb("mh", [128, NCH, 256], BF16, les)
            mhb = S.buf("mh")
            kx = self.sb("kx", [128, NCH, 256], BF16, les)
            kxb = S.buf("kx")
            vx = self.sb("vx", [128, 2, D], BF16, les)
            vxb = S.buf("vx")
            qx = self.sb("qx", [128, NCH, TT], BF16, les)
            qxb = S.buf("qx")
            px = self.sb("px", [128, 2, 2, TT], BF16, les)
            pxb = [S.buf("px0"), S.buf("px1")]
            rec = self.sb("rec3", [128, TT], F32, les)
            recb = S.buf("rec3")
            ox = self.sb("ox", [128, NCH, TT], BF16, les)
            oxb = S.buf("ox")
            yt = self.sb("yt3", [128, NCH, TT], F32, les)
            ytb = S.buf("yt3")
            S.dma("sp", lambda: nc.sync.dma_start(out=mT[:, :, :], in_=L["memT_d"]), w=[mTb])
            self.rms_rstd(mT[:, :, :], [mTb], 256, sq, sqb, rstd, rstdb)
            self.normalize(lambda c: mT[:, c, :], [mTb], GV_MEM, lambda c: mh[:, c, :], [mhb], rstd, rstdb, 256)
            mf = lambda lo=0, n=256: (lambda k: mh[:, k, lo:lo + n])
            wkv = [self.ring_load(w_xkv_v[:, :, 512 * j:512 * j + 512], (8, 512)) for j in range(4)]
            for oc in range(8):
                wv, wb = wkv[oc // 4]
                bk = self.proj_fm(wv, wb, 128 * (oc % 4), mf(), [mhb], 256)
                self.evac(bk, kx[:, oc, :], [kxb], n=256, scale=1.0 / 16)
            for mt in range(2):
                for hf2 in range(2):
                    wv, wb = wkv[2 + hf2]
                    bk = self.proj_tm(wv, wb, 0, 512, mf(128 * mt, 128), [mhb])
                    self.evac(bk, vx[:, mt, 512 * hf2:512 * hf2 + 512], [vxb])
            wq = [self.ring_load(w_xq_v[:, :, 512 * j:512 * j + 512], (8, 512)) for j in range(2)]
            wo = [self.ring_load(w_xo_v[:, :, 512 * j:512 * j + 512], (8, 512)) for j in range(2)]
            pxi = 0
            for tt in range(NT):
                sl = slice(tt * TT, (tt + 1) * TT)
                self.rms_rstd(self.x[:, :, sl], [self.xb[tt]], TT, sq, sqb, rstd, rstdb)
                self.normalize(lambda c: self.x[:, c, sl], [self.xb[tt]], GV_PRE_X, lambda c: h[:, c, :], [hb],
                               rstd, rstdb, TT)
                hf = lambda k: h[:, k, :]
                for oc in range(8):
                    wv, wb = wq[oc // 4]
                    bk = self.proj_fm(wv, wb, 128 * (oc % 4), hf, [hb], TT)
                    self.evac(bk, qx[:, oc, :], [qxb])
                for hd in range(4):
                    pi = pxi
                    pxi = 1 - pxi
                    for mt in range(2):
                        bk = self.bank()

                        def mm(bk=bk, hd=hd, mt=mt):
                            nc.tensor.matmul(self.ps[:, bk, :], lhsT=kx[:, 2 * hd, 128 * mt:128 * mt + 128],
                                             rhs=qx[:, 2 * hd, :], start=True, stop=False)
                            return nc.tensor.matmul(self.ps[:, bk, :], lhsT=kx[:, 2 * hd + 1, 128 * mt:128 * mt + 128],
                                                    rhs=qx[:, 2 * hd + 1, :], start=False, stop=True)
                        S.op("pe", mm, r=[kxb, qxb], w=[self.pb[bk]])
                        S.op("act", lambda bk=bk, pi=pi, mt=mt: nc.scalar.activation(
                            out=px[:, pi, mt, :], in_=self.ps[:, bk, :], func=AF.Exp), r=[self.pb[bk]], w=[pxb[pi]])
                    bd = self.bank()

                    def mmd(bd=bd, pi=pi):
                        nc.tensor.matmul(self.ps[:, bd, :], lhsT=self.ones_bf[:, :], rhs=px[:, pi, 0, :],
                                         start=True, stop=False)
                        return nc.tensor.matmul(self.ps[:, bd, :], lhsT=self.ones_bf[:, :], rhs=px[:, pi, 1, :],
                                                start=False, stop=True)
                    S.op("pe", mmd, r=[pxb[pi], self.cb], w=[self.pb[bd]])
                    S.op("dve", lambda bd=bd: nc.vector.reciprocal(out=rec[:, :], in_=self.ps[:, bd, :]),
                         r=[self.pb[bd]], w=[recb])
                    for c in range(2):
                        bo = self.bank()

                        def mmo(bo=bo, pi=pi, hd=hd, c=c):
                            cc = (2 * hd + c) * 128
                            nc.tensor.matmul(self.ps[:, bo, :], lhsT=vx[:, 0, cc:cc + 128], rhs=px[:, pi, 0, :],
                                             start=True, stop=False)
                            return nc.tensor.matmul(self.ps[:, bo, :], lhsT=vx[:, 1, cc:cc + 128], rhs=px[:, pi, 1, :],
                                                    start=False, stop=True)
                        S.op("pe", mmo, r=[vxb, pxb[pi]], w=[self.pb[bo]])
                        S.op("dve", lambda bo=bo, hd=hd, c=c: nc.vector.tensor_tensor(
                            out=ox[:, 2 * hd + c, :], in0=self.ps[:, bo, :], in1=rec[:, :], op=ALU.mult),
                            r=[self.pb[bo], recb], w=[oxb])
                of = lambda k: ox[:, k, :]
                for oc in range(8):
                    wv, wb = wo[oc // 4]
                    bk = self.proj_fm(wv, wb, 128 * (oc % 4), of, [oxb], TT)
                    self.evac(bk, yt[:, oc, :], [ytb])
                self.post_norm_res(yt[:, :, :], ytb, tt, GV_POST_X, sq, sqb, rstd, rstdb)
        S.barrier()
        if self.stop_after == "p3":
            return self.finish(x_o)
        with ExitStack() as les:
            hh = self.sb("hf", [128, NCH, 2 * TT], BF16, les)
            hhb = [S.buf("hf0"), S.buf("hf1")]
            sq = self.sb("sq4", [128, NCH, TT], BF16, les)
            sqb = S.buf("sq4")
            rstd = self.sb("rstd4", [128, TT], F32, les)
            rstdb = S.buf("rstd4")
            yacc = self.sb("yacc", [128, NCH, 2 * TT], F32, les)
            yaccb = [S.buf("yacc0"), S.buf("yacc1")]
            ft = self.sb("ft", [128, 2, NCH, TT], BF16, les)
            ftb = [S.buf("ft0"), S.buf("ft1")]
            fi = 0
            for half in range(2):
                for j in range(2):
                    tt = 2 * half + j
                    sl = slice(tt * TT, (tt + 1) * TT)
                    jl = slice(j * TT, (j + 1) * TT)
                    self.rms_rstd(self.x[:, :, sl], [self.xb[tt]], TT, sq, sqb, rstd, rstdb)
                    self.normalize(lambda c: self.x[:, c, sl], [self.xb[tt]], GV_PRE_FFN, lambda c: hh[:, c, jl],
                                   [hhb[j]], rstd, rstdb, TT)
                for qtr in range(4):
                    w1 = [self.ring_load(w_ff1_v[:, :, 1024 * qtr + 512 * i:1024 * qtr + 512 * i + 512], (8, 512))
                          for i in range(2)]
                    w2 = [self.ring_load(w_ff2_v[:, 8 * qtr + 4 * i:8 * qtr + 4 * i + 4, :], (4, 1024))
                          for i in range(2)]
                    fs = []
                    for j in range(2):
                        jl = slice(j * TT, (j + 1) * TT)
                        hf = lambda k, jl=jl: hh[:, k, jl]
                        f = fi
                        fi = 1 - fi
                        fs.append(f)
                        for fc in range(8):
                            wv, wb = w1[fc // 4]
                            bk = self.proj_fm(wv, wb, 128 * (fc % 4), hf, [hhb[j]], TT)
                            self.evac(bk, ft[:, f, fc, :], [ftb[f]], func=AF.Relu)
                        S.op("dve", lambda f=f: nc.vector.tensor_tensor(out=ft[:, f, :, :], in0=ft[:, f, :, :],
                                                                        in1=ft[:, f, :, :], op=ALU.mult),
                             r=[ftb[f]], w=[ftb[f]])
                    for j in range(2):
                        jl = slice(j * TT, (j + 1) * TT)
                        f = fs[j]
                        for oc in range(8):
                            bk = self.bank()

                            def mm(bk=bk, oc=oc, f=f, w2=w2):
                                for kc in range(8):
                                    wv, _ = w2[kc // 4]
                                    ins = nc.tensor.matmul(self.ps[:, bk, :], lhsT=wv[:, kc % 4, 128 * oc:128 * oc + 128],
                                                           rhs=ft[:, f, kc, :], start=(kc == 0), stop=(kc == 7))
                                return ins
                            S.op("pe", mm, r=[w2[0][1], w2[1][1], ftb[f]], w=[self.pb[bk]])
                            if qtr == 0:
                                self.evac(bk, yacc[:, oc, jl], [yaccb[j]])
                            else:
                                S.op("dve", lambda bk=bk, oc=oc, jl=jl: nc.vector.tensor_tensor(
                                    out=yacc[:, oc, jl], in0=self.ps[:, bk, :], in1=yacc[:, oc, jl], op=ALU.add),
                                    r=[self.pb[bk], yaccb[j]], w=[yaccb[j]])
                for j in range(2):
                    tt = 2 * half + j
                    self.post_norm_res(yacc[:, :, j * TT:(j + 1) * TT], yaccb[j], tt, GV_POST_FFN, sq, sqb, rstd, rstdb)
        S.barrier()
        if fused and lyr < L["nlayers"] - 1:
            return
        return self.finish(x_o)

    def finish(self, x_o):
        nc, S = self.nc, self.S
        outs = []
        for tt in range(NT):
            outs.append(S.dma("sp", lambda tt=tt: nc.sync.dma_start(out=x_o[:, :, tt * TT:(tt + 1) * TT],
                                                                    in_=self.x[:, :, tt * TT:(tt + 1) * TT]),
                              r=[self.xb[tt]], semkey=self.xb[tt]))
        S.final_wait("sp", outs)

    def debug_out(self, L, yaT, qT, ycT):
        nc, S = self.nc, self.S
        dbg = L["dbg_o"]
        kb = Buf("dbg")
        t1 = S.dma("sp", lambda: nc.sync.dma_start(out=dbg[:, 0:2, :], in_=yaT[:, :, :]), semkey=kb)
        t2 = S.dma("sp", lambda: nc.sync.dma_start(out=dbg[:, 2:6, :], in_=qT[:, :, :]), semkey=kb)
        t3 = S.dma("sp", lambda: nc.sync.dma_start(out=dbg[:, 6:8, :], in_=ycT[:, :, :]), semkey=kb)
        S.final_wait("sp", [t3])


_PROGS = {}


def get_prog(mode):
    if mode not in _PROGS:
        _PROGS[mode] = Builder(mode).build()
    return _PROGS[mode]


def to_fm(a):
    t, f = a.shape
    return np.ascontiguousarray(a.reshape(t, f // 128, 128).transpose(2, 1, 0))


def from_fm(a):
    p, c, t = a.shape
    return np.ascontiguousarray(a.transpose(2, 1, 0).reshape(t, c * p))


def col(v):
    return np.ascontiguousarray(v.reshape(-1, 128).T)


def make_gv(inp, l):
    gv = np.zeros((128, NGV), np.float32)
    for off, key in ((GV_PRE_MIX, "pre_mix_g"), (GV_POST_MIX, "post_mix_g"), (GV_PRE_X, "pre_x_g"),
                     (GV_MEM, "mem_g"), (GV_POST_X, "post_x_g"), (GV_PRE_FFN, "pre_ffn_g"),
                     (GV_POST_FFN, "post_ffn_g")):
        gv[:, off:off + 8] = col(inp[key][l])
    for off, key in ((GV_LN_G, "gate_ln_g"), (GV_LN_B, "gate_ln_b"), (GV_BDW, "b_dw"), (GV_GN_G, "conv_gn_g"),
                     (GV_GN_B, "conv_gn_b")):
        gv[:, off:off + 2] = col(inp[key][l])
    wdw = inp["w_dw"][l]
    for k in range(31):
        gv[:, GV_WDW + 2 * k:GV_WDW + 2 * k + 2] = col(wdw[k])
    return gv


def run_kv(inp, l, xs):
    nc = get_prog("kv")
    ones = np.ones((128, 128), ml_dtypes.bfloat16)
    gv = make_gv(inp, l)
    w_in = np.ascontiguousarray(inp["w_in"][l])
    in_maps = [{"xT": xs[c], "gv": gv, "w_in": w_in, "ones_bf": ones} for c in range(8)]
    res = run_bass_kernel_spmd(nc, in_maps, core_ids=list(range(8)))
    return res.results


BF = ml_dtypes.bfloat16


def static_tables():
    t = {}
    t["ones_bf"] = np.ones((128, 128), BF)
    t["ident"] = np.eye(128, dtype=np.float32)
    blk = np.zeros((128, 128), np.float32)
    blk[:64, :64] = 1
    blk[64:, 64:] = 1
    t["blkones"] = blk.astype(BF)
    s_ = np.arange(128)[:, None]
    t_ = np.arange(128)[None, :]
    t["trilT"] = (s_ <= t_).astype(np.float32)
    k_ = np.arange(128)[:, None, None]
    d_ = np.arange(4)[None, :, None]
    q_ = np.arange(512)[None, None, :]
    t["cmask"] = np.where((128 * d_ + k_) <= q_, 0.0, NEG).astype(np.float32).astype(BF)
    s = np.arange(SEQ)
    kb = np.zeros((64, SEQ), np.float32)
    for e in range(2):
        o = 32 * e
        kb[o + (s // 256), s] = 1.0
        kb[o + 16] = (s // 64) * 64
        kb[o + 17] = s % 64
        kb[o + 18] = 1.0
        kb[o + 19] = 1.0
    t["kbt"] = kb.astype(BF)
    tq = 2048 + np.arange(T)
    qb = np.zeros((64, 4, T), np.float32)
    for p in range(4):
        for e in range(2):
            hh = 2 * p + e
            slope = 2.0 ** (-(hh + 1))
            o = 32 * e
            qb[o + 16, p] = slope
            qb[o + 17, p] = slope
            qb[o + 18, p] = -slope * ((tq // 64) * 64)
            qb[o + 19, p] = -slope * (tq % 64)
    t["qbs"] = qb.astype(BF)
    return t


def run_main(inp, l, xs, kvres, stop_after=None):
    key = "main" if stop_after is None else "main_" + stop_after
    if key not in _PROGS:
        _PROGS[key] = Builder("main", stop_after).build()
    nc = _PROGS[key]
    st = static_tables()
    gv = make_gv(inp, l)
    wsT = np.ascontiguousarray(inp["w_s"][l].transpose(2, 0, 1))
    bsb = np.ascontiguousarray(np.broadcast_to(inp["b_s"][l][None], (128, 4, 128))).astype(np.float32)
    common = {"gv": gv, "ones_bf": st["ones_bf"], "ident": st["ident"], "blkones": st["blkones"],
              "trilT": st["trilT"], "cmask": st["cmask"], "kbt": st["kbt"], "qbs": st["qbs"],
              "wsT": wsT, "bsb": bsb}
    for k_ in ("w_in", "w_out", "w_xq", "w_xkv", "w_xo", "w_ff1", "w_ff2"):
        common[k_] = np.ascontiguousarray(inp[k_][l])
    in_maps = []
    for c in range(8):
        b, half = c // 2, c % 2
        ra, rb = kvres[2 * b], kvres[2 * b + 1]
        kt = np.zeros((128, 4, SEQ), BF)
        vr = np.zeros((128, 32, 512), BF)
        ks = np.zeros((128, 4, 16), np.float32)
        gb = np.zeros((128, 16), np.float32)
        yh = np.zeros((128, 2, 32), BF)
        if half == 1:
            kt[:, :, :T] = ra["kt_o"]
            kt[:, :, T:] = rb["kt_o"]
            vr[:, :16] = ra["v_o"]
            vr[:, 16:] = rb["v_o"]
            ks[:, :, :8] = ra["ks_o"]
            ks[:, :, 8:] = rb["ks_o"]
            yh[:] = ra["yh_o"]
        else:
            kt[:, :, T:] = ra["kt_o"]
            vr[:, 16:] = ra["v_o"]
            ks[:, :, 8:] = ra["ks_o"]
            gb[:, :8] = -1e30
        m = dict(common)
        m.update({"xT": xs[c], "memT": to_fm(inp["mem"][b]), "kt_rel": kt, "v_rel": vr, "ks_rel": ks, "gb": gb,
                  "yh_in": yh})
        in_maps.append(m)
    return nc, in_maps


def kernel_unfused(**inp):
    inp = {k: np.asarray(v) for k, v in inp.items()}
    x = inp["x"]
    xs = [to_fm(x[c // 2, (c % 2) * T:(c % 2 + 1) * T]) for c in range(8)]
    for l in range(DEPTH):
        kvres = run_kv(inp, l, xs)
        nc, in_maps = run_main(inp, l, xs, kvres)
        res = run_bass_kernel_spmd(nc, in_maps, core_ids=list(range(8)))
        xs = [np.asarray(res.results[c]["x_o"]) for c in range(8)]
    out = np.zeros_like(x)
    for c in range(8):
        out[c // 2, (c % 2) * T:(c % 2 + 1) * T] = from_fm(xs[c])
    return out


def fused_inputs(inp, cores=range(8)):
    st = static_tables()
    x = inp["x"]
    gv = np.ascontiguousarray(np.stack([make_gv(inp, l) for l in range(DEPTH)], axis=1))
    wsT = np.ascontiguousarray(inp["w_s"].transpose(0, 3, 1, 2))
    bsb = np.ascontiguousarray(np.broadcast_to(inp["b_s"][:, None], (DEPTH, 128, 4, 128))).astype(np.float32)
    common = {"gv": gv, "ones_bf": st["ones_bf"], "ident": st["ident"], "blkones": st["blkones"],
              "trilT": st["trilT"], "cmask": st["cmask"], "kbt": st["kbt"], "qbs": st["qbs"], "wsT": wsT, "bsb": bsb}
    for k_ in ("w_in", "w_out", "w_xq", "w_xkv", "w_xo", "w_ff1", "w_ff2"):
        common[k_] = np.ascontiguousarray(inp[k_])
    in_maps = []
    for c in cores:
        b, half = c // 2, c % 2
        gb = np.zeros((128, 16), np.float32)
        if half == 0:
            gb[:, :8] = -1e30
        hsc = np.full((128, 1), float(half), np.float32)
        m = dict(common)
        m.update({"xT": to_fm(x[b, half * T:(half + 1) * T]), "memT": to_fm(inp["mem"][b]), "gb": gb, "hsc": hsc})
        in_maps.append(m)
    return in_maps


def kernel_fused(**inp):
    inp = {k: np.asarray(v) for k, v in inp.items()}
    if "fused" not in _PROGS:
        b = Builder("fused")
        _PROGS["fused"] = b.build_fused()
    nc = _PROGS["fused"]
    in_maps = fused_inputs(inp)
    res = run_bass_kernel_spmd(nc, in_maps, core_ids=list(range(8)))
    x = inp["x"]
    out = np.zeros_like(x)
    for c in range(8):
        out[c // 2, (c % 2) * T:(c % 2 + 1) * T] = from_fm(np.asarray(res.results[c]["x_o"]))
    return out


def kernel(**inp):
    return kernel_fused(**inp)
```
